# Optimizing a Trainium2 kernel written in Bass

```python
import jax
import jax.numpy as jnp
from jax import lax
import numpy as np

D_MODEL = 1024
BATCH = 8
SEQ = 4096
DEPTH = 2

GRID_W = 64
CTX_LEN = 256
N_EVEN = (DEPTH + 1) // 2
N_ODD = DEPTH // 2

RET_HEADS = 4
RET_DIM = 128
HGRN_HEADS = 4
HGRN_DIM = 128
RET_WIDTH = RET_HEADS * RET_DIM
HGRN_WIDTH = HGRN_HEADS * HGRN_DIM
MIX_IN_SIZES = [RET_WIDTH] * 4 + [HGRN_WIDTH] * 5
MIX_IN_COLS = sum(MIX_IN_SIZES)
MIX_IN_SPLITS = np.cumsum(MIX_IN_SIZES)[:-1].tolist()
RET_CHUNK = 128
HGRN_CHUNK = 64

MLA_HEADS = 8
MLA_NOPE = 128
MLA_ROPE = 64
MLA_V = 128
MLA_Q_RANK = 384
MLA_KV_RANK = 256
MLA_DOWN_COLS = MLA_Q_RANK + MLA_KV_RANK + MLA_ROPE
Q_BLOCK = 128

FFN_DIM = 2816
N_EXPERTS = 8
TOP_K = 2
MOE_DIM = 2816

ROPE_BASE = 10000.0
NORM_EPS = 1e-6

kernel_name = 'hybrid_retention_hgrn2_mla_moe_prefix_dit'


def rmsnorm(x, gain=None):
    xf = x.astype(jnp.float32)
    y = (xf * lax.rsqrt(jnp.mean(xf * xf, axis=-1, keepdims=True) + NORM_EPS)).astype(x.dtype)
    return y if gain is None else y * gain


def axial_rope_tables(n_rows, dim):
    rows = jnp.repeat(jnp.arange(n_rows), GRID_W).astype(jnp.float32)
    cols = jnp.tile(jnp.arange(GRID_W), n_rows).astype(jnp.float32)
    n_freq = dim // 4
    inv = ROPE_BASE ** (-jnp.arange(n_freq, dtype=jnp.float32) / n_freq)
    ang = jnp.concatenate([rows[:, None] * inv, cols[:, None] * inv], axis=-1)
    return jnp.cos(ang), jnp.sin(ang)


def apply_rope(x, cos, sin):
    half = x.shape[-1] // 2
    x1, x2 = x[..., :half], x[..., half:]
    cos = cos.astype(x.dtype)
    sin = sin.astype(x.dtype)
    return jnp.concatenate([x1 * cos - x2 * sin, x1 * sin + x2 * cos], axis=-1)


def _to_chunks(t, size):
    b, h, n, d = t.shape
    return jnp.moveaxis(t.astype(jnp.float32).reshape(b, h, n // size, size, d), 2, 0)


def _from_chunks(t):
    n, b, h, c, d = t.shape
    return jnp.moveaxis(t, 0, 2).reshape(b, h, n * c, d)


def retention_scan(q, k, v, log_gamma, s0):
    cs = RET_CHUNK
    pos = jnp.arange(cs, dtype=jnp.float32)
    lg = log_gamma.astype(jnp.float32)[:, None]
    rel = pos[:, None] - pos[None, :]
    intra = jnp.where(rel >= 0, jnp.exp(lg[:, :, None] * jnp.maximum(rel, 0.0)), 0.0)
    q_dec = jnp.exp(lg * (pos + 1.0))[:, :, None]
    k_dec = jnp.exp(lg * (cs - 1.0 - pos))[:, :, None]
    c_dec = jnp.exp(lg * cs)[:, :, None]

    def step(state, inp):
        qc, kc, vc = inp
        att = jnp.einsum('bhid,bhjd->bhij', qc, kc) * intra
        o = jnp.einsum('bhij,bhje->bhie', att, vc) + jnp.einsum('bhid,bhde->bhie', qc * q_dec, state)
        state = state * c_dec + jnp.einsum('bhjd,bhje->bhde', kc * k_dec, vc)
        return state, o

    state, o = lax.scan(step, s0, (_to_chunks(q, cs), _to_chunks(k, cs), _to_chunks(v, cs)))
    return _from_chunks(o), state


def gated_scan(q, k, v, log_f, s0):
    cs = HGRN_CHUNK
    lower = jnp.tril(jnp.ones((cs, cs), dtype=bool))[:, :, None]

    def step(state, inp):
        qc, kc, vc, gc = inp
        b = jnp.cumsum(gc, axis=2)
        diff = b[:, :, :, None, :] - b[:, :, None, :, :]
        w = jnp.exp(jnp.where(lower, diff, -jnp.inf))
        att = jnp.einsum('bhtsd,bhsd->bhts', qc[:, :, :, None, :] * w, kc)
        o = jnp.einsum('bhts,bhse->bhte', att, vc) + jnp.einsum('bhtd,bhde->bhte', qc * jnp.exp(b), state)
        b_last = b[:, :, -1:, :]
        state = jnp.exp(b_last[:, :, 0, :])[..., None] * state + jnp.einsum(
            'bhsd,bhse->bhde', kc * jnp.exp(b_last - b), vc)
        return state, o

    state, o = lax.scan(step, s0, (_to_chunks(q, cs), _to_chunks(k, cs), _to_chunks(v, cs),
                                   _to_chunks(log_f, cs)))
    return _from_chunks(o), state


def bidirectional_scan(scan_fn, directions, per_token_decay):
    out_c, out_x = 0.0, 0.0
    for reverse, (ctx_args, lat_args) in zip((False, True), directions):
        def orient(args):
            if not reverse:
                return args
            q, k, v, d = args
            fl = lambda t: jnp.flip(t, axis=2)
            return fl(q), fl(k), fl(v), (fl(d) if per_token_decay else d)
        qc, kc, vc, dc = orient(ctx_args)
        s0 = jnp.zeros(qc.shape[:2] + (qc.shape[-1], vc.shape[-1]), jnp.float32)
        o_c, s_c = scan_fn(qc, kc, vc, dc, s0)
        o_x, _ = scan_fn(*orient(lat_args), s_c)
        if reverse:
            o_c, o_x = jnp.flip(o_c, axis=2), jnp.flip(o_x, axis=2)
        out_c = out_c + o_c
        out_x = out_x + o_x
    return out_c, out_x


def _heads(t, d):
    b, n, _ = t.shape
    return t.reshape(b, n, -1, d).transpose(0, 2, 1, 3)


def retention_hgrn_mixer(hx, hc, w_in, ret_decay_logit, hgrn_lb, w_out, rope):
    lb = hgrn_lb.astype(jnp.float32).reshape(HGRN_HEADS, 1, HGRN_DIM)

    def project(h, rope_tab):
        aq, ak, av, ag, bq, bff, bfb, bi, bg = jnp.split(h @ w_in, MIX_IN_SPLITS, axis=-1)
        aq = _heads(aq, RET_DIM)
        ak = _heads(ak, RET_DIM) * (RET_DIM ** -0.5)
        if rope_tab is not None:
            aq = apply_rope(aq, *rope_tab)
            ak = apply_rope(ak, *rope_tab)

        def forget(z):
            f = lb + (1.0 - lb) * jax.nn.sigmoid(_heads(z, HGRN_DIM).astype(jnp.float32))
            return 1.0 - f, jnp.log(f)
        k_f, g_f = forget(bff)
        k_b, g_b = forget(bfb)
        bq = jax.nn.silu(_heads(bq, HGRN_DIM))
        bi = _heads(bi, HGRN_DIM)
        return (aq, ak, _heads(av, RET_DIM)), (bq, k_f, bi, g_f), (bq, k_b, bi, g_b), ag, bg

    c_ret, c_fwd, c_bwd, c_ag, c_bg = project(hc, None)
    x_ret, x_fwd, x_bwd, x_ag, x_bg = project(hx, rope)
    log_gamma = jax.nn.log_sigmoid(ret_decay_logit.astype(jnp.float32))
    ret_c, ret_x = bidirectional_scan(
        retention_scan,
        [((*c_ret, log_gamma[0]), (*x_ret, log_gamma[0])),
         ((*c_ret, log_gamma[1]), (*x_ret, log_gamma[1]))],
        per_token_decay=False)
    hg_c, hg_x = bidirectional_scan(gated_scan, [(c_fwd, x_fwd), (c_bwd, x_bwd)], per_token_decay=True)

    def merge(ret, hg, ag, bg, dtype):
        b, _, n, _ = ret.shape
        flat = lambda t: rmsnorm(t).transpose(0, 2, 1, 3).reshape(b, n, -1).astype(dtype)
        y = jnp.concatenate([flat(ret) * jax.nn.silu(ag), flat(hg) * jax.nn.silu(bg)], axis=-1)
        return y @ w_out

    return merge(ret_x, hg_x, x_ag, x_bg, hx.dtype), merge(ret_c, hg_c, c_ag, c_bg, hc.dtype)


def _mla_queries(down, w_uq, q_norm_g, rope_tab):
    b, n, _ = down.shape
    q = (rmsnorm(down[..., :MLA_Q_RANK], q_norm_g) @ w_uq).reshape(b, n, MLA_HEADS, MLA_NOPE + MLA_ROPE)
    q_nope, q_rope = q[..., :MLA_NOPE], q[..., MLA_NOPE:]
    if rope_tab is not None:
        q_rope = apply_rope(q_rope, rope_tab[0][:, None, :], rope_tab[1][:, None, :])
    return q_nope, q_rope


def _mla_keys_values(down, w_ukv, kv_norm_g, rope_tab):
    b, n, _ = down.shape
    ckv = down[..., MLA_Q_RANK:MLA_Q_RANK + MLA_KV_RANK]
    k_rope = down[..., MLA_Q_RANK + MLA_KV_RANK:]
    kv = (rmsnorm(ckv, kv_norm_g) @ w_ukv).reshape(b, n, MLA_HEADS, MLA_NOPE + MLA_V)
    if rope_tab is not None:
        k_rope = apply_rope(k_rope, *rope_tab)
    return kv[..., :MLA_NOPE], k_rope, kv[..., MLA_NOPE:]


def block_attention(q_nope, q_rope, k_nope, k_rope, v, scale):
    b, n, h, dn = q_nope.shape
    nb = n // Q_BLOCK
    qn = q_nope.reshape(b, nb, Q_BLOCK, h, dn).swapaxes(0, 1)
    qr = q_rope.reshape(b, nb, Q_BLOCK, h, q_rope.shape[-1]).swapaxes(0, 1)

    def one_block(args):
        qn_b, qr_b = args
        s = jnp.einsum('bqhd,bkhd->bhqk', qn_b, k_nope) + jnp.einsum('bqhr,bkr->bhqk', qr_b, k_rope)
        p = jax.nn.softmax(s.astype(jnp.float32) * scale, axis=-1).astype(v.dtype)
        return jnp.einsum('bhqk,bkhd->bqhd', p, v)

    o = lax.map(one_block, (qn, qr))
    return o.swapaxes(0, 1).reshape(b, n, h * v.shape[-1])


def mla_mixer(hx, hc, w_down, q_norm_g, kv_norm_g, w_uq, w_ukv, w_out, rope, need_ctx_out):
    scale = (MLA_NOPE + MLA_ROPE) ** -0.5
    down_c = hc @ w_down
    down_x = hx @ w_down
    c_kn, c_kr, c_v = _mla_keys_values(down_c, w_ukv, kv_norm_g, None)
    x_kn, x_kr, x_v = _mla_keys_values(down_x, w_ukv, kv_norm_g, rope)
    x_qn, x_qr = _mla_queries(down_x, w_uq, q_norm_g, rope)
    k_nope = jnp.concatenate([c_kn, x_kn], axis=1)
    k_rope = jnp.concatenate([c_kr, x_kr], axis=1)
    v = jnp.concatenate([c_v, x_v], axis=1)
    out_x = block_attention(x_qn, x_qr, k_nope, k_rope, v, scale) @ w_out
    out_c = None
    if need_ctx_out:
        c_qn, c_qr = _mla_queries(down_c, w_uq, q_norm_g, None)
        out_c = block_attention(c_qn, c_qr, c_kn, c_kr, c_v, scale) @ w_out
    return out_x, out_c


def swiglu(h, w_in, w_out):
    a, b = jnp.split(h @ w_in, 2, axis=-1)
    return (jax.nn.silu(a) * b) @ w_out


def moe_swiglu(h, router_w, w_in, w_out):
    logits = (h @ router_w).astype(jnp.float32)
    top_v, top_i = lax.top_k(logits, TOP_K)
    weights = jax.nn.softmax(top_v, axis=-1)
    gates = jnp.sum(jax.nn.one_hot(top_i, N_EXPERTS, dtype=jnp.float32) * weights[..., None], axis=-2)
    gates = gates.astype(h.dtype)
    y = 0.0
    for e in range(N_EXPERTS):
        y = y + gates[..., e:e + 1] * swiglu(h, w_in[e], w_out[e])
    return y


def setup_inputs(seed: int = 0) -> dict:
    key = jax.random.key(seed)
    keys = iter(jax.random.split(key, 32))
    f32 = jnp.float32

    def normal(shape, scale=1.0):
        return scale * jax.random.normal(next(keys), shape, f32)

    def weight(shape, fan_in, gain=1.0):
        return normal(shape, gain * fan_in ** -0.5)

    gammas = 1.0 - 2.0 ** (-5.0 - jnp.arange(RET_HEADS, dtype=f32))
    ret_logit = jnp.log(gammas) - jnp.log1p(-gammas)
    mix_w = RET_WIDTH + HGRN_WIDTH
    return {
        'x': normal((BATCH, SEQ, D_MODEL)),
        'c': normal((BATCH, D_MODEL)),
        'ctx': normal((BATCH, CTX_LEN, D_MODEL)),
        'c_ctx': normal((D_MODEL,)),
        'mod_w': weight((DEPTH, D_MODEL, 6 * D_MODEL), D_MODEL, 0.5),
        'mod_b': normal((DEPTH, 6 * D_MODEL), 0.02),
        'norm_g': 1.0 + normal((DEPTH, 4, D_MODEL), 0.05),
        'mix_in_w': weight((N_EVEN, D_MODEL, MIX_IN_COLS), D_MODEL),
        'ret_decay_logit': ret_logit + normal((N_EVEN, 2, RET_HEADS), 0.05),
        'hgrn_lb_logit': normal((N_EVEN + 1, HGRN_WIDTH), 0.5),
        'mix_out_w': weight((N_EVEN, mix_w, D_MODEL), mix_w),
        'ffn_in_w': weight((N_EVEN, D_MODEL, 2 * FFN_DIM), D_MODEL),
        'ffn_out_w': weight((N_EVEN, FFN_DIM, D_MODEL), FFN_DIM),
        'mla_down_w': weight((N_ODD, D_MODEL, MLA_DOWN_COLS), D_MODEL),
        'mla_q_norm_g': 1.0 + normal((N_ODD, MLA_Q_RANK), 0.05),
        'mla_kv_norm_g': 1.0 + normal((N_ODD, MLA_KV_RANK), 0.05),
        'mla_uq_w': weight((N_ODD, MLA_Q_RANK, MLA_HEADS * (MLA_NOPE + MLA_ROPE)), MLA_Q_RANK),
        'mla_ukv_w': weight((N_ODD, MLA_KV_RANK, MLA_HEADS * (MLA_NOPE + MLA_V)), MLA_KV_RANK),
        'mla_out_w': weight((N_ODD, MLA_HEADS * MLA_V, D_MODEL), MLA_HEADS * MLA_V),
        'router_w': weight((N_ODD, D_MODEL, N_EXPERTS), D_MODEL),
        'moe_in_w': weight((N_ODD, N_EXPERTS, D_MODEL, 2 * MOE_DIM), D_MODEL),
        'moe_out_w': weight((N_ODD, N_EXPERTS, MOE_DIM, D_MODEL), MOE_DIM),
    }


def reference(x, c, ctx, c_ctx, mod_w, mod_b, norm_g, mix_in_w, ret_decay_logit, hgrn_lb_logit,
              mix_out_w, ffn_in_w, ffn_out_w, mla_down_w, mla_q_norm_g, mla_kv_norm_g, mla_uq_w,
              mla_ukv_w, mla_out_w, router_w, moe_in_w, moe_out_w):
    n_rows = x.shape[1] // GRID_W
    ret_rope = axial_rope_tables(n_rows, RET_DIM)
    mla_rope = axial_rope_tables(n_rows, MLA_ROPE)
    lb_all = jnp.cumsum(jax.nn.softmax(hgrn_lb_logit.astype(jnp.float32), axis=0), axis=0)
    silu_c = jax.nn.silu(c)
    silu_cc = jax.nn.silu(c_ctx)
    for layer in range(DEPTH):
        last = layer == DEPTH - 1
        j = layer // 2
        g = norm_g[layer]
        mx = [m[:, None, :] for m in jnp.split(silu_c @ mod_w[layer] + mod_b[layer], 6, axis=-1)]
        mc = jnp.split(silu_cc @ mod_w[layer] + mod_b[layer], 6, axis=-1)
        hx = rmsnorm(x, g[0]) * (1.0 + mx[1]) + mx[0]
        hc = rmsnorm(ctx, g[0]) * (1.0 + mc[1]) + mc[0]
        if layer % 2 == 0:
            ox, oc = retention_hgrn_mixer(hx, hc, mix_in_w[j], ret_decay_logit[j], lb_all[j],
                                          mix_out_w[j], ret_rope)
            ffn = lambda h, j=j: swiglu(h, ffn_in_w[j], ffn_out_w[j])
        else:
            ox, oc = mla_mixer(hx, hc, mla_down_w[j], mla_q_norm_g[j], mla_kv_norm_g[j], mla_uq_w[j],
                               mla_ukv_w[j], mla_out_w[j], mla_rope, need_ctx_out=not last)
            ffn = lambda h, j=j: moe_swiglu(h, router_w[j], moe_in_w[j], moe_out_w[j])
        x = x + mx[2] * rmsnorm(ox, g[1])
        if not last:
            ctx = ctx + mc[2] * rmsnorm(oc, g[1])
        hx = rmsnorm(x, g[2]) * (1.0 + mx[4]) + mx[3]
        x = x + mx[5] * rmsnorm(ffn(hx), g[3])
        if not last:
            hc = rmsnorm(ctx, g[2]) * (1.0 + mc[4]) + mc[3]
            ctx = ctx + mc[5] * rmsnorm(ffn(hc), g[3])
    return x
```

```python
import contextlib
import numpy as np
import concourse.bass as bass
import concourse.mybir as mybir
from concourse.bass_utils import run_bass_kernel_spmd

F32 = mybir.dt.float32
BF16 = mybir.dt.bfloat16
AF = mybir.ActivationFunctionType
ALU = mybir.AluOpType

ENGS = ("pe", "act", "dve", "pool", "sp")
N_DMA_SEMS = 32

D = 1024
KC = 8
CTX = 256
FFN = 2816
FC = 22
NE = 8
EPS = 1e-6
GRID_W = 64


class Op:
    __slots__ = ("eng", "fn", "reads", "writes", "is_dma", "deps", "has_dep", "ticket", "dsem", "dval",
                 "dprev", "extra")

    def __init__(self, eng, fn, reads, writes, is_dma):
        self.eng = eng
        self.fn = fn
        self.reads = reads
        self.writes = writes
        self.is_dma = is_dma
        self.deps = []
        self.has_dep = False
        self.ticket = None
        self.dsem = None
        self.dval = None
        self.dprev = None
        self.extra = ()


class Prog:
    def __init__(self, nc):
        self.nc = nc
        self.ops = []
        self.phase_start = 0

    def add(self, eng, fn, reads=(), writes=(), is_dma=False):
        self.ops.append(Op(eng, fn, tuple(reads), tuple(writes), is_dma))

    def dma(self, eng, out, in_, reads=(), writes=(), **kw):
        self.add(eng, lambda e: e.dma_start(out=out, in_=in_, **kw), reads, writes, is_dma=True)

    def mm(self, out, lhsT, rhs, start, stop, reads=(), writes=()):
        self.add("pe", lambda e: e.matmul(out, lhsT, rhs, start=start, stop=stop), reads, writes)

    def tr(self, out, in_, ident, reads=(), writes=()):
        self.add("pe", lambda e: e.transpose(out, in_, ident), reads, writes)

    def barrier(self):
        last = {}
        dmas = []
        for i in range(self.phase_start, len(self.ops)):
            op = self.ops[i]
            if op.fn is None:
                continue
            if op.is_dma:
                dmas.append(i)
            else:
                last[op.eng] = i
        extra = tuple(sorted(set(last.values()) | set(dmas)))
        for e in ENGS:
            op = Op(e, None, (), (), False)
            op.extra = extra
            self.ops.append(op)
        self.phase_start = len(self.ops)

    def _analyse(self):
        last_w = {}
        readers = {}
        ops = self.ops
        for i, op in enumerate(ops):
            deps = set()
            for r in op.reads:
                w = last_w.get(r)
                if w is not None:
                    deps.add(w)
            for r in op.writes:
                w = last_w.get(r)
                if w is not None:
                    deps.add(w)
                rd = readers.get(r)
                if rd:
                    deps.update(rd.values())
            keep = []
            for j in deps:
                p = ops[j]
                if p.eng == op.eng and not p.is_dma and not op.is_dma and op.eng == "pe":
                    continue
                keep.append(j)
            for j in op.extra:
                p = ops[j]
                if p.eng == op.eng and not p.is_dma:
                    continue
                keep.append(j)
            op.deps = sorted(set(keep))
            for j in op.deps:
                ops[j].has_dep = True
            key = ("dma", i) if op.is_dma else op.eng
            for r in op.reads:
                d = readers.get(r)
                if d is None:
                    d = readers[r] = {}
                d[key] = i
            for r in op.writes:
                last_w[r] = i
                readers[r] = {}

    def emit(self, final_wait_resources=()):
        nc = self.nc
        self.add("sp", None, reads=tuple(final_wait_resources), writes=())
        self._analyse()
        with contextlib.ExitStack() as st:
            esem = {e: st.enter_context(nc.semaphore("s_" + e)) for e in ENGS}
            dsems = [st.enter_context(nc.semaphore("d%d" % k)) for k in range(N_DMA_SEMS)]
            cnt = {e: 0 for e in ENGS}
            dcnt = [0] * N_DMA_SEMS
            dlast = [None] * N_DMA_SEMS
            k = 0
            kq = {"sp": 0, "pool": 0}
            half = N_DMA_SEMS // 2
            for i, op in enumerate(self.ops):
                if op.is_dma:
                    s = (kq[op.eng] % half) + (0 if op.eng == "sp" else half)
                    kq[op.eng] += 1
                    k += 1
                    op.dsem = s
                    dcnt[s] += 16
                    op.dval = dcnt[s]
                    op.dprev = dlast[s]
                    dlast[s] = i
                elif op.has_dep:
                    cnt[op.eng] += 1
                    op.ticket = cnt[op.eng]
            self.stats = dict(cnt)
            self.stats["n_ops"] = len(self.ops)
            self.stats["n_dma"] = k
            block = st.enter_context(nc.Block())
            ops = self.ops

            def run(engname):
                def body(e):
                    seen_e = {x: 0 for x in ENGS}
                    seen_d = [0] * N_DMA_SEMS
                    for op in ops:
                        if op.eng != engname:
                            continue
                        deps = op.deps
                        if op.is_dma and op.dprev is not None:
                            deps = deps + [op.dprev]
                        for j in deps:
                            p = ops[j]
                            if p.is_dma:
                                if seen_d[p.dsem] < p.dval:
                                    e.wait_ge(dsems[p.dsem], p.dval)
                                    seen_d[p.dsem] = p.dval
                            else:
                                if seen_e[p.eng] < p.ticket:
                                    e.wait_ge(esem[p.eng], p.ticket)
                                    seen_e[p.eng] = p.ticket
                        if op.fn is None:
                            continue
                        ins = op.fn(e)
                        if op.is_dma:
                            ins.then_inc(dsems[op.dsem], 16)
                        elif op.ticket is not None:
                            ins.then_inc(esem[op.eng], 1)
                return body

            block.tensor(run("pe"))
            block.scalar(run("act"))
            block.vector(run("dve"))
            block.gpsimd(run("pool"))
            block.sync(run("sp"))


class Cx:
    pass


class Phase:
    def __init__(self, cx, name):
        self.cx = cx
        self.name = name
        self.st = contextlib.ExitStack()
        self.n = 0

    def __enter__(self):
        self.st.__enter__()
        return self

    def sb(self, shape, dt, name=None):
        self.n += 1
        nm = "%s_%s%d" % (self.name, name or "t", self.n)
        t = self.st.enter_context(self.cx.nc.sbuf_tensor(nm, list(shape), dt))
        return t

    def __exit__(self, *a):
        self.cx.P.barrier()
        return self.st.__exit__(*a)


def bcast_row(ap_row, n):
    return ap_row.to_broadcast([n, ap_row.shape[-1]])


class PrePost:
    def __init__(self, cx, ph, tag):
        self.cx = cx
        self.tag = tag
        self.xt = [ph.sb([128, D], F32, "xt") for _ in range(2)]
        self.t1 = ph.sb([128, D], F32, "t1")
        self.hb = ph.sb([128, D], BF16, "hb")
        self.sq = ph.sb([128, D], BF16, "sq")
        self.st = ph.sb([128, 8], F32, "st")
        self.n = 0

    def rstd(self, ss, rs, inv_n, deps_r, deps_w):
        P = self.cx.P
        P.add("dve", lambda e: e.tensor_scalar(rs, ss, inv_n, EPS, op0=ALU.mult, op1=ALU.add),
              reads=deps_r, writes=deps_w)
        P.add("act", lambda e: e.activation(rs, rs, AF.Sqrt), reads=deps_w, writes=deps_w)
        P.add("dve", lambda e: e.reciprocal(rs, rs), reads=deps_w, writes=deps_w)

    def pre(self, src_ap, src_res, A, B, mod_res, hT_dst, hT_res, bank):
        cx, P, tg = self.cx, self.cx.P, self.tag
        i = self.n
        self.n += 1
        xt = self.xt[i % 2]
        rx = (tg, "xt", i % 2)
        P.dma("sp", xt[:], src_ap, reads=[src_res], writes=[rx])
        ss = self.st[:, 0:1]
        rs = self.st[:, 1:2]
        P.add("act", lambda e: e.activation(self.sq[:], xt[:], AF.Square, accum_out=ss),
              reads=[rx], writes=[(tg, "sq"), (tg, "ss")])
        self.rstd(ss, rs, 1.0 / D, [(tg, "ss")], [(tg, "rs")])
        P.add("dve", lambda e: e.scalar_tensor_tensor(self.t1[:], xt[:], rs, A, op0=ALU.mult, op1=ALU.mult),
              reads=[rx, (tg, "rs"), mod_res], writes=[(tg, "t1")])
        P.add("pool", lambda e: e.tensor_tensor(self.hb[:], self.t1[:], B, op=ALU.add),
              reads=[(tg, "t1"), mod_res], writes=[(tg, "hb")])
        psb = cx.PSB[bank]
        for k in range(KC):
            P.tr(psb[:, k * 128:(k + 1) * 128], self.hb[:, k * 128:(k + 1) * 128], cx.ident[:],
                 reads=[(tg, "hb")], writes=[("ps", bank)])
        P.add("act", lambda e: e.activation(hT_dst, psb[:, 0:D].rearrange("p (k n) -> p k n", k=KC), AF.Copy),
              reads=[], writes=[("ps", bank), hT_res])

    def post(self, o_src, o_res, res_ap, res_res, G, mod_res, dst_ap, dst_res):
        cx, P, tg = self.cx, self.cx.P, self.tag
        i = self.n
        self.n += 1
        xt = self.xt[i % 2]
        rx = (tg, "xt", i % 2)
        P.dma("sp", xt[:], res_ap, reads=[res_res], writes=[rx])
        ss = self.st[:, 2:3]
        rs = self.st[:, 3:4]
        P.add("act", lambda e: e.activation(self.sq[:], o_src, AF.Square, accum_out=ss),
              reads=[o_res], writes=[(tg, "sq"), (tg, "ss2")])
        self.rstd(ss, rs, 1.0 / D, [(tg, "ss2")], [(tg, "rs2")])
        P.add("dve", lambda e: e.scalar_tensor_tensor(self.t1[:], o_src, rs, G, op0=ALU.mult, op1=ALU.mult),
              reads=[o_res, (tg, "rs2"), mod_res], writes=[(tg, "t1")])
        P.add("pool", lambda e: e.tensor_tensor(xt[:], self.t1[:], xt[:], op=ALU.add),
              reads=[(tg, "t1"), rx], writes=[rx])
        P.dma("sp", dst_ap, xt[:], reads=[rx], writes=[dst_res])


def load_mod(cx, ph, layer, stream, tag):
    t = ph.sb([128, 6, D], F32, "mod")
    res = (tag, "mod", stream)
    cx.P.dma("sp", t[:], cx.modb[layer, stream].rearrange("i p n -> p i n"), reads=[("modb", layer, stream)],
             writes=[res])
    return t, res


def tok_tiles(cx, with_ctx):
    tl = []
    if with_ctx:
        tl += [(1, i) for i in range(CTX // 128)]
    tl += [(0, i) for i in range(cx.NT)]
    return tl


def phase_mod(cx):
    P = cx.P
    with Phase(cx, "M") as ph:
        sc = ph.sb([128, 16], F32, "sc")
        lh = ph.sb([128, 16, 128], BF16, "lh")
        P.dma("sp", sc[:], cx.ccol, writes=["M_sc"])
        P.add("act", lambda e: e.activation(sc[:], sc[:], AF.Silu), reads=["M_sc"], writes=["M_sc"])
        for j in range(16):
            P.add("dve", lambda e, j=j: e.tensor_copy(lh[:, j, :], sc[:, j:j + 1].to_broadcast([128, 128])),
                  reads=["M_sc"], writes=["M_lh"])
        NH = 3072
        wb = ph.sb([128, KC, NH], BF16, "wb")
        bb = ph.sb([128, NH], F32, "bb")
        gb = ph.sb([128, 4, D], F32, "gb")
        m = ph.sb([128, NH], F32, "m")
        o = ph.sb([128, 3, D], F32, "o")
        for l in range(2):
            P.dma("sp", gb[:], bcast_row(cx.norm_g[l:l + 1].rearrange("o i n -> o (i n)"), 128)
                  .rearrange("p (i n) -> p i n", i=4), writes=["M_gb"])
            for half in range(2):
                for k in range(KC):
                    P.dma("pool", wb[:, k, :], cx.mod_w[l, k * 128:(k + 1) * 128, half * NH:(half + 1) * NH],
                          writes=[("M_wb", k)])
                P.dma("sp", bb[:], bcast_row(cx.mod_b[l:l + 1, half * NH:(half + 1) * NH], 128), writes=["M_bb"])
                for s in range(2):
                    for nb in range(NH // 512):
                        bank = nb % 4
                        for k in range(KC):
                            P.mm(cx.PS[bank][:], lh[:, s * 8 + k, :], wb[:, k, nb * 512:(nb + 1) * 512],
                                 k == 0, k == KC - 1, reads=["M_lh", ("M_wb", k)], writes=[("ps", bank)])
                        P.add("dve", lambda e, nb=nb, bank=bank: e.tensor_tensor(
                            m[:, nb * 512:(nb + 1) * 512], cx.PS[bank][:], bb[:, nb * 512:(nb + 1) * 512], op=ALU.add),
                            reads=["M_bb"], writes=[("ps", bank), "M_m"])
                    g_a = gb[:, 2 * half, :]
                    g_g = gb[:, 2 * half + 1, :]
                    P.add("dve", lambda e, g_a=g_a: e.scalar_tensor_tensor(o[:, 0, :], m[:, D:2 * D], 1.0, g_a,
                                                                          op0=ALU.add, op1=ALU.mult),
                          reads=["M_m", "M_gb"], writes=["M_o"])
                    P.add("pool", lambda e: e.tensor_copy(o[:, 1, :], m[:, 0:D]), reads=["M_m", "M_o"], writes=["M_o"])
                    P.add("pool", lambda e, g_g=g_g: e.tensor_tensor(o[:, 2, :], m[:, 2 * D:3 * D], g_g, op=ALU.mult),
                          reads=["M_m", "M_gb", "M_o"], writes=["M_o"])
                    P.dma("sp", cx.modb[l, s, 3 * half:3 * half + 3].rearrange("i p n -> p i n"), o[:],
                          reads=["M_o"], writes=[("modb", l, s)])


MOD_A, MOD_B, MOD_G = 0, 1, 2


def phase_ffn(cx, layer, w_in, w_out, router, n_exp, src, dst, with_ctx, tag):
    P = cx.P
    SBT = 12
    FG = [4, 4, 4, 4, 4, 2]
    tiles = tok_tiles(cx, with_ctx)
    with Phase(cx, tag) as ph:
        pp = PrePost(cx, ph, tag)
        mods = {}
        for s in ([0, 1] if with_ctx else [0]):
            mods[s] = load_mod(cx, ph, layer, s, tag)
        hT = ph.sb([128, KC, SBT * 128], BF16, "hT")
        y = ph.sb([128, SBT, D], F32, "y")
        wi = [ph.sb([128, KC, 2, 512], BF16, "wi") for _ in range(2)]
        wo = [ph.sb([128, 4, D], BF16, "wo") for _ in range(2)]
        u = [ph.sb([128, 4, 512], BF16, "u") for _ in range(2)]
        sa = [ph.sb([128, 512], F32, "sa") for _ in range(2)]
        gates = ph.sb([128, SBT, NE], F32, "gates")
        rt = ph.sb([128, 64], F32, "rt")
        if router is not None:
            rw = ph.sb([128, KC, NE], BF16, "rw")
            P.dma("pool", rw[:], router.rearrange("(k p) n -> p k n", p=128), writes=[(tag, "rw")])
        piece = 0
        nblk_total = 0
        for sb0 in range(0, len(tiles), SBT):
            sbt = tiles[sb0:sb0 + SBT]
            n = len(sbt)
            for j, (s, ti) in enumerate(sbt):
                modt, modr = mods[s]
                sap, sres = src[s]
                pp.pre(sap[ti * 128:(ti + 1) * 128, :], (sres, ti), modt[:, 3 + MOD_A, :], modt[:, 3 + MOD_B, :], modr,
                       hT[:, :, j * 128:(j + 1) * 128], (tag, "hT", j), bank=j % 2)
                P.add("pool", lambda e, j=j: e.memset(y[:, j, :], 0.0), writes=[(tag, "y", j)])
                if router is not None:
                    bank = 2 + j % 2
                    for k in range(KC):
                        P.mm(cx.PS[bank][:, 0:NE], hT[:, k, j * 128:(j + 1) * 128], rw[:, k, :], k == 0, k == KC - 1,
                             reads=[(tag, "hT", j), (tag, "rw")], writes=[("ps", bank)])
                    lg = rt[:, 0:8]
                    mx = rt[:, 8:16]
                    sc = rt[:, 16:24]
                    g1 = rt[:, 24:32]
                    g2 = rt[:, 32:40]
                    R = (tag, "rt")
                    P.add("dve", lambda e, bank=bank: e.tensor_copy(lg, cx.PS[bank][:, 0:NE]), reads=[R],
                          writes=[("ps", bank), R])
                    P.add("dve", lambda e: e.max(out=mx, in_=lg), reads=[R], writes=[R])
                    P.add("dve", lambda e: e.tensor_tensor(sc[:, 0:1], mx[:, 1:2], mx[:, 0:1], op=ALU.subtract),
                          reads=[R], writes=[R])
                    P.add("act", lambda e: e.activation(sc[:, 1:2], sc[:, 0:1], AF.Exp), reads=[R], writes=[R])
                    P.add("dve", lambda e: e.tensor_scalar(sc[:, 2:3], sc[:, 1:2], 1.0, None, op0=ALU.add),
                          reads=[R], writes=[R])
                    P.add("dve", lambda e: e.reciprocal(sc[:, 3:4], sc[:, 2:3]), reads=[R], writes=[R])
                    P.add("dve", lambda e: e.tensor_tensor(sc[:, 4:5], sc[:, 1:2], sc[:, 3:4], op=ALU.mult),
                          reads=[R], writes=[R])
                    P.add("dve", lambda e: e.tensor_scalar(g1, lg, mx[:, 0:1], sc[:, 3:4], op0=ALU.is_equal,
                                                           op1=ALU.mult), reads=[R], writes=[R])
                    P.add("dve", lambda e: e.tensor_scalar(g2, lg, mx[:, 1:2], sc[:, 4:5], op0=ALU.is_equal,
                                                           op1=ALU.mult), reads=[R], writes=[R])
                    P.add("dve", lambda e, j=j: e.tensor_tensor(gates[:, j, :], g1, g2, op=ALU.add), reads=[R],
                          writes=[R, (tag, "gates", j)])
            blocks = [(b0, min(4, n - b0)) for b0 in range(0, n, 4)]
            for ex in range(n_exp):
                f0 = 0
                for gsz in FG:
                    pb = piece % 2
                    piece += 1
                    wit, wot = wi[pb], wo[pb]
                    Rwi, Rwo = (tag, "wi", pb), (tag, "wo", pb)
                    for ab in range(2):
                        P.dma("pool", wit[:, :, ab, 0:gsz * 128],
                              w_in[ex, :, ab * FFN + f0 * 128: ab * FFN + (f0 + gsz) * 128]
                              .rearrange("(k p) n -> p k n", p=128), writes=[Rwi])
                    P.dma("pool", wot[:, 0:gsz, :],
                          w_out[ex, f0 * 128:(f0 + gsz) * 128, :].rearrange("(c p) n -> p c n", p=128), writes=[Rwo])
                    for (b0, bn) in blocks:
                        W = bn * 128
                        ub = nblk_total % 2
                        nblk_total += 1
                        ut = u[ub]
                        for c in range(gsz):
                            pa, pbk = (0, 1) if c % 2 == 0 else (2, 3)
                            for ab, bank in ((0, pa), (1, pbk)):
                                for k in range(KC):
                                    P.mm(cx.PS[bank][:, 0:W], wit[:, k, ab, c * 128:(c + 1) * 128],
                                         hT[:, k, b0 * 128:b0 * 128 + W], k == 0, k == KC - 1,
                                         reads=[Rwi] + [(tag, "hT", b0 + q) for q in range(bn)], writes=[("ps", bank)])
                            sat = sa[c % 2]
                            P.add("act", lambda e, sat=sat, pa=pa, W=W: e.activation(sat[:, 0:W], cx.PS[pa][:, 0:W], AF.Silu),
                                  writes=[("ps", pa), (tag, "sa", c % 2)])
                            P.add("dve", lambda e, sat=sat, pbk=pbk, W=W, ut=ut, c=c: e.tensor_tensor(
                                ut[:, c, 0:W], sat[:, 0:W], cx.PS[pbk][:, 0:W], op=ALU.mult),
                                reads=[(tag, "sa", c % 2)], writes=[("ps", pbk), (tag, "u", ub, c)])
                        oi = 0
                        for q in range(bn):
                            j = b0 + q
                            for mh in range(2):
                                bank = 4 + oi % 4
                                oi += 1
                                for c in range(gsz):
                                    P.mm(cx.PS[bank][:], ut[:, c, q * 128:(q + 1) * 128], wot[:, c, mh * 512:(mh + 1) * 512],
                                         c == 0, c == gsz - 1, reads=[Rwo, (tag, "u", ub, c)], writes=[("ps", bank)])
                                gsc = gates[:, j, ex:ex + 1] if router is not None else 1.0
                                P.add("dve", lambda e, bank=bank, j=j, mh=mh, gsc=gsc: e.scalar_tensor_tensor(
                                    y[:, j, mh * 512:(mh + 1) * 512], cx.PS[bank][:], gsc, y[:, j, mh * 512:(mh + 1) * 512],
                                    op0=ALU.mult, op1=ALU.add),
                                    reads=[(tag, "gates", j)], writes=[("ps", bank), (tag, "y", j)])
                    f0 += gsz
            for j, (s, ti) in enumerate(sbt):
                modt, modr = mods[s]
                sap, sres = src[s]
                dap, dres = dst[s]
                pp.post(y[:, j, :], (tag, "y", j), sap[ti * 128:(ti + 1) * 128, :], (sres, ti), modt[:, 3 + MOD_G, :], modr,
                        dap[ti * 128:(ti + 1) * 128, :], (dres, ti))


MLA_H = 8
MLA_QR = 384
MLA_KVR = 256
MLA_SCALE = (128 + 64) ** -0.5


def rope_tok(P, eng, out, x1, x2, cos, sin, tmp_a, tmp_b, R):
    o1, o2 = out
    P.add(eng, lambda e: e.tensor_tensor(tmp_a, x1, cos, op=ALU.mult), reads=R, writes=R)
    P.add(eng, lambda e: e.tensor_tensor(tmp_b, x2, sin, op=ALU.mult), reads=R, writes=R)
    P.add(eng, lambda e: e.tensor_tensor(o1, tmp_a, tmp_b, op=ALU.subtract), reads=R, writes=R)
    P.add(eng, lambda e: e.tensor_tensor(tmp_a, x1, sin, op=ALU.mult), reads=R, writes=R)
    P.add(eng, lambda e: e.tensor_tensor(tmp_b, x2, cos, op=ALU.mult), reads=R, writes=R)
    P.add(eng, lambda e: e.tensor_tensor(o2, tmp_a, tmp_b, op=ALU.add), reads=R, writes=R)


def phase_mla(cx, layer, src, dst):
    P = cx.P
    T, TA, NT, NA = cx.T, cx.TA, cx.NT, cx.NA
    tag = "A"
    tiles = tok_tiles(cx, True)
    with Phase(cx, "A") as ph:
        qnT = ph.sb([128, 3, T], BF16, "qnT")
        ckvT = ph.sb([128, 2, TA], BF16, "ckvT")
        krT = ph.sb([128, TA], BF16, "krT")
        qrT = ph.sb([128, 4, T], BF16, "qrT")
        with Phase(cx, "A1") as p1:
            pp = PrePost(cx, p1, "A1")
            mods = {s: load_mod(cx, p1, layer, s, "A1") for s in (0, 1)}
            wd = p1.sb([128, KC, 704], BF16, "wd")
            for (c0_, c1_) in ((0, 512), (512, 704)):
                P.dma("pool", wd[:, :, c0_:c1_], cx.mla_down_w[0][:, c0_:c1_].rearrange("(k p) n -> p k n", p=128), writes=["A_wd"])
            wqr = p1.sb([128, 3, 8, 64], BF16, "wqr")
            for k in range(3):
                P.dma("pool", wqr[:, k], cx.mla_uq_w[0, k * 128:(k + 1) * 128, :].rearrange("p (h d) -> p h d", d=192)[:, :, 128:192],
                      writes=["A_wqr"])
            gq = p1.sb([128, 640], F32, "gq")
            P.dma("sp", gq[:, 0:384], bcast_row(cx.mla_q_norm_g[0:1, :], 128), writes=["A_gq"])
            P.dma("sp", gq[:, 384:640], bcast_row(cx.mla_kv_norm_g[0:1, :], 128), writes=["A_gq"])
            hT = [p1.sb([128, KC, 128], BF16, "hT") for _ in range(2)]
            dn = p1.sb([128, 704], F32, "dn")
            nb = p1.sb([128, 768], BF16, "nb")
            st = p1.sb([128, 8], F32, "st")
            cs = [p1.sb([128, 64], F32, "cs") for _ in range(2)]
            tmp = p1.sb([128, 2, 256], F32, "tmp")
            qr32 = p1.sb([128, 8, 64], F32, "qr32")
            qrb = p1.sb([128, 8, 64], BF16, "qrb")
            for idx, (s, ti) in enumerate(tiles):
                ta = idx
                hb_ = idx % 2
                modt, modr = mods[s]
                sap, sres = src[s]
                Rh = ("A_hT", hb_)
                pp.pre(sap[ti * 128:(ti + 1) * 128, :], (sres, ti), modt[:, MOD_A, :], modt[:, MOD_B, :], modr,
                       hT[hb_][:], Rh, bank=idx % 2)
                for (bank, c0, c1) in ((2, 0, 512), (3, 512, 704)):
                    for k in range(KC):
                        P.mm(cx.PS[bank][:, 0:c1 - c0], hT[hb_][:, k, :], wd[:, k, c0:c1], k == 0, k == KC - 1,
                             reads=[Rh, "A_wd"], writes=[("ps", bank)])
                    P.add("act", lambda e, bank=bank, c0=c0, c1=c1: e.activation(dn[:, c0:c1], cx.PS[bank][:, 0:c1 - c0], AF.Copy),
                          writes=[("ps", bank), "A_dn"])
                P.add("act", lambda e, sqj=pp.sq: e.activation(sqj[:, 0:384], dn[:, 0:384], AF.Square, accum_out=st[:, 0:1]),
                      reads=["A_dn"], writes=["A_sq", "A_st"])
                P.add("act", lambda e, sqj=pp.sq: e.activation(sqj[:, 384:640], dn[:, 384:640], AF.Square, accum_out=st[:, 1:2]),
                      reads=["A_dn"], writes=["A_sq", "A_st"])
                P.add("dve", lambda e: e.tensor_scalar(st[:, 2:3], st[:, 0:1], 1.0 / 384, EPS, op0=ALU.mult, op1=ALU.add),
                      reads=["A_st"], writes=["A_st2"])
                P.add("dve", lambda e: e.tensor_scalar(st[:, 3:4], st[:, 1:2], 1.0 / 256, EPS, op0=ALU.mult, op1=ALU.add),
                      reads=["A_st"], writes=["A_st2"])
                P.add("act", lambda e: e.activation(st[:, 6:8], st[:, 2:4], AF.Sqrt), reads=["A_st2"], writes=["A_st2b"])
                P.add("dve", lambda e: e.reciprocal(st[:, 4:6], st[:, 6:8]), reads=["A_st2b"], writes=["A_st3"])
                Rnb = "A_nb"
                if s == 0:
                    P.add("dve", lambda e: e.scalar_tensor_tensor(nb[:, 0:384], dn[:, 0:384], st[:, 4:5], gq[:, 0:384],
                                                                  op0=ALU.mult, op1=ALU.mult),
                          reads=["A_dn", "A_st3", "A_gq"], writes=[Rnb])
                P.add("dve", lambda e: e.scalar_tensor_tensor(nb[:, 384:640], dn[:, 384:640], st[:, 5:6], gq[:, 384:640],
                                                              op0=ALU.mult, op1=ALU.mult),
                      reads=["A_dn", "A_st3", "A_gq"], writes=[Rnb])
                if s == 0:
                    cst = cs[ti % 2]
                    Rcs = ("A_cs", ti % 2)
                    P.dma("sp", cst[:], cx.rope_mla[ti * 128:(ti + 1) * 128, :], writes=[Rcs])
                    RR = ["A_dn", Rcs, "A_tmp", Rnb]
                    rope_tok(P, "pool", (nb[:, 640:672], nb[:, 672:704]), dn[:, 640:672], dn[:, 672:704],
                             cst[:, 0:32], cst[:, 32:64], tmp[:, 0, 0:32], tmp[:, 1, 0:32], RR)
                else:
                    P.add("pool", lambda e: e.tensor_copy(nb[:, 640:704], dn[:, 640:704]), reads=["A_dn"], writes=[Rnb])
                P.add("pool", lambda e: e.tensor_copy(nb[:, 704:768], nb[:, 640:704]), reads=[Rnb], writes=[Rnb])
                bank = 4 + idx % 2
                psb = cx.PSB[bank]
                blocks = ([0, 1, 2] if s == 0 else []) + [3, 4, 5]
                for c in blocks:
                    P.tr(psb[:, c * 128:(c + 1) * 128], nb[:, c * 128:(c + 1) * 128], cx.ident[:], reads=[Rnb],
                         writes=[("ps", bank)])
                if s == 0:
                    P.add("act", lambda e, psb=psb, ti=ti: e.activation(
                        qnT[:, :, ti * 128:(ti + 1) * 128], psb[:, 0:384].rearrange("p (k n) -> p k n", k=3), AF.Copy),
                        writes=[("ps", bank), ("A_qnT", ti)])
                P.add("dve", lambda e, psb=psb, ta=ta: e.tensor_copy(
                    ckvT[:, :, ta * 128:(ta + 1) * 128], psb[:, 384:640].rearrange("p (k n) -> p k n", k=2)),
                    writes=[("ps", bank), ("A_ckvT", ta)])
                P.add("dve", lambda e, psb=psb, ta=ta: e.tensor_copy(krT[:, ta * 128:(ta + 1) * 128], psb[:, 640:768]),
                      writes=[("ps", bank), ("A_krT", ta)])
                if s == 0:
                    bank = 6
                    for k in range(3):
                        P.mm(cx.PS[bank][:], qnT[:, k, ti * 128:(ti + 1) * 128], wqr[:, k].rearrange("p h d -> p (h d)"),
                             k == 0, k == 2, reads=[("A_qnT", ti), "A_wqr"], writes=[("ps", bank)])
                    P.add("act", lambda e, bank=bank: e.activation(qr32[:].rearrange("p h d -> p (h d)"), cx.PS[bank][:], AF.Copy),
                          writes=[("ps", bank), "A_qr32"])
                    cosb = cst[:, 0:32].unsqueeze(1).to_broadcast([128, 8, 32])
                    sinb = cst[:, 32:64].unsqueeze(1).to_broadcast([128, 8, 32])
                    ta_ = tmp[:, 0, :].rearrange("p (h d) -> p h d", h=8)
                    tb_ = tmp[:, 1, :].rearrange("p (h d) -> p h d", h=8)
                    RR = ["A_qr32", Rcs, "A_tmp", "A_qrb"]
                    rope_tok(P, "pool", (qrb[:, :, 0:32], qrb[:, :, 32:64]), qr32[:, :, 0:32], qr32[:, :, 32:64],
                             cosb, sinb, ta_, tb_, RR)
                    bank = 7
                    psb7 = cx.PSB[bank]
                    qrf = qrb[:].rearrange("p h d -> p (h d)")
                    for c in range(4):
                        P.tr(psb7[:, c * 128:(c + 1) * 128], qrf[:, c * 128:(c + 1) * 128], cx.ident[:], reads=["A_qrb"],
                             writes=[("ps", bank)])
                    P.add("act", lambda e, psb7=psb7, ti=ti: e.activation(
                        qrT[:, :, ti * 128:(ti + 1) * 128], psb7[:, 0:512].rearrange("p (k n) -> p k n", k=4), AF.Copy),
                        writes=[("ps", bank), ("A_qrT", ti)])
        if getattr(cx, "dbg", None):
            P.dma("sp", cx.dbg["qnT"], qnT[:], reads=[("A_qnT", t) for t in range(NT)], writes=["dbg1"])
            P.dma("sp", cx.dbg["ckvT"], ckvT[:], reads=[("A_ckvT", t) for t in range(NA)], writes=["dbg2"])
            P.dma("sp", cx.dbg["krT"], krT[:], reads=[("A_krT", t) for t in range(NA)], writes=["dbg3"])
            P.dma("sp", cx.dbg["qrT"], qrT[:], reads=[("A_qrT", t) for t in range(NT)], writes=["dbg4"])
        with Phase(cx, "A2") as p2:
            wqn = p2.sb([128, 3, 8, 128], BF16, "wqn")
            for k in range(3):
                P.dma("pool", wqn[:, k], cx.mla_uq_w[0, k * 128:(k + 1) * 128, :].rearrange("p (h d) -> p h d", d=192)[:, :, 0:128],
                      writes=["B_wqn"])
            wkv = p2.sb([128, 2, 2048], BF16, "wkv")
            P.dma("pool", wkv[:], cx.mla_ukv_w[0].rearrange("(k p) n -> p k n", p=128), writes=["B_wkv"])
            KhT = [p2.sb([128, TA], BF16, "KhT") for _ in range(2)]
            Vh = [p2.sb([128, NA, 128], BF16, "Vh") for _ in range(2)]
            QhT = [p2.sb([128, T], BF16, "QhT") for _ in range(2)]
            pT = [p2.sb([128, 512], BF16, "pT") for _ in range(3)]
            rden = p2.sb([128, 512], F32, "rden")
            at = [p2.sb([128, 512], BF16, "at") for _ in range(2)]
            all_ckv = [("A_ckvT", ta) for ta in range(NA)]
            all_qn = [("A_qnT", ti) for ti in range(NT)]

            def proj(h):
                par = h % 2
                pc = 0
                for kb0 in range(0, TA, 512):
                    W = min(512, TA - kb0)
                    bank = 7
                    for k in range(2):
                        P.mm(cx.PS[bank][:, 0:W], wkv[:, k, h * 256:h * 256 + 128], ckvT[:, k, kb0:kb0 + W], k == 0, k == 1,
                             reads=["B_wkv"] + all_ckv, writes=[("ps", bank)])
                    P.add("dve", lambda e, par=par, kb0=kb0, W=W, bank=bank: e.tensor_copy(KhT[par][:, kb0:kb0 + W], cx.PS[bank][:, 0:W]),
                          writes=[("ps", bank), ("B_K", par)])
                for t0 in range(0, NA, 4):
                    tn = min(4, NA - t0)
                    bank = 7
                    for q in range(tn):
                        for k in range(2):
                            P.mm(cx.PS[bank][:, q * 128:(q + 1) * 128], ckvT[:, k, (t0 + q) * 128:(t0 + q + 1) * 128],
                                 wkv[:, k, h * 256 + 128:h * 256 + 256], k == 0, k == 1,
                                 reads=["B_wkv"] + all_ckv, writes=[("ps", bank)])
                    P.add("dve", lambda e, par=par, t0=t0, tn=tn, bank=bank: e.tensor_copy(
                        Vh[par][:, t0:t0 + tn, :], cx.PS[bank][:, 0:tn * 128].rearrange("p (t d) -> p t d", d=128)),
                        writes=[("ps", bank), ("B_V", par)])
                for qb0 in range(0, T, 512):
                    bank = 7
                    for k in range(3):
                        P.mm(cx.PS[bank][:], wqn[:, k, h, :], qnT[:, k, qb0:qb0 + 512], k == 0, k == 2,
                             reads=["B_wqn"] + all_qn, writes=[("ps", bank)])
                    P.add("dve", lambda e, par=par, qb0=qb0, bank=bank: e.tensor_copy(QhT[par][:, qb0:qb0 + 512], cx.PS[bank][:]),
                          writes=[("ps", bank), ("B_Q", par)])

            NQ = T // 512
            steps = [(h, qb, kt) for h in range(MLA_H) for qb in range(NQ) for kt in range(NA)]
            NS = len(steps)

            def emit_S(i):
                h, qb, kt = steps[i]
                par = h % 2
                hp = h % 2
                qb0 = qb * 512
                sbk = i % 3
                P.mm(cx.PS[sbk][:], KhT[par][:, kt * 128:(kt + 1) * 128], QhT[par][:, qb0:qb0 + 512], True, False,
                     reads=[("B_K", par), ("B_Q", par)], writes=[("ps", sbk)])
                P.mm(cx.PS[sbk][:], krT[hp * 64:(hp + 1) * 64, kt * 128:(kt + 1) * 128],
                     qrT[hp * 64:(hp + 1) * 64, h // 2, qb0:qb0 + 512], False, True,
                     reads=[("A_krT", kt)] + [("A_qrT", (qb0 // 128) + q) for q in range(4)], writes=[("ps", sbk)])
                pt = pT[sbk]
                P.add("act", lambda e, pt=pt, sbk=sbk: e.activation(pt[:], cx.PS[sbk][:], AF.Exp, scale=MLA_SCALE),
                      writes=[("ps", sbk), ("B_pT", sbk)])

            def emit_PV(i):
                h, qb, kt = steps[i]
                par = h % 2
                qb0 = qb * 512
                sbk = i % 3
                blk = i // NA
                ob = 3 + 2 * (blk % 2)
                db = ob + 1
                pt = pT[sbk]
                P.mm(cx.PS[ob][:, :], Vh[par][:, kt, :], pt[:], kt == 0, kt == NA - 1,
                     reads=[("B_V", par), ("B_pT", sbk)], writes=[("ps", ob)])
                P.mm(cx.PS[db][:, :], cx.ones[:], pt[:], kt == 0, kt == NA - 1,
                     reads=[("B_pT", sbk)], writes=[("ps", db)])
                if kt == NA - 1:
                    P.add("dve", lambda e, db=db: e.reciprocal(rden[:], cx.PS[db][:]), writes=[("ps", db), "B_rden"])
                    att = at[blk % 2]
                    Rat = ("B_at", blk % 2)
                    P.add("dve", lambda e, att=att, ob=ob: e.tensor_tensor(att[:], cx.PS[ob][:], rden[:], op=ALU.mult),
                          reads=["B_rden"], writes=[("ps", ob), Rat])
                    P.dma("sp", cx.attnT[h, :, qb0:qb0 + 512], att[:], reads=[Rat], writes=[("attnT", qb0 // 512)])

            proj(0)
            proj(1)
            emit_S(0)
            emit_S(1)
            for i in range(NS):
                h, qb, kt = steps[i]
                if qb == 0 and kt == 0 and h >= 1 and h + 1 < MLA_H:
                    proj(h + 1)
                if i + 2 < NS:
                    emit_S(i + 2)
                emit_PV(i)
    with Phase(cx, "A3") as p3:
        pp = PrePost(cx, p3, "A3")
        modt, modr = load_mod(cx, p3, layer, 0, "A3")
        wo = p3.sb([128, 8, D], BF16, "wo")
        P.dma("pool", wo[:], cx.mla_out_w[0].rearrange("(h p) n -> p h n", p=128), writes=["C_wo"])
        a_in = [p3.sb([128, 8, 128], BF16, "a_in") for _ in range(2)]
        o32 = [p3.sb([128, D], F32, "o32") for _ in range(2)]
        sap, sres = src[0]
        dap, dres = dst[0]
        for ti in range(NT):
            ab = ti % 2
            P.dma("sp", a_in[ab][:], cx.attnT[:, :, ti * 128:(ti + 1) * 128].rearrange("h p n -> p h n"),
                  reads=[("attnT", ti // 4)], writes=[("C_a", ab)])
            for mh in range(2):
                bank = 2 * ab + mh
                for h in range(8):
                    P.mm(cx.PS[bank][:], a_in[ab][:, h, :], wo[:, h, mh * 512:(mh + 1) * 512], h == 0, h == 7,
                         reads=[("C_a", ab), "C_wo"], writes=[("ps", bank)])
                P.add("act", lambda e, ab=ab, mh=mh, bank=bank: e.activation(o32[ab][:, mh * 512:(mh + 1) * 512], cx.PS[bank][:], AF.Copy),
                      writes=[("ps", bank), ("C_o", ab)])
            pp.post(o32[ab][:], ("C_o", ab), sap[ti * 128:(ti + 1) * 128, :], (sres, ti), modt[:, MOD_G, :], modr,
                    dap[ti * 128:(ti + 1) * 128, :], (dres, ti))


CH = 64
C_MHF, C_MHB, C_MSK, C_END = 0, 128, 256, 768


def phase_mix0(cx, src, dst):
    P = cx.P
    T, TA, NT, NA = cx.T, cx.TA, cx.NT, cx.NA
    NC = TA // CH
    tiles = tok_tiles(cx, True)
    blocks = [(0, CTX)] + [(CTX + b, 512) for b in range(0, T, 512)]
    with Phase(cx, "X1") as p1:
        pp = PrePost(cx, p1, "X1")
        mods = {s: load_mod(cx, p1, 0, s, "X1") for s in (0, 1)}
        hTt = [p1.sb([128, KC, 128], BF16, "hTt") for _ in range(2)]
        for idx, (s, ti) in enumerate(tiles):
            modt, modr = mods[s]
            sap, sres = src[s]
            R = ("X1_hT", idx % 2)
            pp.pre(sap[ti * 128:(ti + 1) * 128, :], (sres, ti), modt[:, MOD_A, :], modt[:, MOD_B, :], modr,
                   hTt[idx % 2][:], R, bank=idx % 2)
            P.dma("sp", cx.hTd[:, :, idx * 128:(idx + 1) * 128].rearrange("k p n -> p k n"), hTt[idx % 2][:],
                  reads=[R], writes=[("hTd", idx // 4)])
    with Phase(cx, "X2") as ph:
        cst = ph.sb([128, C_END], F32, "cst")
        P.dma("sp", cst[:], cx.cst, writes=["X_cst"])
        lgt = ph.sb([128, 8], F32, "lgt")
        P.dma("sp", lgt[:], bcast_row(cx.ret_decay_logit[0:1].rearrange("o a b -> o (a b)"), 128), writes=["X_lg"])
        P.add("act", lambda e: e.activation(lgt[:], lgt[:], AF.Sigmoid), reads=["X_lg"], writes=["X_lg"])
        P.add("act", lambda e: e.activation(lgt[:], lgt[:], AF.Ln), reads=["X_lg"], writes=["X_lg"])
        lb = ph.sb([128, 3, 4], F32, "lb")
        P.dma("sp", lb[:, 0:2, :], cx.hgrn_lbl, writes=["X_lb"])
        P.add("dve", lambda e: e.tensor_tensor(lb[:, 2, :], lb[:, 0, :], lb[:, 1, :], op=ALU.subtract), reads=["X_lb"],
              writes=["X_lb2"])
        P.add("act", lambda e: e.activation(lb[:, 0, :], lb[:, 2, :], AF.Sigmoid), reads=["X_lb2"], writes=["X_lb3"])
        P.add("dve", lambda e: e.tensor_scalar(lb[:, 1, :], lb[:, 0, :], -1.0, 1.0, op0=ALU.mult, op1=ALU.add),
              reads=["X_lb3"], writes=["X_lb4"])
        LB = ["X_lb3", "X_lb4"]
        hTb = [ph.sb([128, KC, 512], BF16, "hTb") for _ in range(2)]
        wh = ph.sb([128, KC, 5, 128], BF16, "wh")
        qs = ph.sb([128, TA], BF16, "qs")
        gs = ph.sb([128, TA], BF16, "gs")
        kkr = ph.sb([128, TA], BF16, "kkr")
        v_h = ph.sb([128, NA, 128], BF16, "v_h")
        o_acc = ph.sb([128, TA], F32, "o_acc")
        q1 = ph.sb([128, TA], BF16, "q1")
        k1 = ph.sb([128, TA], BF16, "k1")
        k1t = ph.sb([128, NA, 128], BF16, "k1t")
        S_all = ph.sb([128, NC, 128], BF16, "S_all")
        S32 = [ph.sb([128, 128], F32, "S32") for _ in range(2)]
        er = ph.sb([128, NC], F32, "er")
        etad = ph.sb([128, NC], F32, "etad")
        ek = ph.sb([128, NC], F32, "ek")
        F = [ph.sb([128, 512], F32, "F%d" % i) for i in range(8)]
        raw = [ph.sb([128, 512], BF16, "raw%d" % i) for i in range(2)]
        csb = [ph.sb([128, 2, 512], F32, "csb") for _ in range(2)]
        am = [ph.sb([128, 128], BF16, "am") for _ in range(2)]
        kvt = [ph.sb([128, 128], F32, "kvt") for _ in range(4)]
        gconst = ph.sb([128, 512], F32, "gconst")
        sqb = ph.sb([128, 512], BF16, "sqb")
        yT = [ph.sb([128, 512], BF16, "yT") for _ in range(2)]
        nld = [0]

        def load_h(bi):
            b0, W = blocks[bi]
            i = nld[0] % 2
            nld[0] += 1
            R = ("X_hTb", i)
            P.dma("sp", hTb[i][:, :, 0:W], cx.hTd[:, :, b0:b0 + W].rearrange("k p n -> p k n"),
                  reads=[("hTd", q) for q in range(b0 // 512, (b0 + W + 511) // 512)], writes=[R])
            return hTb[i], R

        def proj_fm(ht, R, W, widx, bank):
            for k in range(KC):
                P.mm(cx.PS[bank][:, 0:W], wh[:, k, widx, :], ht[:, k, 0:W], k == 0, k == KC - 1, reads=[R, "X_wh"],
                     writes=[("ps", bank)])

        for hd in range(8):
            is_ret = hd < 4
            h = hd % 4
            cols = ([0, 512, 1536, 1024] if is_ret else [2048, 2560, 3072, 4096, 3584])
            for wi_, c0 in enumerate(cols):
                P.dma("pool", wh[:, :, wi_, :], cx.mix_in_w[0][:, c0 + h * 128:c0 + (h + 1) * 128]
                      .rearrange("(k p) n -> p k n", p=128), writes=["X_wh"])
            VI = 3 if is_ret else 4
            GI = 2 if is_ret else 3
            for bi, (b0, W) in enumerate(blocks):
                ht, R = load_h(bi)
                sl = slice(b0, b0 + W)
                lat = b0 >= CTX
                proj_fm(ht, R, W, 0, 0)
                if is_ret:
                    proj_fm(ht, R, W, 1, 1)
                proj_fm(ht, R, W, GI, 2)
                P.add("act", lambda e, sl=sl, W=W: e.activation(gs[:, sl], cx.PS[2][:, 0:W], AF.Silu),
                      writes=[("ps", 2), "X_gs"])
                if not is_ret:
                    P.add("act", lambda e, sl=sl, W=W: e.activation(qs[:, sl], cx.PS[0][:, 0:W], AF.Silu),
                          writes=[("ps", 0), "X_qs"])
                else:
                    for (bank, dstt, scl, Rd) in ((0, qs, 1.0, "X_qs"), (1, kkr, 128 ** -0.5, "X_kk")):
                        if not lat:
                            P.add("act", lambda e, bank=bank, dstt=dstt, scl=scl, sl=sl, W=W: e.activation(
                                dstt[:, sl], cx.PS[bank][:, 0:W], AF.Copy, scale=scl), writes=[("ps", bank), Rd])
                            continue
                        rw_ = raw[bank]
                        Rr = ("X_raw", bank)
                        P.add("act", lambda e, bank=bank, rw_=rw_, scl=scl, W=W: e.activation(
                            rw_[:, 0:W], cx.PS[bank][:, 0:W], AF.Copy, scale=scl), writes=[("ps", bank), Rr])
                        ci = (bi + bank) % 2
                        Rc = ("X_cs", ci)
                        if bank == 0:
                            P.dma("sp", csb[ci][:, :, 0:W], cx.rope_ret[:, :, b0 - CTX:b0 - CTX + W].rearrange("c p n -> p c n"),
                                  writes=[Rc])
                        else:
                            ci = bi % 2
                            Rc = ("X_cs", ci)
                        sb_ = 3
                        P.mm(cx.PS[sb_][:, 0:W], cx.perm[:], rw_[:, 0:W], True, True, reads=[Rr], writes=[("ps", sb_)])
                        P.add("pool", lambda e, rw_=rw_, ci=ci, W=W: e.tensor_tensor(F[0][:, 0:W], rw_[:, 0:W], csb[ci][:, 0, 0:W], op=ALU.mult),
                              reads=[Rr, Rc], writes=["X_F0"])
                        P.add("dve", lambda e, ci=ci, W=W, sb_=sb_: e.tensor_tensor(F[1][:, 0:W], cx.PS[sb_][:, 0:W], csb[ci][:, 1, 0:W], op=ALU.mult),
                              reads=[Rc], writes=[("ps", sb_), "X_F1"])
                        P.add("pool", lambda e, dstt=dstt, sl=sl, W=W: e.tensor_tensor(dstt[:, sl], F[0][:, 0:W], F[1][:, 0:W], op=ALU.add),
                              reads=["X_F0", "X_F1"], writes=[Rd])
                for q in range(W // 128):
                    ta = b0 // 128 + q
                    bank = 4 + ta % 2
                    for k in range(KC):
                        P.mm(cx.PS[bank][:, 0:128], ht[:, k, q * 128:(q + 1) * 128], wh[:, k, VI, :], k == 0, k == KC - 1,
                             reads=[R, "X_wh"], writes=[("ps", bank)])
                    P.add("dve", lambda e, ta=ta, bank=bank: e.tensor_copy(v_h[:, ta, :], cx.PS[bank][:, 0:128]),
                          writes=[("ps", bank), "X_v"])
            for dr in range(2):
                mh = cst[:, C_MHF:C_MHF + 128] if dr == 0 else cst[:, C_MHB:C_MHB + 128]
                if is_ret:
                    col = dr * 4 + h
                    P.add("dve", lambda e, col=col: e.tensor_copy(gconst[:], lgt[:, col:col + 1].to_broadcast([128, 512])),
                          reads=["X_lg"], writes=["X_gc"])
                def pipe(W, c0, g_ap, Rg):
                    nch = W // CH
                    v3 = lambda t, W=W: t[:, 0:W].rearrange("p (c n) -> p c n", n=CH)
                    P.add("dve", lambda e, W=W, g_ap=g_ap: e.tensor_tensor_scan(F[2][:, 0:W], cst[:, C_MSK:C_MSK + W], g_ap[:, 0:W], 0.0,
                                                                          op0=ALU.mult, op1=ALU.add),
                          reads=[Rg, "X_cst"], writes=["X_F2"])
                    tot = v3(F[2])[:, :, CH - 1:CH]
                    if dr == 0:
                        b_t = F[2]
                        Rb = "X_F2"
                    else:
                        P.add("dve", lambda e, W=W, g_ap=g_ap: e.tensor_tensor(F[3][:, 0:W], g_ap[:, 0:W], F[2][:, 0:W], op=ALU.subtract),
                              reads=[Rg, "X_F2"], writes=["X_F3"])
                        P.add("pool", lambda e, v3=v3, tot=tot, nch=nch: e.tensor_tensor(v3(F[3]), v3(F[3]), tot.to_broadcast([128, nch, CH]), op=ALU.add),
                              reads=["X_F2", "X_F3"], writes=["X_F3"])
                        b_t = F[3]
                        Rb = "X_F3"
                    rr = v3(b_t)[:, :, 31:32]
                    P.add("act", lambda e, rr=rr, c0=c0, nch=nch: e.activation(er[:, c0:c0 + nch].unsqueeze(2), rr, AF.Exp),
                          reads=[Rb], writes=["X_er"])
                    P.add("act", lambda e, tot=tot, c0=c0, nch=nch: e.activation(etad[:, c0:c0 + nch].unsqueeze(2), tot, AF.Exp),
                          reads=["X_F2"], writes=["X_etad"])
                    P.add("dve", lambda e, tot=tot, rr=rr, c0=c0, nch=nch: e.tensor_tensor(ek[:, c0:c0 + nch].unsqueeze(2), tot, rr, op=ALU.subtract),
                          reads=["X_F2", Rb], writes=["X_ek"])
                    P.add("act", lambda e, c0=c0, nch=nch: e.activation(ek[:, c0:c0 + nch], ek[:, c0:c0 + nch], AF.Exp),
                          reads=["X_ek"], writes=["X_ek"])
                    P.add("dve", lambda e, v3=v3, b_t=b_t, rr=rr, nch=nch: e.tensor_tensor(v3(F[4]), v3(b_t), rr.to_broadcast([128, nch, CH]), op=ALU.subtract),
                          reads=[Rb], writes=["X_F4"])
                    P.add("act", lambda e, W=W: e.activation(F[5][:, 0:W], F[4][:, 0:W], AF.Exp), reads=["X_F4"], writes=["X_F5"])
                    P.add("act", lambda e, W=W: e.activation(F[6][:, 0:W], F[4][:, 0:W], AF.Exp, scale=-1.0), reads=["X_F4"], writes=["X_F6"])

                if is_ret:
                    pipe(512, 0, gconst, "X_gc")
                    for tl_, Rt in ((er, "X_er"), (etad, "X_etad"), (ek, "X_ek")):
                        P.add("dve", lambda e, tl_=tl_: e.tensor_copy(tl_[:, 8:NC], tl_[:, 0:1].to_broadcast([128, NC - 8])),
                              reads=[Rt], writes=[Rt])
                for bi, (b0, W) in enumerate(blocks):
                    sl = slice(b0, b0 + W)
                    if is_ret:
                        kk_ap = kkr[:, sl]
                        Rkk = "X_kk"
                    else:
                        ht, R = load_h(bi)
                        proj_fm(ht, R, W, 1 + dr, 0)
                        P.add("act", lambda e, W=W: e.activation(F[0][:, 0:W], cx.PS[0][:, 0:W], AF.Sigmoid),
                              writes=[("ps", 0), "X_F0"])
                        P.add("dve", lambda e, W=W, h=h: e.tensor_scalar(F[0][:, 0:W], F[0][:, 0:W], lb[:, 1, h:h + 1], lb[:, 0, h:h + 1],
                                                                       op0=ALU.mult, op1=ALU.add), reads=["X_F0"] + LB, writes=["X_F0"])
                        P.add("act", lambda e, W=W: e.activation(F[1][:, 0:W], F[0][:, 0:W], AF.Ln), reads=["X_F0"], writes=["X_F1"])
                        rk = raw[bi % 2]
                        P.add("dve", lambda e, W=W, rk=rk: e.tensor_scalar(rk[:, 0:W], F[0][:, 0:W], -1.0, 1.0, op0=ALU.mult, op1=ALU.add),
                              reads=["X_F0"], writes=[("X_raw", bi % 2)])
                        kk_ap = rk[:, 0:W]
                        Rkk = ("X_raw", bi % 2)
                        pipe(W, b0 // CH, F[1], "X_F1")
                    P.add("pool", lambda e, sl=sl, W=W: e.tensor_tensor(q1[:, sl], qs[:, sl], F[5][:, 0:W], op=ALU.mult),
                          reads=["X_qs", "X_F5"], writes=["X_q1"])
                    P.add("dve", lambda e, sl=sl, W=W, kk_ap=kk_ap: e.tensor_tensor(k1[:, sl], kk_ap, F[6][:, 0:W], op=ALU.mult),
                          reads=[Rkk, "X_F6"], writes=["X_k1"])
                    bank = 6 + bi % 2
                    nq = W // 128
                    for q in range(nq):
                        P.tr(cx.PSB[bank][:, q * 128:(q + 1) * 128], k1[:, b0 + q * 128:b0 + (q + 1) * 128], cx.ident[:],
                             reads=["X_k1"], writes=[("ps", bank)])
                    P.add("act", lambda e, b0=b0, nq=nq, bank=bank: e.activation(
                        k1t[:, b0 // 128:b0 // 128 + nq, :], cx.PSB[bank][:, 0:nq * 128].rearrange("p (t d) -> p t d", d=128), AF.Copy),
                        writes=[("ps", bank), "X_k1t"])
                if dr == 0:
                    order = list(range(NA))
                else:
                    order = [1, 0] + list(range(NA - 1, 1, -1))
                halves = (0, 1) if dr == 0 else (1, 0)
                P.add("pool", lambda e: e.memset(S32[0][:], 0.0), writes=[("X_S32", 0)])
                n_t = len(order)

                def st_A(i):
                    ta = order[i]
                    for hi, hf in enumerate(halves):
                        k = 2 * i + hi
                        c = ta * 2 + hf
                        kb = k % 2
                        kt_ = kvt[k % 4]
                        Rkt = ("X_kvt", k % 4)
                        P.mm(cx.PS[kb][:, 0:128], k1t[hf * 64:(hf + 1) * 64, ta, :], v_h[hf * 64:(hf + 1) * 64, ta, :], True, True,
                             reads=["X_k1t", "X_v"], writes=[("ps", kb)])
                        P.add("act", lambda e, kt_=kt_, kb=kb, c=c: e.activation(kt_[:], cx.PS[kb][:, 0:128], AF.Copy, scale=ek[:, c:c + 1]),
                              reads=["X_ek"], writes=[("ps", kb), Rkt])
                    ab_ = 2 + i % 2
                    tsl = slice(ta * 128, (ta + 1) * 128)
                    P.mm(cx.PS[ab_][:, 0:128], k1[:, tsl], q1[:, tsl], True, True, reads=["X_k1", "X_q1"], writes=[("ps", ab_)])

                def st_B(i):
                    ta = order[i]
                    for hi, hf in enumerate(halves):
                        k = 2 * i + hi
                        c = ta * 2 + hf
                        src, dst = S32[k % 2], S32[(k + 1) % 2]
                        P.add("act", lambda e, c=c, src=src: e.activation(S_all[:, c, :], src[:], AF.Copy, scale=er[:, c:c + 1]),
                              reads=[("X_S32", k % 2), "X_er"], writes=[("X_Sall", c % 8)])
                        kt_ = kvt[k % 4]
                        P.add("dve", lambda e, kt_=kt_, c=c, src=src, dst=dst: e.scalar_tensor_tensor(
                            dst[:], src[:], etad[:, c:c + 1], kt_[:], op0=ALU.mult, op1=ALU.add),
                            reads=[("X_S32", k % 2), "X_etad", ("X_kvt", k % 4)], writes=[("X_S32", (k + 1) % 2)])
                    ab_ = 2 + i % 2
                    amt = am[i % 2]
                    P.add("dve", lambda e, amt=amt, ab_=ab_, mh=mh: e.tensor_tensor(amt[:], cx.PS[ab_][:, 0:128], mh, op=ALU.mult),
                          reads=["X_cst"], writes=[("ps", ab_), ("X_am", i % 2)])

                def st_C(i):
                    ta = order[i]
                    amt = am[i % 2]
                    ob = 4 + i % 2
                    P.mm(cx.PS[ob][:, 0:128], v_h[:, ta, :], amt[:], True, False, reads=["X_v", ("X_am", i % 2)], writes=[("ps", ob)])
                    for hf in (0, 1):
                        c = ta * 2 + hf
                        P.mm(cx.PS[ob][:, hf * 64:(hf + 1) * 64], S_all[:, c, :], q1[:, ta * 128 + hf * 64:ta * 128 + (hf + 1) * 64],
                             False, hf == 1, reads=[("X_Sall", c % 8), "X_q1"], writes=[("ps", ob)])

                def st_D(i):
                    ta = order[i]
                    ob = 4 + i % 2
                    tsl = slice(ta * 128, (ta + 1) * 128)
                    if dr == 0:
                        P.add("dve", lambda e, tsl=tsl, ob=ob: e.tensor_copy(o_acc[:, tsl], cx.PS[ob][:, 0:128]),
                              writes=[("ps", ob), "X_oacc"])
                    else:
                        P.add("dve", lambda e, tsl=tsl, ob=ob: e.tensor_tensor(o_acc[:, tsl], o_acc[:, tsl], cx.PS[ob][:, 0:128], op=ALU.add),
                              reads=["X_oacc"], writes=[("ps", ob), "X_oacc"])

                for i in range(n_t + 3):
                    if i < n_t:
                        st_A(i)
                    if 0 <= i - 1 < n_t:
                        st_B(i - 1)
                    if 0 <= i - 2 < n_t:
                        st_C(i - 2)
                    if 0 <= i - 3 < n_t:
                        st_D(i - 3)
            for bi, (b0, W) in enumerate(blocks):
                sl = slice(b0, b0 + W)
                P.add("act", lambda e, sl=sl, W=W: e.activation(sqb[:, 0:W], o_acc[:, sl], AF.Square),
                      reads=["X_oacc"], writes=["X_sqb"])
                bank = 7
                P.mm(cx.PS[bank][:, 0:W], cx.ones[:], sqb[:, 0:W], True, True, reads=["X_sqb"], writes=[("ps", bank)])
                P.add("dve", lambda e, W=W, bank=bank: e.tensor_scalar(F[7][:, 0:W], cx.PS[bank][:, 0:W], 1.0 / 128, EPS, op0=ALU.mult, op1=ALU.add),
                      writes=[("ps", bank), "X_F7"])
                P.add("act", lambda e, W=W: e.activation(F[7][:, 0:W], F[7][:, 0:W], AF.Sqrt), reads=["X_F7"], writes=["X_F7"])
                P.add("dve", lambda e, W=W: e.reciprocal(F[7][:, 0:W], F[7][:, 0:W]), reads=["X_F7"], writes=["X_F7"])
                P.add("dve", lambda e, sl=sl, W=W: e.tensor_tensor(F[7][:, 0:W], F[7][:, 0:W], o_acc[:, sl], op=ALU.mult),
                      reads=["X_F7", "X_oacc"], writes=["X_F7"])
                yt = yT[bi % 2]
                Ry = ("X_yT", bi % 2)
                P.add("pool", lambda e, sl=sl, W=W, yt=yt: e.tensor_tensor(yt[:, 0:W], F[7][:, 0:W], gs[:, sl], op=ALU.mult),
                      reads=["X_F7", "X_gs"], writes=[Ry])
                P.dma("sp", cx.yTd[hd, :, b0:b0 + W], yt[:, 0:W], reads=[Ry], writes=[("yTd", b0 // 512)])
    with Phase(cx, "X3") as p3:
        pp = PrePost(cx, p3, "X3")
        mods = {s: load_mod(cx, p3, 0, s, "X3") for s in (0, 1)}
        wo = p3.sb([128, 8, D], BF16, "wo")
        P.dma("pool", wo[:], cx.mix_out_w[0].rearrange("(h p) n -> p h n", p=128), writes=["X3_wo"])
        a_in = [p3.sb([128, 8, 128], BF16, "a_in") for _ in range(2)]
        o32 = [p3.sb([128, D], F32, "o32") for _ in range(2)]
        for idx, (s, ti) in enumerate(tiles):
            ab = idx % 2
            modt, modr = mods[s]
            sap, sres = src[s]
            dap, dres = dst[s]
            P.dma("sp", a_in[ab][:], cx.yTd[:, :, idx * 128:(idx + 1) * 128].rearrange("h p n -> p h n"),
                  reads=[("yTd", q) for q in range(0, (TA + 511) // 512)], writes=[("X3_a", ab)])
            for mh_ in range(2):
                bank = 2 * ab + mh_
                for hh in range(8):
                    P.mm(cx.PS[bank][:], a_in[ab][:, hh, :], wo[:, hh, mh_ * 512:(mh_ + 1) * 512], hh == 0, hh == 7,
                         reads=[("X3_a", ab), "X3_wo"], writes=[("ps", bank)])
                P.add("act", lambda e, ab=ab, mh_=mh_, bank=bank: e.activation(o32[ab][:, mh_ * 512:(mh_ + 1) * 512], cx.PS[bank][:], AF.Copy),
                      writes=[("ps", bank), ("X3_o", ab)])
            pp.post(o32[ab][:], ("X3_o", ab), sap[ti * 128:(ti + 1) * 128, :], (sres, ti), modt[:, MOD_G, :], modr,
                    dap[ti * 128:(ti + 1) * 128, :], (dres, ti))


ALL_PHASES = ("mix0", "ffn0", "mla", "moe")

W_SHAPES = {
    "mod_w": [2, D, 6 * D], "mod_b": [2, 6 * D], "norm_g": [2, 4, D], "mix_in_w": [1, D, 4608],
    "ret_decay_logit": [1, 2, 4], "mix_out_w": [1, D, D], "ffn_in_w": [1, D, 2 * FFN], "ffn_out_w": [1, FFN, D],
    "mla_down_w": [1, D, 704], "mla_q_norm_g": [1, 384], "mla_kv_norm_g": [1, 256], "mla_uq_w": [1, 384, 1536],
    "mla_ukv_w": [1, 256, 2048], "mla_out_w": [1, D, D], "router_w": [1, D, NE], "moe_in_w": [1, NE, D, 2 * FFN],
    "moe_out_w": [1, NE, FFN, D],
}


def build(T, phases=ALL_PHASES):
    nc = bass.Bass("TRN2", target_bir_lowering=False)
    cx = Cx()
    cx.nc = nc
    cx.T, cx.TA, cx.NT, cx.NA = T, T + CTX, T // 128, (T + CTX) // 128
    TA = cx.TA

    def din(name, shape, dt=F32):
        return nc.dram_tensor(name, list(shape), dt, kind="ExternalInput").ap()

    def dscr(name, shape, dt=F32):
        return nc.dram_tensor(name, list(shape), dt, kind="Internal").ap()

    cx.x = din("x", [T, D])
    cx.ctx = din("ctx", [CTX, D])
    cx.ccol = din("ccol", [128, 16])
    for k, shp in W_SHAPES.items():
        setattr(cx, k, din(k, shp))
    cx.hgrn_lbl = din("hgrn_lbl", [128, 2, 4])
    cx.cst = din("cst", [128, C_END])
    cx.mats = din("mats", [3, 128, 128])
    cx.rope_ret = din("rope_ret", [2, 128, T])
    cx.rope_mla = din("rope_mla", [T, 64])
    cx.out = nc.dram_tensor("out", [T, D], F32, kind="ExternalOutput").ap()
    cx.modb = dscr("modb", [2, 2, 6, 128, D])
    cx.rx = dscr("rx", [T, D])
    cx.rc = dscr("rc", [CTX, D])
    cx.hTd = dscr("hTd", [KC, 128, TA], BF16)
    cx.yTd = dscr("yTd", [8, 128, TA], BF16)
    import os
    DBG = os.environ.get("KDBG", "")
    cx.attnT = (nc.dram_tensor("attnT", [8, 128, T], BF16, kind="ExternalOutput").ap() if DBG == "attnT"
                else dscr("attnT", [8, 128, T], BF16))
    P = cx.P = Prog(nc)
    if DBG == "attnT":
        cx.dbg = {"qnT": nc.dram_tensor("d_qnT", [128, 3, T], BF16, kind="ExternalOutput").ap(),
                  "ckvT": nc.dram_tensor("d_ckvT", [128, 2, TA], BF16, kind="ExternalOutput").ap(),
                  "krT": nc.dram_tensor("d_krT", [128, TA], BF16, kind="ExternalOutput").ap(),
                  "qrT": nc.dram_tensor("d_qrT", [128, 4, T], BF16, kind="ExternalOutput").ap()}
    with contextlib.ExitStack() as gst:
        cx.PS = [gst.enter_context(nc.psum_tensor("ps%d" % b, [128, 512], F32)) for b in range(8)]
        cx.PSB = [p[:].bitcast(BF16) for p in cx.PS]
        mt = gst.enter_context(nc.sbuf_tensor("mats_sb", [128, 3, 128], BF16))
        P.dma("pool", mt[:], cx.mats.rearrange("i p n -> p i n"), writes=["mats"])
        P.barrier()
        cx.ident, cx.ones, cx.perm = mt[:, 0, :], mt[:, 1, :], mt[:, 2, :]
        phase_mod(cx)
        cur = {0: (cx.x, "x_in"), 1: (cx.ctx, "c_in")}
        res = {0: (cx.rx, "rx"), 1: (cx.rc, "rc")}
        for i, phn in enumerate(phases):
            last = i == len(phases) - 1
            dst = dict(res)
            if last:
                dst[0] = (cx.out, "out")
            if phn == "mix0":
                phase_mix0(cx, cur, dst)
            elif phn == "ffn0":
                phase_ffn(cx, 0, cx.ffn_in_w, cx.ffn_out_w, None, 1, cur, dst, True, "F0")
            elif phn == "mla":
                phase_mla(cx, 1, cur, dst)
            elif phn == "moe":
                phase_ffn(cx, 1, cx.moe_in_w[0], cx.moe_out_w[0], cx.router_w[0], NE, cur, dst, False, "F1")
            if phn in ("mix0", "ffn0"):
                cur = dict(dst)
            else:
                cur = {0: dst[0], 1: cur[1]}
        P.emit(final_wait_resources=[("out", t) for t in range(cx.NT)])
    cx.stats = P.stats
    return nc, cx


def rope_tables(T, dim):
    n_rows = T // GRID_W
    rows = np.repeat(np.arange(n_rows), GRID_W).astype(np.float32)
    cols = np.tile(np.arange(GRID_W), n_rows).astype(np.float32)
    n_freq = dim // 4
    inv = (np.float32(10000.0) ** (-np.arange(n_freq, dtype=np.float32) / np.float32(n_freq))).astype(np.float32)
    ang = np.concatenate([rows[:, None] * inv, cols[:, None] * inv], axis=-1).astype(np.float32)
    return np.cos(ang).astype(np.float32), np.sin(ang).astype(np.float32)


def const_inputs(T):
    p = np.arange(128)
    same = (p[:, None] // CH) == (p[None, :] // CH)
    cst = np.zeros((128, C_END), np.float32)
    cst[:, C_MHF:C_MHF + 128] = (same & (p[:, None] <= p[None, :])).astype(np.float32)
    cst[:, C_MHB:C_MHB + 128] = (same & (p[:, None] >= p[None, :])).astype(np.float32)
    msk = np.ones(512, np.float32)
    msk[::CH] = 0.0
    cst[:, C_MSK:C_MSK + 512] = msk[None, :]
    mats = np.zeros((3, 128, 128), np.float32)
    mats[0] = np.eye(128)
    mats[1] = 1.0
    mats[2][(p + 64) % 128, p] = 1.0
    cr, sr = rope_tables(T, 128)
    rope_ret = np.stack([np.concatenate([cr.T, cr.T], 0), np.concatenate([-sr.T, sr.T], 0)]).astype(np.float32)
    cm, sm = rope_tables(T, 64)
    rope_mla = np.concatenate([cm, sm], axis=1).astype(np.float32)
    return {"cst": cst, "mats": mats, "rope_ret": np.ascontiguousarray(rope_ret), "rope_mla": np.ascontiguousarray(rope_mla)}


def core_inputs(inputs, b, consts):
    f = lambda a: np.ascontiguousarray(np.asarray(a, dtype=np.float32))
    m = {"x": f(inputs["x"][b]), "ctx": f(inputs["ctx"][b])}
    c = f(inputs["c"][b]).reshape(8, 128).T
    cc = f(inputs["c_ctx"]).reshape(8, 128).T
    m["ccol"] = np.ascontiguousarray(np.concatenate([c, cc], axis=1))
    for k in W_SHAPES:
        m[k] = f(inputs[k])
    m["hgrn_lbl"] = np.ascontiguousarray(f(inputs["hgrn_lb_logit"]).reshape(2, 4, 128).transpose(2, 0, 1))
    m.update(consts)
    return m


_CACHE = {}


def kernel(**inputs):
    T = int(np.asarray(inputs["x"]).shape[1])
    B = int(np.asarray(inputs["x"]).shape[0])
    if T not in _CACHE:
        _CACHE[T] = build(T)[0]
    nc = _CACHE[T]
    consts = const_inputs(T)
    in_maps = [core_inputs(inputs, b, consts) for b in range(B)]
    res = run_bass_kernel_spmd(nc, in_maps, core_ids=list(range(B)))
    return np.stack([np.asarray(r["out"], dtype=np.float32) for r in res.results], axis=0)
```

```python
import contextlib
import numpy as np
import concourse.bass as bass
import concourse.mybir as mybir
from concourse.bass_utils import run_bass_kernel_spmd

F32 = mybir.dt.float32
BF16 = mybir.dt.bfloat16
AF = mybir.ActivationFunctionType
ALU = mybir.AluOpType

ENGS = ("pe", "act", "dve", "pool", "sp")
N_DMA_SEMS = 32

D = 1024
KC = 8
CTX = 256
FFN = 2816
FC = 22
NE = 8
EPS = 1e-6
GRID_W = 64


class Op:
    __slots__ = ("eng", "fn", "reads", "writes", "is_dma", "deps", "has_dep", "ticket", "dsem", "dval",
                 "dprev", "extra")

    def __init__(self, eng, fn, reads, writes, is_dma):
        self.eng = eng
        self.fn = fn
        self.reads = reads
        self.writes = writes
        self.is_dma = is_dma
        self.deps = []
        self.has_dep = False
        self.ticket = None
        self.dsem = None
        self.dval = None
        self.dprev = None
        self.extra = ()


class Prog:
    def __init__(self, nc):
        self.nc = nc
        self.ops = []
        self.phase_start = 0

    def add(self, eng, fn, reads=(), writes=(), is_dma=False):
        self.ops.append(Op(eng, fn, tuple(reads), tuple(writes), is_dma))

    def dma(self, eng, out, in_, reads=(), writes=(), **kw):
        self.add(eng, lambda e: e.dma_start(out=out, in_=in_, **kw), reads, writes, is_dma=True)

    def mm(self, out, lhsT, rhs, start, stop, reads=(), writes=()):
        self.add("pe", lambda e: e.matmul(out, lhsT, rhs, start=start, stop=stop), reads, writes)

    def tr(self, out, in_, ident, reads=(), writes=()):
        self.add("pe", lambda e: e.transpose(out, in_, ident), reads, writes)

    def barrier(self):
        last = {}
        dmas = []
        for i in range(self.phase_start, len(self.ops)):
            op = self.ops[i]
            if op.fn is None:
                continue
            if op.is_dma:
                dmas.append(i)
            else:
                last[op.eng] = i
        extra = tuple(sorted(set(last.values()) | set(dmas)))
        for e in ENGS:
            op = Op(e, None, (), (), False)
            op.extra = extra
            self.ops.append(op)
        self.phase_start = len(self.ops)

    def _analyse(self):
        last_w = {}
        readers = {}
        ops = self.ops
        for i, op in enumerate(ops):
            deps = set()
            for r in op.reads:
                w = last_w.get(r)
                if w is not None:
                    deps.add(w)
            for r in op.writes:
                w = last_w.get(r)
                if w is not None:
                    deps.add(w)
                rd = readers.get(r)
                if rd:
                    deps.update(rd.values())
            keep = []
            for j in deps:
                p = ops[j]
                if p.eng == op.eng and not p.is_dma and not op.is_dma and op.eng == "pe":
                    continue
                keep.append(j)
            for j in op.extra:
                p = ops[j]
                if p.eng == op.eng and not p.is_dma:
                    continue
                keep.append(j)
            op.deps = sorted(set(keep))
            for j in op.deps:
                ops[j].has_dep = True
            key = ("dma", i) if op.is_dma else op.eng
            for r in op.reads:
                d = readers.get(r)
                if d is None:
                    d = readers[r] = {}
                d[key] = i
            for r in op.writes:
                last_w[r] = i
                readers[r] = {}

    def emit(self, final_wait_resources=()):
        nc = self.nc
        self.add("sp", None, reads=tuple(final_wait_resources), writes=())
        self._analyse()
        with contextlib.ExitStack() as st:
            esem = {e: st.enter_context(nc.semaphore("s_" + e)) for e in ENGS}
            dsems = [st.enter_context(nc.semaphore("d%d" % k)) for k in range(N_DMA_SEMS)]
            cnt = {e: 0 for e in ENGS}
            dcnt = [0] * N_DMA_SEMS
            dlast = [None] * N_DMA_SEMS
            k = 0
            kq = {"sp": 0, "pool": 0}
            half = N_DMA_SEMS // 2
            for i, op in enumerate(self.ops):
                if op.is_dma:
                    s = (kq[op.eng] % half) + (0 if op.eng == "sp" else half)
                    kq[op.eng] += 1
                    k += 1
                    op.dsem = s
                    dcnt[s] += 16
                    op.dval = dcnt[s]
                    op.dprev = dlast[s]
                    dlast[s] = i
                elif op.has_dep:
                    cnt[op.eng] += 1
                    op.ticket = cnt[op.eng]
            self.stats = dict(cnt)
            self.stats["n_ops"] = len(self.ops)
            self.stats["n_dma"] = k
            block = st.enter_context(nc.Block())
            ops = self.ops

            def run(engname):
                def body(e):
                    seen_e = {x: 0 for x in ENGS}
                    seen_d = [0] * N_DMA_SEMS
                    for op in ops:
                        if op.eng != engname:
                            continue
                        deps = op.deps
                        if op.is_dma and op.dprev is not None:
                            deps = deps + [op.dprev]
                        for j in deps:
                            p = ops[j]
                            if p.is_dma:
                                if seen_d[p.dsem] < p.dval:
                                    e.wait_ge(dsems[p.dsem], p.dval)
                                    seen_d[p.dsem] = p.dval
                            else:
                                if seen_e[p.eng] < p.ticket:
                                    e.wait_ge(esem[p.eng], p.ticket)
                                    seen_e[p.eng] = p.ticket
                        if op.fn is None:
                            continue
                        ins = op.fn(e)
                        if op.is_dma:
                            ins.then_inc(dsems[op.dsem], 16)
                        elif op.ticket is not None:
                            ins.then_inc(esem[op.eng], 1)
                return body

            block.tensor(run("pe"))
            block.scalar(run("act"))
            block.vector(run("dve"))
            block.gpsimd(run("pool"))
            block.sync(run("sp"))


class Cx:
    pass


class Phase:
    def __init__(self, cx, name):
        self.cx = cx
        self.name = name
        self.st = contextlib.ExitStack()
        self.n = 0

    def __enter__(self):
        self.st.__enter__()
        return self

    def sb(self, shape, dt, name=None):
        self.n += 1
        nm = "%s_%s%d" % (self.name, name or "t", self.n)
        t = self.st.enter_context(self.cx.nc.sbuf_tensor(nm, list(shape), dt))
        return t

    def __exit__(self, *a):
        self.cx.P.barrier()
        return self.st.__exit__(*a)


def bcast_row(ap_row, n):
    return ap_row.to_broadcast([n, ap_row.shape[-1]])


class PrePost:
    def __init__(self, cx, ph, tag):
        self.cx = cx
        self.tag = tag
        self.xt = [ph.sb([128, D], F32, "xt") for _ in range(2)]
        self.t1 = [ph.sb([128, D], F32, "t1") for _ in range(2)]
        self.hb = [ph.sb([128, D], BF16, "hb") for _ in range(2)]
        self.sq = ph.sb([128, D], BF16, "sq")
        self.st = ph.sb([128, 2, 8], F32, "st")
        self.n = 0

    def rstd(self, ss, rs, inv_n, deps_r, deps_w):
        P = self.cx.P
        P.add("dve", lambda e: e.tensor_scalar(rs, ss, inv_n, EPS, op0=ALU.mult, op1=ALU.add),
              reads=deps_r, writes=deps_w)
        P.add("act", lambda e: e.activation(rs, rs, AF.Sqrt), reads=deps_w, writes=deps_w)
        P.add("dve", lambda e: e.reciprocal(rs, rs), reads=deps_w, writes=deps_w)

    def pre(self, src_ap, src_res, A, B, mod_res, hT_dst, hT_res, bank):
        cx, P, tg = self.cx, self.cx.P, self.tag
        i = self.n
        self.n += 1
        b = i % 2
        xt, t1, hb, sq = self.xt[b], self.t1[b], self.hb[b], self.sq
        rx = (tg, "xt", b)
        P.dma("sp", xt[:], src_ap, reads=[src_res], writes=[rx])
        ss = self.st[:, b, 0:1]
        rs = self.st[:, b, 1:2]
        P.add("act", lambda e: e.activation(sq[:], xt[:], AF.Square, accum_out=ss),
              reads=[rx], writes=[(tg, "sq"), (tg, "ss", b)])
        self.rstd(ss, rs, 1.0 / D, [(tg, "ss", b)], [(tg, "rs", b)])
        P.add("dve", lambda e: e.scalar_tensor_tensor(t1[:], xt[:], rs, A, op0=ALU.mult, op1=ALU.mult),
              reads=[rx, (tg, "rs", b), mod_res], writes=[(tg, "t1", b)])
        P.add("pool", lambda e: e.tensor_tensor(hb[:], t1[:], B, op=ALU.add),
              reads=[(tg, "t1", b), mod_res], writes=[(tg, "hb", b)])
        psb = cx.PSB[bank]
        for k in range(KC):
            P.tr(psb[:, k * 128:(k + 1) * 128], hb[:, k * 128:(k + 1) * 128], cx.ident[:],
                 reads=[(tg, "hb", b)], writes=[("ps", bank)])
        P.add("act", lambda e: e.activation(hT_dst, psb[:, 0:D].rearrange("p (k n) -> p k n", k=KC), AF.Copy),
              reads=[], writes=[("ps", bank), hT_res])

    def post(self, o_src, o_res, res_ap, res_res, G, mod_res, dst_ap, dst_res):
        cx, P, tg = self.cx, self.cx.P, self.tag
        i = self.n
        self.n += 1
        b = i % 2
        xt, t1, sq = self.xt[b], self.t1[b], self.sq
        rx = (tg, "xt", b)
        P.dma("sp", xt[:], res_ap, reads=[res_res], writes=[rx])
        ss = self.st[:, b, 2:3]
        rs = self.st[:, b, 3:4]
        P.add("act", lambda e: e.activation(sq[:], o_src, AF.Square, accum_out=ss),
              reads=[o_res], writes=[(tg, "sq"), (tg, "ss2", b)])
        self.rstd(ss, rs, 1.0 / D, [(tg, "ss2", b)], [(tg, "rs2", b)])
        P.add("dve", lambda e: e.scalar_tensor_tensor(t1[:], o_src, rs, G, op0=ALU.mult, op1=ALU.mult),
              reads=[o_res, (tg, "rs2", b), mod_res], writes=[(tg, "t1", b)])
        P.add("pool", lambda e: e.tensor_tensor(xt[:], t1[:], xt[:], op=ALU.add),
              reads=[(tg, "t1", b), rx], writes=[rx])
        P.dma("sp", dst_ap, xt[:], reads=[rx], writes=[dst_res])


def load_mod(cx, ph, layer, stream, tag):
    t = ph.sb([128, 6, D], F32, "mod")
    res = (tag, "mod", stream)
    cx.P.dma("sp", t[:], cx.modb[layer, stream].rearrange("i p n -> p i n"), reads=[("modb", layer, stream)],
             writes=[res])
    return t, res


def tok_tiles(cx, with_ctx):
    tl = []
    if with_ctx:
        tl += [(1, i) for i in range(CTX // 128)]
    tl += [(0, i) for i in range(cx.NT)]
    return tl


def phase_mod(cx):
    P = cx.P
    with Phase(cx, "M") as ph:
        sc = ph.sb([128, 16], F32, "sc")
        lh = ph.sb([128, 16, 128], BF16, "lh")
        P.dma("sp", sc[:], cx.ccol, writes=["M_sc"])
        P.add("act", lambda e: e.activation(sc[:], sc[:], AF.Silu), reads=["M_sc"], writes=["M_sc"])
        for j in range(16):
            P.add("dve", lambda e, j=j: e.tensor_copy(lh[:, j, :], sc[:, j:j + 1].to_broadcast([128, 128])),
                  reads=["M_sc"], writes=["M_lh"])
        NH = 3072
        wb = ph.sb([128, KC, NH], BF16, "wb")
        bb = ph.sb([128, NH], F32, "bb")
        gb = ph.sb([128, 4, D], F32, "gb")
        m = ph.sb([128, NH], F32, "m")
        o = ph.sb([128, 3, D], F32, "o")
        for l in range(2):
            P.dma("sp", gb[:], bcast_row(cx.norm_g[l:l + 1].rearrange("o i n -> o (i n)"), 128)
                  .rearrange("p (i n) -> p i n", i=4), writes=["M_gb"])
            for half in range(2):
                for k in range(KC):
                    P.dma("pool", wb[:, k, :], cx.mod_w[l, k * 128:(k + 1) * 128, half * NH:(half + 1) * NH],
                          writes=[("M_wb", k)])
                P.dma("sp", bb[:], bcast_row(cx.mod_b[l:l + 1, half * NH:(half + 1) * NH], 128), writes=["M_bb"])
                for s in range(2):
                    for nb in range(NH // 512):
                        bank = nb % 4
                        for k in range(KC):
                            P.mm(cx.PS[bank][:], lh[:, s * 8 + k, :], wb[:, k, nb * 512:(nb + 1) * 512],
                                 k == 0, k == KC - 1, reads=["M_lh", ("M_wb", k)], writes=[("ps", bank)])
                        P.add("dve", lambda e, nb=nb, bank=bank: e.tensor_tensor(
                            m[:, nb * 512:(nb + 1) * 512], cx.PS[bank][:], bb[:, nb * 512:(nb + 1) * 512], op=ALU.add),
                            reads=["M_bb"], writes=[("ps", bank), "M_m"])
                    g_a = gb[:, 2 * half, :]
                    g_g = gb[:, 2 * half + 1, :]
                    P.add("dve", lambda e, g_a=g_a: e.scalar_tensor_tensor(o[:, 0, :], m[:, D:2 * D], 1.0, g_a,
                                                                          op0=ALU.add, op1=ALU.mult),
                          reads=["M_m", "M_gb"], writes=["M_o"])
                    P.add("pool", lambda e: e.tensor_copy(o[:, 1, :], m[:, 0:D]), reads=["M_m", "M_o"], writes=["M_o"])
                    P.add("pool", lambda e, g_g=g_g: e.tensor_tensor(o[:, 2, :], m[:, 2 * D:3 * D], g_g, op=ALU.mult),
                          reads=["M_m", "M_gb", "M_o"], writes=["M_o"])
                    P.dma("sp", cx.modb[l, s, 3 * half:3 * half + 3].rearrange("i p n -> p i n"), o[:],
                          reads=["M_o"], writes=[("modb", l, s)])


MOD_A, MOD_B, MOD_G = 0, 1, 2


def phase_ffn(cx, layer, w_in, w_out, router, n_exp, src, dst, with_ctx, tag):
    P = cx.P
    SBT = 12
    FG = [4, 4, 4, 4, 4, 2]
    tiles = tok_tiles(cx, with_ctx)
    with Phase(cx, tag) as ph:
        pp = PrePost(cx, ph, tag)
        mods = {}
        for s in ([0, 1] if with_ctx else [0]):
            mods[s] = load_mod(cx, ph, layer, s, tag)
        hT = ph.sb([128, KC, SBT * 128], BF16, "hT")
        y = ph.sb([128, SBT, D], F32, "y")
        wi = [ph.sb([128, KC, 2, 512], BF16, "wi") for _ in range(2)]
        wo = [ph.sb([128, 4, D], BF16, "wo") for _ in range(2)]
        u = [ph.sb([128, 4, 512], BF16, "u") for _ in range(2)]
        sa = [ph.sb([128, 512], F32, "sa") for _ in range(2)]
        gates = ph.sb([128, SBT, NE], F32, "gates")
        rt = ph.sb([128, 64], F32, "rt")
        if router is not None:
            rw = ph.sb([128, KC, NE], BF16, "rw")
            P.dma("pool", rw[:], router.rearrange("(k p) n -> p k n", p=128), writes=[(tag, "rw")])
        piece = 0
        nblk_total = 0
        for sb0 in range(0, len(tiles), SBT):
            sbt = tiles[sb0:sb0 + SBT]
            n = len(sbt)
            for j, (s, ti) in enumerate(sbt):
                modt, modr = mods[s]
                sap, sres = src[s]
                pp.pre(sap[ti * 128:(ti + 1) * 128, :], (sres, ti), modt[:, 3 + MOD_A, :], modt[:, 3 + MOD_B, :], modr,
                       hT[:, :, j * 128:(j + 1) * 128], (tag, "hT", j), bank=j % 2)
                P.add("pool", lambda e, j=j: e.memset(y[:, j, :], 0.0), writes=[(tag, "y", j)])
                if router is not None:
                    bank = 2 + j % 2
                    for k in range(KC):
                        P.mm(cx.PS[bank][:, 0:NE], hT[:, k, j * 128:(j + 1) * 128], rw[:, k, :], k == 0, k == KC - 1,
                             reads=[(tag, "hT", j), (tag, "rw")], writes=[("ps", bank)])
                    lg = rt[:, 0:8]
                    mx = rt[:, 8:16]
                    sc = rt[:, 16:24]
                    g1 = rt[:, 24:32]
                    g2 = rt[:, 32:40]
                    R = (tag, "rt")
                    P.add("dve", lambda e, bank=bank: e.tensor_copy(lg, cx.PS[bank][:, 0:NE]), reads=[R],
                          writes=[("ps", bank), R])
                    P.add("dve", lambda e: e.max(out=mx, in_=lg), reads=[R], writes=[R])
                    P.add("dve", lambda e: e.tensor_tensor(sc[:, 0:1], mx[:, 1:2], mx[:, 0:1], op=ALU.subtract),
                          reads=[R], writes=[R])
                    P.add("act", lambda e: e.activation(sc[:, 1:2], sc[:, 0:1], AF.Exp), reads=[R], writes=[R])
                    P.add("dve", lambda e: e.tensor_scalar(sc[:, 2:3], sc[:, 1:2], 1.0, None, op0=ALU.add),
                          reads=[R], writes=[R])
                    P.add("dve", lambda e: e.reciprocal(sc[:, 3:4], sc[:, 2:3]), reads=[R], writes=[R])
                    P.add("dve", lambda e: e.tensor_tensor(sc[:, 4:5], sc[:, 1:2], sc[:, 3:4], op=ALU.mult),
                          reads=[R], writes=[R])
                    P.add("dve", lambda e: e.tensor_scalar(g1, lg, mx[:, 0:1], sc[:, 3:4], op0=ALU.is_equal,
                                                           op1=ALU.mult), reads=[R], writes=[R])
                    P.add("dve", lambda e: e.tensor_scalar(g2, lg, mx[:, 1:2], sc[:, 4:5], op0=ALU.is_equal,
                                                           op1=ALU.mult), reads=[R], writes=[R])
                    P.add("dve", lambda e, j=j: e.tensor_tensor(gates[:, j, :], g1, g2, op=ALU.add), reads=[R],
                          writes=[R, (tag, "gates", j)])
            blocks = [(b0, min(4, n - b0)) for b0 in range(0, n, 4)]
            for ex in range(n_exp):
                f0 = 0
                for gsz in FG:
                    pb = piece % 2
                    piece += 1
                    wit, wot = wi[pb], wo[pb]
                    Rwi, Rwo = (tag, "wi", pb), (tag, "wo", pb)
                    for ab in range(2):
                        P.dma("pool", wit[:, :, ab, 0:gsz * 128],
                              w_in[ex, :, ab * FFN + f0 * 128: ab * FFN + (f0 + gsz) * 128]
                              .rearrange("(k p) n -> p k n", p=128), writes=[Rwi])
                    P.dma("pool", wot[:, 0:gsz, :],
                          w_out[ex, f0 * 128:(f0 + gsz) * 128, :].rearrange("(c p) n -> p c n", p=128), writes=[Rwo])
                    for (b0, bn) in blocks:
                        W = bn * 128
                        ub = nblk_total % 2
                        nblk_total += 1
                        ut = u[ub]
                        for c in range(gsz):
                            pa, pbk = (0, 1) if c % 2 == 0 else (2, 3)
                            for ab, bank in ((0, pa), (1, pbk)):
                                for k in range(KC):
                                    P.mm(cx.PS[bank][:, 0:W], wit[:, k, ab, c * 128:(c + 1) * 128],
                                         hT[:, k, b0 * 128:b0 * 128 + W], k == 0, k == KC - 1,
                                         reads=[Rwi] + [(tag, "hT", b0 + q) for q in range(bn)], writes=[("ps", bank)])
                            sat = sa[c % 2]
                            P.add("act", lambda e, sat=sat, pa=pa, W=W: e.activation(sat[:, 0:W], cx.PS[pa][:, 0:W], AF.Silu),
                                  writes=[("ps", pa), (tag, "sa", c % 2)])
                            P.add("dve", lambda e, sat=sat, pbk=pbk, W=W, ut=ut, c=c: e.tensor_tensor(
                                ut[:, c, 0:W], sat[:, 0:W], cx.PS[pbk][:, 0:W], op=ALU.mult),
                                reads=[(tag, "sa", c % 2)], writes=[("ps", pbk), (tag, "u", ub, c)])
                        oi = 0
                        for q in range(bn):
                            j = b0 + q
                            for mh in range(2):
                                bank = 4 + oi % 4
                                oi += 1
                                for c in range(gsz):
                                    P.mm(cx.PS[bank][:], ut[:, c, q * 128:(q + 1) * 128], wot[:, c, mh * 512:(mh + 1) * 512],
                                         c == 0, c == gsz - 1, reads=[Rwo, (tag, "u", ub, c)], writes=[("ps", bank)])
                                gsc = gates[:, j, ex:ex + 1] if router is not None else 1.0
                                P.add("dve", lambda e, bank=bank, j=j, mh=mh, gsc=gsc: e.scalar_tensor_tensor(
                                    y[:, j, mh * 512:(mh + 1) * 512], cx.PS[bank][:], gsc, y[:, j, mh * 512:(mh + 1) * 512],
                                    op0=ALU.mult, op1=ALU.add),
                                    reads=[(tag, "gates", j)], writes=[("ps", bank), (tag, "y", j)])
                    f0 += gsz
            for j, (s, ti) in enumerate(sbt):
                modt, modr = mods[s]
                sap, sres = src[s]
                dap, dres = dst[s]
                pp.post(y[:, j, :], (tag, "y", j), sap[ti * 128:(ti + 1) * 128, :], (sres, ti), modt[:, 3 + MOD_G, :], modr,
                        dap[ti * 128:(ti + 1) * 128, :], (dres, ti))


MLA_H = 8
MLA_QR = 384
MLA_KVR = 256
MLA_SCALE = (128 + 64) ** -0.5


def rope_tok(P, eng, out, x1, x2, cos, sin, tmp_a, tmp_b, R):
    o1, o2 = out
    P.add(eng, lambda e: e.tensor_tensor(tmp_a, x1, cos, op=ALU.mult), reads=R, writes=R)
    P.add(eng, lambda e: e.tensor_tensor(tmp_b, x2, sin, op=ALU.mult), reads=R, writes=R)
    P.add(eng, lambda e: e.tensor_tensor(o1, tmp_a, tmp_b, op=ALU.subtract), reads=R, writes=R)
    P.add(eng, lambda e: e.tensor_tensor(tmp_a, x1, sin, op=ALU.mult), reads=R, writes=R)
    P.add(eng, lambda e: e.tensor_tensor(tmp_b, x2, cos, op=ALU.mult), reads=R, writes=R)
    P.add(eng, lambda e: e.tensor_tensor(o2, tmp_a, tmp_b, op=ALU.add), reads=R, writes=R)


def phase_mla(cx, layer, src, dst):
    P = cx.P
    T, TA, NT, NA = cx.T, cx.TA, cx.NT, cx.NA
    tag = "A"
    tiles = tok_tiles(cx, True)
    with Phase(cx, "A") as ph:
        qnT = ph.sb([128, 3, T], BF16, "qnT")
        ckvT = ph.sb([128, 2, TA], BF16, "ckvT")
        krT = ph.sb([128, TA], BF16, "krT")
        qrT = ph.sb([128, 4, T], BF16, "qrT")
        with Phase(cx, "A1") as p1:
            pp = PrePost(cx, p1, "A1")
            mods = {s: load_mod(cx, p1, layer, s, "A1") for s in (0, 1)}
            wd = p1.sb([128, KC, 704], BF16, "wd")
            for (c0_, c1_) in ((0, 512), (512, 704)):
                P.dma("pool", wd[:, :, c0_:c1_], cx.mla_down_w[0][:, c0_:c1_].rearrange("(k p) n -> p k n", p=128), writes=["A_wd"])
            wqr = p1.sb([128, 3, 8, 64], BF16, "wqr")
            for k in range(3):
                P.dma("pool", wqr[:, k], cx.mla_uq_w[0, k * 128:(k + 1) * 128, :].rearrange("p (h d) -> p h d", d=192)[:, :, 128:192],
                      writes=["A_wqr"])
            gq = p1.sb([128, 640], F32, "gq")
            P.dma("sp", gq[:, 0:384], bcast_row(cx.mla_q_norm_g[0:1, :], 128), writes=["A_gq"])
            P.dma("sp", gq[:, 384:640], bcast_row(cx.mla_kv_norm_g[0:1, :], 128), writes=["A_gq"])
            hT = [p1.sb([128, KC, 128], BF16, "hT") for _ in range(2)]
            dn2 = [p1.sb([128, 704], F32, "dn") for _ in range(2)]
            nb2 = [p1.sb([128, 768], BF16, "nb") for _ in range(2)]
            st2 = [p1.sb([128, 8], F32, "st") for _ in range(2)]
            cs = [p1.sb([128, 64], F32, "cs") for _ in range(2)]
            tmp2 = [p1.sb([128, 2, 256], F32, "tmp") for _ in range(2)]
            qr322 = [p1.sb([128, 8, 64], F32, "qr32") for _ in range(2)]
            qrb2 = [p1.sb([128, 8, 64], BF16, "qrb") for _ in range(2)]
            sqj = pp.sq

            def a1_tile(idx, s, ti, par, hTt, dn, nb, st, tmp, qr32, qrb, cst):
                ta = idx
                modt, modr = mods[s]
                sap, sres = src[s]
                Rh = ("A_hT", par)
                Rdn, Rst, Rst2, Rst2b, Rst3, Rnb = ("A_dn", par), ("A_st", par), ("A_st2", par), ("A_st2b", par), ("A_st3", par), ("A_nb", par)
                Rcs, Rtmp, Rqr32, Rqrb = ("A_cs", par), ("A_tmp", par), ("A_qr32", par), ("A_qrb", par)
                pp.pre(sap[ti * 128:(ti + 1) * 128, :], (sres, ti), modt[:, MOD_A, :], modt[:, MOD_B, :], modr,
                       hTt[:], Rh, bank=par)
                for (bank, c0, c1) in ((2, 0, 512), (3, 512, 704)):
                    for k in range(KC):
                        P.mm(cx.PS[bank][:, 0:c1 - c0], hTt[:, k, :], wd[:, k, c0:c1], k == 0, k == KC - 1,
                             reads=[Rh, "A_wd"], writes=[("ps", bank)])
                    P.add("act", lambda e, bank=bank, c0=c0, c1=c1: e.activation(dn[:, c0:c1], cx.PS[bank][:, 0:c1 - c0], AF.Copy),
                          writes=[("ps", bank), Rdn])
                P.add("act", lambda e: e.activation(sqj[:, 0:384], dn[:, 0:384], AF.Square, accum_out=st[:, 0:1]),
                      reads=[Rdn], writes=["A_sq", Rst])
                P.add("act", lambda e: e.activation(sqj[:, 384:640], dn[:, 384:640], AF.Square, accum_out=st[:, 1:2]),
                      reads=[Rdn], writes=["A_sq", Rst])
                P.add("dve", lambda e: e.tensor_scalar(st[:, 2:3], st[:, 0:1], 1.0 / 384, EPS, op0=ALU.mult, op1=ALU.add),
                      reads=[Rst], writes=[Rst2])
                P.add("dve", lambda e: e.tensor_scalar(st[:, 3:4], st[:, 1:2], 1.0 / 256, EPS, op0=ALU.mult, op1=ALU.add),
                      reads=[Rst], writes=[Rst2])
                P.add("act", lambda e: e.activation(st[:, 6:8], st[:, 2:4], AF.Sqrt), reads=[Rst2], writes=[Rst2b])
                P.add("dve", lambda e: e.reciprocal(st[:, 4:6], st[:, 6:8]), reads=[Rst2b], writes=[Rst3])
                if s == 0:
                    P.add("dve", lambda e: e.scalar_tensor_tensor(nb[:, 0:384], dn[:, 0:384], st[:, 4:5], gq[:, 0:384],
                                                                  op0=ALU.mult, op1=ALU.mult),
                          reads=[Rdn, Rst3, "A_gq"], writes=[Rnb])
                P.add("dve", lambda e: e.scalar_tensor_tensor(nb[:, 384:640], dn[:, 384:640], st[:, 5:6], gq[:, 384:640],
                                                              op0=ALU.mult, op1=ALU.mult),
                      reads=[Rdn, Rst3, "A_gq"], writes=[Rnb])
                if s == 0:
                    P.dma("sp", cst[:], cx.rope_mla[ti * 128:(ti + 1) * 128, :], writes=[Rcs])
                    RR = [Rdn, Rcs, Rtmp, Rnb]
                    rope_tok(P, "dve", (nb[:, 640:672], nb[:, 672:704]), dn[:, 640:672], dn[:, 672:704],
                             cst[:, 0:32], cst[:, 32:64], tmp[:, 0, 0:32], tmp[:, 1, 0:32], RR)
                else:
                    P.add("dve", lambda e: e.tensor_copy(nb[:, 640:704], dn[:, 640:704]), reads=[Rdn], writes=[Rnb])
                P.add("dve", lambda e: e.tensor_copy(nb[:, 704:768], nb[:, 640:704]), reads=[Rnb], writes=[Rnb])
                bank = 4 + par
                psb = cx.PSB[bank]
                tblocks = ([0, 1, 2] if s == 0 else []) + [3, 4, 5]
                for c in tblocks:
                    P.tr(psb[:, c * 128:(c + 1) * 128], nb[:, c * 128:(c + 1) * 128], cx.ident[:], reads=[Rnb],
                         writes=[("ps", bank)])
                if s == 0:
                    P.add("act", lambda e: e.activation(
                        qnT[:, :, ti * 128:(ti + 1) * 128], psb[:, 0:384].rearrange("p (k n) -> p k n", k=3), AF.Copy),
                        writes=[("ps", bank), ("A_qnT", ti)])
                P.add("dve", lambda e: e.tensor_copy(
                    ckvT[:, :, ta * 128:(ta + 1) * 128], psb[:, 384:640].rearrange("p (k n) -> p k n", k=2)),
                    writes=[("ps", bank), ("A_ckvT", ta)])
                P.add("dve", lambda e: e.tensor_copy(krT[:, ta * 128:(ta + 1) * 128], psb[:, 640:768]),
                      writes=[("ps", bank), ("A_krT", ta)])
                if s == 0:
                    bank6 = 6
                    for k in range(3):
                        P.mm(cx.PS[bank6][:], qnT[:, k, ti * 128:(ti + 1) * 128], wqr[:, k].rearrange("p h d -> p (h d)"),
                             k == 0, k == 2, reads=[("A_qnT", ti), "A_wqr"], writes=[("ps", bank6)])
                    P.add("act", lambda e: e.activation(qr32[:].rearrange("p h d -> p (h d)"), cx.PS[bank6][:], AF.Copy),
                          writes=[("ps", bank6), Rqr32])
                    cosb = cst[:, 0:32].unsqueeze(1).to_broadcast([128, 8, 32])
                    sinb = cst[:, 32:64].unsqueeze(1).to_broadcast([128, 8, 32])
                    ta_ = tmp[:, 0, :].rearrange("p (h d) -> p h d", h=8)
                    tb_ = tmp[:, 1, :].rearrange("p (h d) -> p h d", h=8)
                    RR = [Rqr32, Rcs, Rtmp, Rqrb]
                    rope_tok(P, "dve", (qrb[:, :, 0:32], qrb[:, :, 32:64]), qr32[:, :, 0:32], qr32[:, :, 32:64],
                             cosb, sinb, ta_, tb_, RR)
                    bank7 = 7
                    psb7 = cx.PSB[bank7]
                    qrf = qrb[:].rearrange("p h d -> p (h d)")
                    for c in range(4):
                        P.tr(psb7[:, c * 128:(c + 1) * 128], qrf[:, c * 128:(c + 1) * 128], cx.ident[:], reads=[Rqrb],
                             writes=[("ps", bank7)])
                    P.add("act", lambda e: e.activation(
                        qrT[:, :, ti * 128:(ti + 1) * 128], psb7[:, 0:512].rearrange("p (k n) -> p k n", k=4), AF.Copy),
                        writes=[("ps", bank7), ("A_qrT", ti)])

            for idx, (s, ti) in enumerate(tiles):
                par = idx % 2
                a1_tile(idx, s, ti, par, hT[par], dn2[par], nb2[par], st2[par], tmp2[par], qr322[par], qrb2[par], cs[par])
        if getattr(cx, "dbg", None):
            P.dma("sp", cx.dbg["qnT"], qnT[:], reads=[("A_qnT", t) for t in range(NT)], writes=["dbg1"])
            P.dma("sp", cx.dbg["ckvT"], ckvT[:], reads=[("A_ckvT", t) for t in range(NA)], writes=["dbg2"])
            P.dma("sp", cx.dbg["krT"], krT[:], reads=[("A_krT", t) for t in range(NA)], writes=["dbg3"])
            P.dma("sp", cx.dbg["qrT"], qrT[:], reads=[("A_qrT", t) for t in range(NT)], writes=["dbg4"])
        with Phase(cx, "A2") as p2:
            wqn = p2.sb([128, 3, 8, 128], BF16, "wqn")
            for k in range(3):
                P.dma("pool", wqn[:, k], cx.mla_uq_w[0, k * 128:(k + 1) * 128, :].rearrange("p (h d) -> p h d", d=192)[:, :, 0:128],
                      writes=["B_wqn"])
            wkv = p2.sb([128, 2, 2048], BF16, "wkv")
            P.dma("pool", wkv[:], cx.mla_ukv_w[0].rearrange("(k p) n -> p k n", p=128), writes=["B_wkv"])
            KhT = [p2.sb([128, TA], BF16, "KhT") for _ in range(2)]
            Vh = [p2.sb([128, NA, 128], BF16, "Vh") for _ in range(2)]
            QhT = [p2.sb([128, T], BF16, "QhT") for _ in range(2)]
            pT = [p2.sb([128, 512], BF16, "pT") for _ in range(3)]
            rden = p2.sb([128, 512], F32, "rden")
            at = [p2.sb([128, 512], BF16, "at") for _ in range(2)]
            accD = [p2.sb([128, 512], F32, "accD") for _ in range(2)]
            accP = [p2.sb([128, 512], F32, "accP") for _ in range(2)]
            accb = p2.sb([128, 512], BF16, "accb")
            all_ckv = [("A_ckvT", ta) for ta in range(NA)]
            all_qn = [("A_qnT", ti) for ti in range(NT)]

            def proj(h):
                par = h % 2
                pc = 0
                for kb0 in range(0, TA, 512):
                    W = min(512, TA - kb0)
                    bank = 7
                    for k in range(2):
                        P.mm(cx.PS[bank][:, 0:W], wkv[:, k, h * 256:h * 256 + 128], ckvT[:, k, kb0:kb0 + W], k == 0, k == 1,
                             reads=["B_wkv"] + all_ckv, writes=[("ps", bank)])
                    P.add("dve", lambda e, par=par, kb0=kb0, W=W, bank=bank: e.tensor_copy(KhT[par][:, kb0:kb0 + W], cx.PS[bank][:, 0:W]),
                          writes=[("ps", bank), ("B_K", par)])
                for t0 in range(0, NA, 4):
                    tn = min(4, NA - t0)
                    bank = 7
                    for q in range(tn):
                        for k in range(2):
                            P.mm(cx.PS[bank][:, q * 128:(q + 1) * 128], ckvT[:, k, (t0 + q) * 128:(t0 + q + 1) * 128],
                                 wkv[:, k, h * 256 + 128:h * 256 + 256], k == 0, k == 1,
                                 reads=["B_wkv"] + all_ckv, writes=[("ps", bank)])
                    P.add("dve", lambda e, par=par, t0=t0, tn=tn, bank=bank: e.tensor_copy(
                        Vh[par][:, t0:t0 + tn, :], cx.PS[bank][:, 0:tn * 128].rearrange("p (t d) -> p t d", d=128)),
                        writes=[("ps", bank), ("B_V", par)])
                for qb0 in range(0, T, 512):
                    bank = 7
                    for k in range(3):
                        P.mm(cx.PS[bank][:], wqn[:, k, h, :], qnT[:, k, qb0:qb0 + 512], k == 0, k == 2,
                             reads=["B_wqn"] + all_qn, writes=[("ps", bank)])
                    P.add("dve", lambda e, par=par, qb0=qb0, bank=bank: e.tensor_copy(QhT[par][:, qb0:qb0 + 512], cx.PS[bank][:]),
                          writes=[("ps", bank), ("B_Q", par)])

            NQ = T // 512
            steps = [(h, qb, kt) for h in range(MLA_H) for qb in range(NQ) for kt in range(NA)]
            NS = len(steps)

            def emit_S(i):
                h, qb, kt = steps[i]
                par = h % 2
                hp = h % 2
                qb0 = qb * 512
                sbk = i % 3
                P.mm(cx.PS[sbk][:], KhT[par][:, kt * 128:(kt + 1) * 128], QhT[par][:, qb0:qb0 + 512], True, False,
                     reads=[("B_K", par), ("B_Q", par)], writes=[("ps", sbk)])
                P.mm(cx.PS[sbk][:], krT[hp * 64:(hp + 1) * 64, kt * 128:(kt + 1) * 128],
                     qrT[hp * 64:(hp + 1) * 64, h // 2, qb0:qb0 + 512], False, True,
                     reads=[("A_krT", kt)] + [("A_qrT", (qb0 // 128) + q) for q in range(4)], writes=[("ps", sbk)])
                pt = pT[sbk]
                P.add("act", lambda e, pt=pt, sbk=sbk: e.activation(pt[:], cx.PS[sbk][:], AF.Exp, scale=MLA_SCALE),
                      writes=[("ps", sbk), ("B_pT", sbk)])

            def emit_PV(i):
                h, qb, kt = steps[i]
                par = h % 2
                qb0 = qb * 512
                sbk = i % 3
                blk = i // NA
                ob = 3 + 2 * (blk % 2)
                db = ob + 1
                pt = pT[sbk]
                P.mm(cx.PS[ob][:, :], Vh[par][:, kt, :], pt[:], kt == 0, kt == NA - 1,
                     reads=[("B_V", par), ("B_pT", sbk)], writes=[("ps", ob)])
                P.mm(cx.PS[db][:, :], cx.ones[:], pt[:], kt == 0, kt == NA - 1,
                     reads=[("B_pT", sbk)], writes=[("ps", db)])
                if kt == NA - 1:
                    P.add("dve", lambda e, db=db: e.reciprocal(rden[:], cx.PS[db][:]), writes=[("ps", db), "B_rden"])
                    att = at[blk % 2]
                    Rat = ("B_at", blk % 2)
                    P.add("dve", lambda e, att=att, ob=ob: e.tensor_tensor(att[:], cx.PS[ob][:], rden[:], op=ALU.mult),
                          reads=["B_rden"], writes=[("ps", ob), Rat])
                    P.dma("sp", cx.attnT[h, :, qb0:qb0 + 512], att[:], reads=[Rat], writes=[("attnT", qb0 // 512)])

            proj(0)
            proj(1)
            emit_S(0)
            emit_S(1)
            for i in range(NS):
                h, qb, kt = steps[i]
                if qb == 0 and kt == 0 and h >= 1 and h + 1 < MLA_H:
                    proj(h + 1)
                if i + 2 < NS:
                    emit_S(i + 2)
                emit_PV(i)
    with Phase(cx, "A3") as p3:
        pp = PrePost(cx, p3, "A3")
        modt, modr = load_mod(cx, p3, layer, 0, "A3")
        wo = p3.sb([128, 8, D], BF16, "wo")
        P.dma("pool", wo[:], cx.mla_out_w[0].rearrange("(h p) n -> p h n", p=128), writes=["C_wo"])
        a_in = [p3.sb([128, 8, 128], BF16, "a_in") for _ in range(2)]
        o32 = [p3.sb([128, D], F32, "o32") for _ in range(2)]
        sap, sres = src[0]
        dap, dres = dst[0]
        for ti in range(NT):
            ab = ti % 2
            P.dma("sp", a_in[ab][:], cx.attnT[:, :, ti * 128:(ti + 1) * 128].rearrange("h p n -> p h n"),
                  reads=[("attnT", ti // 4)], writes=[("C_a", ab)])
            for mh in range(2):
                bank = 2 * ab + mh
                for h in range(8):
                    P.mm(cx.PS[bank][:], a_in[ab][:, h, :], wo[:, h, mh * 512:(mh + 1) * 512], h == 0, h == 7,
                         reads=[("C_a", ab), "C_wo"], writes=[("ps", bank)])
                P.add("act", lambda e, ab=ab, mh=mh, bank=bank: e.activation(o32[ab][:, mh * 512:(mh + 1) * 512], cx.PS[bank][:], AF.Copy),
                      writes=[("ps", bank), ("C_o", ab)])
            pp.post(o32[ab][:], ("C_o", ab), sap[ti * 128:(ti + 1) * 128, :], (sres, ti), modt[:, MOD_G, :], modr,
                    dap[ti * 128:(ti + 1) * 128, :], (dres, ti))


CH = 64
C_MHF, C_MHB, C_MSK, C_END = 0, 128, 256, 768


def phase_mix0(cx, src, dst):
    P = cx.P
    T, TA, NT, NA = cx.T, cx.TA, cx.NT, cx.NA
    NC = TA // CH
    tiles = tok_tiles(cx, True)
    blocks = [(0, CTX)] + [(CTX + b, 512) for b in range(0, T, 512)]
    with Phase(cx, "X1") as p1:
        pp = PrePost(cx, p1, "X1")
        mods = {s: load_mod(cx, p1, 0, s, "X1") for s in (0, 1)}
        hTt = [p1.sb([128, KC, 128], BF16, "hTt") for _ in range(2)]
        for idx, (s, ti) in enumerate(tiles):
            modt, modr = mods[s]
            sap, sres = src[s]
            R = ("X1_hT", idx % 2)
            pp.pre(sap[ti * 128:(ti + 1) * 128, :], (sres, ti), modt[:, MOD_A, :], modt[:, MOD_B, :], modr,
                   hTt[idx % 2][:], R, bank=idx % 2)
            P.dma("sp", cx.hTd[:, :, idx * 128:(idx + 1) * 128].rearrange("k p n -> p k n"), hTt[idx % 2][:],
                  reads=[R], writes=[("hTd", idx // 4)])
    with Phase(cx, "X2") as ph:
        cst = ph.sb([128, C_END], F32, "cst")
        P.dma("sp", cst[:], cx.cst, writes=["X_cst"])
        lgt = ph.sb([128, 8], F32, "lgt")
        P.dma("sp", lgt[:], bcast_row(cx.ret_decay_logit[0:1].rearrange("o a b -> o (a b)"), 128), writes=["X_lg"])
        P.add("act", lambda e: e.activation(lgt[:], lgt[:], AF.Sigmoid), reads=["X_lg"], writes=["X_lg"])
        P.add("act", lambda e: e.activation(lgt[:], lgt[:], AF.Ln), reads=["X_lg"], writes=["X_lg"])
        lb = ph.sb([128, 3, 4], F32, "lb")
        P.dma("sp", lb[:, 0:2, :], cx.hgrn_lbl, writes=["X_lb"])
        P.add("dve", lambda e: e.tensor_tensor(lb[:, 2, :], lb[:, 0, :], lb[:, 1, :], op=ALU.subtract), reads=["X_lb"],
              writes=["X_lb2"])
        P.add("act", lambda e: e.activation(lb[:, 0, :], lb[:, 2, :], AF.Sigmoid), reads=["X_lb2"], writes=["X_lb3"])
        P.add("dve", lambda e: e.tensor_scalar(lb[:, 1, :], lb[:, 0, :], -1.0, 1.0, op0=ALU.mult, op1=ALU.add),
              reads=["X_lb3"], writes=["X_lb4"])
        LB = ["X_lb3", "X_lb4"]
        hTb = [ph.sb([128, KC, 512], BF16, "hTb") for _ in range(2)]
        wh = ph.sb([128, KC, 5, 128], BF16, "wh")
        qs = ph.sb([128, TA], BF16, "qs")
        gs = ph.sb([128, TA], BF16, "gs")
        kkr = ph.sb([128, TA], BF16, "kkr")
        v_h = ph.sb([128, NA, 128], BF16, "v_h")
        o_acc = ph.sb([128, TA], F32, "o_acc")
        q1 = ph.sb([128, TA], BF16, "q1")
        k1 = ph.sb([128, TA], BF16, "k1")
        k1t = ph.sb([128, NA, 128], BF16, "k1t")
        S_all = ph.sb([128, NC, 128], BF16, "S_all")
        S32 = [ph.sb([128, 128], F32, "S32") for _ in range(2)]
        er = ph.sb([128, NC], F32, "er")
        etad = ph.sb([128, NC], F32, "etad")
        ek = ph.sb([128, NC], F32, "ek")
        F = [ph.sb([128, 512], F32, "F%d" % i) for i in range(8)]
        raw = [ph.sb([128, 512], BF16, "raw%d" % i) for i in range(2)]
        csb = [ph.sb([128, 2, 512], F32, "csb") for _ in range(2)]
        am = [ph.sb([128, 128], BF16, "am") for _ in range(2)]
        kvt = [ph.sb([128, 128], F32, "kvt") for _ in range(4)]
        gconst = ph.sb([128, 512], F32, "gconst")
        sqb = ph.sb([128, 512], BF16, "sqb")
        yT = [ph.sb([128, 512], BF16, "yT") for _ in range(2)]
        nld = [0]

        def load_h(bi):
            b0, W = blocks[bi]
            i = nld[0] % 2
            nld[0] += 1
            R = ("X_hTb", i)
            P.dma("sp", hTb[i][:, :, 0:W], cx.hTd[:, :, b0:b0 + W].rearrange("k p n -> p k n"),
                  reads=[("hTd", q) for q in range(b0 // 512, (b0 + W + 511) // 512)], writes=[R])
            return hTb[i], R

        def proj_fm(ht, R, W, widx, bank):
            for k in range(KC):
                P.mm(cx.PS[bank][:, 0:W], wh[:, k, widx, :], ht[:, k, 0:W], k == 0, k == KC - 1, reads=[R, "X_wh"],
                     writes=[("ps", bank)])

        for hd in range(8):
            is_ret = hd < 4
            h = hd % 4
            cols = ([0, 512, 1536, 1024] if is_ret else [2048, 2560, 3072, 4096, 3584])
            for wi_, c0 in enumerate(cols):
                P.dma("pool", wh[:, :, wi_, :], cx.mix_in_w[0][:, c0 + h * 128:c0 + (h + 1) * 128]
                      .rearrange("(k p) n -> p k n", p=128), writes=["X_wh"])
            VI = 3 if is_ret else 4
            GI = 2 if is_ret else 3
            for bi, (b0, W) in enumerate(blocks):
                ht, R = load_h(bi)
                sl = slice(b0, b0 + W)
                lat = b0 >= CTX
                proj_fm(ht, R, W, 0, 0)
                if is_ret:
                    proj_fm(ht, R, W, 1, 1)
                proj_fm(ht, R, W, GI, 2)
                P.add("act", lambda e, sl=sl, W=W: e.activation(gs[:, sl], cx.PS[2][:, 0:W], AF.Silu),
                      writes=[("ps", 2), "X_gs"])
                if not is_ret:
                    P.add("act", lambda e, sl=sl, W=W: e.activation(qs[:, sl], cx.PS[0][:, 0:W], AF.Silu),
                          writes=[("ps", 0), "X_qs"])
                else:
                    for (bank, dstt, scl, Rd) in ((0, qs, 1.0, "X_qs"), (1, kkr, 128 ** -0.5, "X_kk")):
                        if not lat:
                            P.add("act", lambda e, bank=bank, dstt=dstt, scl=scl, sl=sl, W=W: e.activation(
                                dstt[:, sl], cx.PS[bank][:, 0:W], AF.Copy, scale=scl), writes=[("ps", bank), Rd])
                            continue
                        rw_ = raw[bank]
                        Rr = ("X_raw", bank)
                        P.add("act", lambda e, bank=bank, rw_=rw_, scl=scl, W=W: e.activation(
                            rw_[:, 0:W], cx.PS[bank][:, 0:W], AF.Copy, scale=scl), writes=[("ps", bank), Rr])
                        ci = (bi + bank) % 2
                        Rc = ("X_cs", ci)
                        if bank == 0:
                            P.dma("sp", csb[ci][:, :, 0:W], cx.rope_ret[:, :, b0 - CTX:b0 - CTX + W].rearrange("c p n -> p c n"),
                                  writes=[Rc])
                        else:
                            ci = bi % 2
                            Rc = ("X_cs", ci)
                        sb_ = 3
                        P.mm(cx.PS[sb_][:, 0:W], cx.perm[:], rw_[:, 0:W], True, True, reads=[Rr], writes=[("ps", sb_)])
                        P.add("pool", lambda e, rw_=rw_, ci=ci, W=W: e.tensor_tensor(F[0][:, 0:W], rw_[:, 0:W], csb[ci][:, 0, 0:W], op=ALU.mult),
                              reads=[Rr, Rc], writes=["X_F0"])
                        P.add("dve", lambda e, ci=ci, W=W, sb_=sb_: e.tensor_tensor(F[1][:, 0:W], cx.PS[sb_][:, 0:W], csb[ci][:, 1, 0:W], op=ALU.mult),
                              reads=[Rc], writes=[("ps", sb_), "X_F1"])
                        P.add("pool", lambda e, dstt=dstt, sl=sl, W=W: e.tensor_tensor(dstt[:, sl], F[0][:, 0:W], F[1][:, 0:W], op=ALU.add),
                              reads=["X_F0", "X_F1"], writes=[Rd])
                for q in range(W // 128):
                    ta = b0 // 128 + q
                    bank = 4 + ta % 2
                    for k in range(KC):
                        P.mm(cx.PS[bank][:, 0:128], ht[:, k, q * 128:(q + 1) * 128], wh[:, k, VI, :], k == 0, k == KC - 1,
                             reads=[R, "X_wh"], writes=[("ps", bank)])
                    P.add("dve", lambda e, ta=ta, bank=bank: e.tensor_copy(v_h[:, ta, :], cx.PS[bank][:, 0:128]),
                          writes=[("ps", bank), "X_v"])
            for dr in range(2):
                mh = cst[:, C_MHF:C_MHF + 128] if dr == 0 else cst[:, C_MHB:C_MHB + 128]
                if is_ret:
                    col = dr * 4 + h
                    P.add("dve", lambda e, col=col: e.tensor_copy(gconst[:], lgt[:, col:col + 1].to_broadcast([128, 512])),
                          reads=["X_lg"], writes=["X_gc"])
                def pipe(W, c0, g_ap, Rg):
                    nch = W // CH
                    v3 = lambda t, W=W: t[:, 0:W].rearrange("p (c n) -> p c n", n=CH)
                    P.add("dve", lambda e, W=W, g_ap=g_ap: e.tensor_tensor_scan(F[2][:, 0:W], cst[:, C_MSK:C_MSK + W], g_ap[:, 0:W], 0.0,
                                                                          op0=ALU.mult, op1=ALU.add),
                          reads=[Rg, "X_cst"], writes=["X_F2"])
                    tot = v3(F[2])[:, :, CH - 1:CH]
                    if dr == 0:
                        b_t = F[2]
                        Rb = "X_F2"
                    else:
                        P.add("dve", lambda e, W=W, g_ap=g_ap: e.tensor_tensor(F[3][:, 0:W], g_ap[:, 0:W], F[2][:, 0:W], op=ALU.subtract),
                              reads=[Rg, "X_F2"], writes=["X_F3"])
                        P.add("pool", lambda e, v3=v3, tot=tot, nch=nch: e.tensor_tensor(v3(F[3]), v3(F[3]), tot.to_broadcast([128, nch, CH]), op=ALU.add),
                              reads=["X_F2", "X_F3"], writes=["X_F3"])
                        b_t = F[3]
                        Rb = "X_F3"
                    rr = v3(b_t)[:, :, 31:32]
                    P.add("act", lambda e, rr=rr, c0=c0, nch=nch: e.activation(er[:, c0:c0 + nch].unsqueeze(2), rr, AF.Exp),
                          reads=[Rb], writes=["X_er"])
                    P.add("act", lambda e, tot=tot, c0=c0, nch=nch: e.activation(etad[:, c0:c0 + nch].unsqueeze(2), tot, AF.Exp),
                          reads=["X_F2"], writes=["X_etad"])
                    P.add("dve", lambda e, tot=tot, rr=rr, c0=c0, nch=nch: e.tensor_tensor(ek[:, c0:c0 + nch].unsqueeze(2), tot, rr, op=ALU.subtract),
                          reads=["X_F2", Rb], writes=["X_ek"])
                    P.add("act", lambda e, c0=c0, nch=nch: e.activation(ek[:, c0:c0 + nch], ek[:, c0:c0 + nch], AF.Exp),
                          reads=["X_ek"], writes=["X_ek"])
                    P.add("dve", lambda e, v3=v3, b_t=b_t, rr=rr, nch=nch: e.tensor_tensor(v3(F[4]), v3(b_t), rr.to_broadcast([128, nch, CH]), op=ALU.subtract),
                          reads=[Rb], writes=["X_F4"])
                    P.add("act", lambda e, W=W: e.activation(F[5][:, 0:W], F[4][:, 0:W], AF.Exp), reads=["X_F4"], writes=["X_F5"])
                    P.add("act", lambda e, W=W: e.activation(F[6][:, 0:W], F[4][:, 0:W], AF.Exp, scale=-1.0), reads=["X_F4"], writes=["X_F6"])

                if is_ret:
                    pipe(512, 0, gconst, "X_gc")
                    for tl_, Rt in ((er, "X_er"), (etad, "X_etad"), (ek, "X_ek")):
                        P.add("dve", lambda e, tl_=tl_: e.tensor_copy(tl_[:, 8:NC], tl_[:, 0:1].to_broadcast([128, NC - 8])),
                              reads=[Rt], writes=[Rt])
                for bi, (b0, W) in enumerate(blocks):
                    sl = slice(b0, b0 + W)
                    if is_ret:
                        kk_ap = kkr[:, sl]
                        Rkk = "X_kk"
                    else:
                        ht, R = load_h(bi)
                        proj_fm(ht, R, W, 1 + dr, 0)
                        P.add("act", lambda e, W=W: e.activation(F[0][:, 0:W], cx.PS[0][:, 0:W], AF.Sigmoid),
                              writes=[("ps", 0), "X_F0"])
                        P.add("dve", lambda e, W=W, h=h: e.tensor_scalar(F[0][:, 0:W], F[0][:, 0:W], lb[:, 1, h:h + 1], lb[:, 0, h:h + 1],
                                                                       op0=ALU.mult, op1=ALU.add), reads=["X_F0"] + LB, writes=["X_F0"])
                        P.add("act", lambda e, W=W: e.activation(F[1][:, 0:W], F[0][:, 0:W], AF.Ln), reads=["X_F0"], writes=["X_F1"])
                        rk = raw[bi % 2]
                        P.add("dve", lambda e, W=W, rk=rk: e.tensor_scalar(rk[:, 0:W], F[0][:, 0:W], -1.0, 1.0, op0=ALU.mult, op1=ALU.add),
                              reads=["X_F0"], writes=[("X_raw", bi % 2)])
                        kk_ap = rk[:, 0:W]
                        Rkk = ("X_raw", bi % 2)
                        pipe(W, b0 // CH, F[1], "X_F1")
                    P.add("pool", lambda e, sl=sl, W=W: e.tensor_tensor(q1[:, sl], qs[:, sl], F[5][:, 0:W], op=ALU.mult),
                          reads=["X_qs", "X_F5"], writes=["X_q1"])
                    P.add("dve", lambda e, sl=sl, W=W, kk_ap=kk_ap: e.tensor_tensor(k1[:, sl], kk_ap, F[6][:, 0:W], op=ALU.mult),
                          reads=[Rkk, "X_F6"], writes=["X_k1"])
                    bank = 6 + bi % 2
                    nq = W // 128
                    for q in range(nq):
                        P.tr(cx.PSB[bank][:, q * 128:(q + 1) * 128], k1[:, b0 + q * 128:b0 + (q + 1) * 128], cx.ident[:],
                             reads=["X_k1"], writes=[("ps", bank)])
                    P.add("act", lambda e, b0=b0, nq=nq, bank=bank: e.activation(
                        k1t[:, b0 // 128:b0 // 128 + nq, :], cx.PSB[bank][:, 0:nq * 128].rearrange("p (t d) -> p t d", d=128), AF.Copy),
                        writes=[("ps", bank), "X_k1t"])
                if dr == 0:
                    order = list(range(NA))
                else:
                    order = [1, 0] + list(range(NA - 1, 1, -1))
                halves = (0, 1) if dr == 0 else (1, 0)
                P.add("pool", lambda e: e.memset(S32[0][:], 0.0), writes=[("X_S32", 0)])
                n_t = len(order)

                def st_A(i):
                    ta = order[i]
                    for hi, hf in enumerate(halves):
                        k = 2 * i + hi
                        c = ta * 2 + hf
                        kb = k % 2
                        kt_ = kvt[k % 4]
                        Rkt = ("X_kvt", k % 4)
                        P.mm(cx.PS[kb][:, 0:128], k1t[hf * 64:(hf + 1) * 64, ta, :], v_h[hf * 64:(hf + 1) * 64, ta, :], True, True,
                             reads=["X_k1t", "X_v"], writes=[("ps", kb)])
                        P.add("act", lambda e, kt_=kt_, kb=kb, c=c: e.activation(kt_[:], cx.PS[kb][:, 0:128], AF.Copy, scale=ek[:, c:c + 1]),
                              reads=["X_ek"], writes=[("ps", kb), Rkt])
                    ab_ = 2 + i % 2
                    tsl = slice(ta * 128, (ta + 1) * 128)
                    P.mm(cx.PS[ab_][:, 0:128], k1[:, tsl], q1[:, tsl], True, True, reads=["X_k1", "X_q1"], writes=[("ps", ab_)])

                def st_B(i):
                    ta = order[i]
                    for hi, hf in enumerate(halves):
                        k = 2 * i + hi
                        c = ta * 2 + hf
                        src, dst = S32[k % 2], S32[(k + 1) % 2]
                        P.add("act", lambda e, c=c, src=src: e.activation(S_all[:, c, :], src[:], AF.Copy, scale=er[:, c:c + 1]),
                              reads=[("X_S32", k % 2), "X_er"], writes=[("X_Sall", c % 8)])
                        kt_ = kvt[k % 4]
                        P.add("dve", lambda e, kt_=kt_, c=c, src=src, dst=dst: e.scalar_tensor_tensor(
                            dst[:], src[:], etad[:, c:c + 1], kt_[:], op0=ALU.mult, op1=ALU.add),
                            reads=[("X_S32", k % 2), "X_etad", ("X_kvt", k % 4)], writes=[("X_S32", (k + 1) % 2)])
                    ab_ = 2 + i % 2
                    amt = am[i % 2]
                    P.add("dve", lambda e, amt=amt, ab_=ab_, mh=mh: e.tensor_tensor(amt[:], cx.PS[ab_][:, 0:128], mh, op=ALU.mult),
                          reads=["X_cst"], writes=[("ps", ab_), ("X_am", i % 2)])

                def st_C(i):
                    ta = order[i]
                    amt = am[i % 2]
                    ob = 4 + i % 2
                    P.mm(cx.PS[ob][:, 0:128], v_h[:, ta, :], amt[:], True, False, reads=["X_v", ("X_am", i % 2)], writes=[("ps", ob)])
                    for hf in (0, 1):
                        c = ta * 2 + hf
                        P.mm(cx.PS[ob][:, hf * 64:(hf + 1) * 64], S_all[:, c, :], q1[:, ta * 128 + hf * 64:ta * 128 + (hf + 1) * 64],
                             False, hf == 1, reads=[("X_Sall", c % 8), "X_q1"], writes=[("ps", ob)])

                def st_D(i):
                    ta = order[i]
                    ob = 4 + i % 2
                    tsl = slice(ta * 128, (ta + 1) * 128)
                    if dr == 0:
                        P.add("dve", lambda e, tsl=tsl, ob=ob: e.tensor_copy(o_acc[:, tsl], cx.PS[ob][:, 0:128]),
                              writes=[("ps", ob), "X_oacc"])
                    else:
                        P.add("dve", lambda e, tsl=tsl, ob=ob: e.tensor_tensor(o_acc[:, tsl], o_acc[:, tsl], cx.PS[ob][:, 0:128], op=ALU.add),
                              reads=["X_oacc"], writes=[("ps", ob), "X_oacc"])

                for i in range(n_t + 3):
                    if i < n_t:
                        st_A(i)
                    if 0 <= i - 1 < n_t:
                        st_B(i - 1)
                    if 0 <= i - 2 < n_t:
                        st_C(i - 2)
                    if 0 <= i - 3 < n_t:
                        st_D(i - 3)
            for bi, (b0, W) in enumerate(blocks):
                sl = slice(b0, b0 + W)
                P.add("act", lambda e, sl=sl, W=W: e.activation(sqb[:, 0:W], o_acc[:, sl], AF.Square),
                      reads=["X_oacc"], writes=["X_sqb"])
                bank = 7
                P.mm(cx.PS[bank][:, 0:W], cx.ones[:], sqb[:, 0:W], True, True, reads=["X_sqb"], writes=[("ps", bank)])
                P.add("dve", lambda e, W=W, bank=bank: e.tensor_scalar(F[7][:, 0:W], cx.PS[bank][:, 0:W], 1.0 / 128, EPS, op0=ALU.mult, op1=ALU.add),
                      writes=[("ps", bank), "X_F7"])
                P.add("act", lambda e, W=W: e.activation(F[7][:, 0:W], F[7][:, 0:W], AF.Sqrt), reads=["X_F7"], writes=["X_F7"])
                P.add("dve", lambda e, W=W: e.reciprocal(F[7][:, 0:W], F[7][:, 0:W]), reads=["X_F7"], writes=["X_F7"])
                P.add("dve", lambda e, sl=sl, W=W: e.tensor_tensor(F[7][:, 0:W], F[7][:, 0:W], o_acc[:, sl], op=ALU.mult),
                      reads=["X_F7", "X_oacc"], writes=["X_F7"])
                yt = yT[bi % 2]
                Ry = ("X_yT", bi % 2)
                P.add("pool", lambda e, sl=sl, W=W, yt=yt: e.tensor_tensor(yt[:, 0:W], F[7][:, 0:W], gs[:, sl], op=ALU.mult),
                      reads=["X_F7", "X_gs"], writes=[Ry])
                P.dma("sp", cx.yTd[hd, :, b0:b0 + W], yt[:, 0:W], reads=[Ry], writes=[("yTd", b0 // 512)])
    with Phase(cx, "X3") as p3:
        pp = PrePost(cx, p3, "X3")
        mods = {s: load_mod(cx, p3, 0, s, "X3") for s in (0, 1)}
        wo = p3.sb([128, 8, D], BF16, "wo")
        P.dma("pool", wo[:], cx.mix_out_w[0].rearrange("(h p) n -> p h n", p=128), writes=["X3_wo"])
        a_in = [p3.sb([128, 8, 128], BF16, "a_in") for _ in range(2)]
        o32 = [p3.sb([128, D], F32, "o32") for _ in range(2)]
        for idx, (s, ti) in enumerate(tiles):
            ab = idx % 2
            modt, modr = mods[s]
            sap, sres = src[s]
            dap, dres = dst[s]
            P.dma("sp", a_in[ab][:], cx.yTd[:, :, idx * 128:(idx + 1) * 128].rearrange("h p n -> p h n"),
                  reads=[("yTd", q) for q in range(0, (TA + 511) // 512)], writes=[("X3_a", ab)])
            for mh_ in range(2):
                bank = 2 * ab + mh_
                for hh in range(8):
                    P.mm(cx.PS[bank][:], a_in[ab][:, hh, :], wo[:, hh, mh_ * 512:(mh_ + 1) * 512], hh == 0, hh == 7,
                         reads=[("X3_a", ab), "X3_wo"], writes=[("ps", bank)])
                P.add("act", lambda e, ab=ab, mh_=mh_, bank=bank: e.activation(o32[ab][:, mh_ * 512:(mh_ + 1) * 512], cx.PS[bank][:], AF.Copy),
                      writes=[("ps", bank), ("X3_o", ab)])
            pp.post(o32[ab][:], ("X3_o", ab), sap[ti * 128:(ti + 1) * 128, :], (sres, ti), modt[:, MOD_G, :], modr,
                    dap[ti * 128:(ti + 1) * 128, :], (dres, ti))


ALL_PHASES = ("mix0", "ffn0", "mla", "moe")

W_SHAPES = {
    "mod_w": [2, D, 6 * D], "mod_b": [2, 6 * D], "norm_g": [2, 4, D], "mix_in_w": [1, D, 4608],
    "ret_decay_logit": [1, 2, 4], "mix_out_w": [1, D, D], "ffn_in_w": [1, D, 2 * FFN], "ffn_out_w": [1, FFN, D],
    "mla_down_w": [1, D, 704], "mla_q_norm_g": [1, 384], "mla_kv_norm_g": [1, 256], "mla_uq_w": [1, 384, 1536],
    "mla_ukv_w": [1, 256, 2048], "mla_out_w": [1, D, D], "router_w": [1, D, NE], "moe_in_w": [1, NE, D, 2 * FFN],
    "moe_out_w": [1, NE, FFN, D],
}


def build(T, phases=ALL_PHASES):
    nc = bass.Bass("TRN2", target_bir_lowering=False)
    cx = Cx()
    cx.nc = nc
    cx.T, cx.TA, cx.NT, cx.NA = T, T + CTX, T // 128, (T + CTX) // 128
    TA = cx.TA

    def din(name, shape, dt=F32):
        return nc.dram_tensor(name, list(shape), dt, kind="ExternalInput").ap()

    def dscr(name, shape, dt=F32):
        return nc.dram_tensor(name, list(shape), dt, kind="Internal").ap()

    cx.x = din("x", [T, D])
    cx.ctx = din("ctx", [CTX, D])
    cx.ccol = din("ccol", [128, 16])
    for k, shp in W_SHAPES.items():
        setattr(cx, k, din(k, shp))
    cx.hgrn_lbl = din("hgrn_lbl", [128, 2, 4])
    cx.cst = din("cst", [128, C_END])
    cx.mats = din("mats", [3, 128, 128])
    cx.rope_ret = din("rope_ret", [2, 128, T])
    cx.rope_mla = din("rope_mla", [T, 64])
    cx.out = nc.dram_tensor("out", [T, D], F32, kind="ExternalOutput").ap()
    cx.modb = dscr("modb", [2, 2, 6, 128, D])
    cx.rx = dscr("rx", [T, D])
    cx.rc = dscr("rc", [CTX, D])
    cx.hTd = dscr("hTd", [KC, 128, TA], BF16)
    cx.yTd = dscr("yTd", [8, 128, TA], BF16)
    import os
    DBG = os.environ.get("KDBG", "")
    cx.attnT = (nc.dram_tensor("attnT", [8, 128, T], BF16, kind="ExternalOutput").ap() if DBG == "attnT"
                else dscr("attnT", [8, 128, T], BF16))
    P = cx.P = Prog(nc)
    if DBG == "attnT":
        cx.dbg = {"qnT": nc.dram_tensor("d_qnT", [128, 3, T], BF16, kind="ExternalOutput").ap(),
                  "ckvT": nc.dram_tensor("d_ckvT", [128, 2, TA], BF16, kind="ExternalOutput").ap(),
                  "krT": nc.dram_tensor("d_krT", [128, TA], BF16, kind="ExternalOutput").ap(),
                  "qrT": nc.dram_tensor("d_qrT", [128, 4, T], BF16, kind="ExternalOutput").ap()}
    with contextlib.ExitStack() as gst:
        cx.PS = [gst.enter_context(nc.psum_tensor("ps%d" % b, [128, 512], F32)) for b in range(8)]
        cx.PSB = [p[:].bitcast(BF16) for p in cx.PS]
        mt = gst.enter_context(nc.sbuf_tensor("mats_sb", [128, 3, 128], BF16))
        P.dma("pool", mt[:], cx.mats.rearrange("i p n -> p i n"), writes=["mats"])
        P.barrier()
        cx.ident, cx.ones, cx.perm = mt[:, 0, :], mt[:, 1, :], mt[:, 2, :]
        phase_mod(cx)
        cur = {0: (cx.x, "x_in"), 1: (cx.ctx, "c_in")}
        res = {0: (cx.rx, "rx"), 1: (cx.rc, "rc")}
        for i, phn in enumerate(phases):
            last = i == len(phases) - 1
            dst = dict(res)
            if last:
                dst[0] = (cx.out, "out")
            if phn == "mix0":
                phase_mix0(cx, cur, dst)
            elif phn == "ffn0":
                phase_ffn(cx, 0, cx.ffn_in_w, cx.ffn_out_w, None, 1, cur, dst, True, "F0")
            elif phn == "mla":
                phase_mla(cx, 1, cur, dst)
            elif phn == "moe":
                phase_ffn(cx, 1, cx.moe_in_w[0], cx.moe_out_w[0], cx.router_w[0], NE, cur, dst, False, "F1")
            if phn in ("mix0", "ffn0"):
                cur = dict(dst)
            else:
                cur = {0: dst[0], 1: cur[1]}
        P.emit(final_wait_resources=[("out", t) for t in range(cx.NT)])
    cx.stats = P.stats
    return nc, cx


def rope_tables(T, dim):
    n_rows = T // GRID_W
    rows = np.repeat(np.arange(n_rows), GRID_W).astype(np.float32)
    cols = np.tile(np.arange(GRID_W), n_rows).astype(np.float32)
    n_freq = dim // 4
    inv = (np.float32(10000.0) ** (-np.arange(n_freq, dtype=np.float32) / np.float32(n_freq))).astype(np.float32)
    ang = np.concatenate([rows[:, None] * inv, cols[:, None] * inv], axis=-1).astype(np.float32)
    return np.cos(ang).astype(np.float32), np.sin(ang).astype(np.float32)


def const_inputs(T):
    p = np.arange(128)
    same = (p[:, None] // CH) == (p[None, :] // CH)
    cst = np.zeros((128, C_END), np.float32)
    cst[:, C_MHF:C_MHF + 128] = (same & (p[:, None] <= p[None, :])).astype(np.float32)
    cst[:, C_MHB:C_MHB + 128] = (same & (p[:, None] >= p[None, :])).astype(np.float32)
    msk = np.ones(512, np.float32)
    msk[::CH] = 0.0
    cst[:, C_MSK:C_MSK + 512] = msk[None, :]
    mats = np.zeros((3, 128, 128), np.float32)
    mats[0] = np.eye(128)
    mats[1] = 1.0
    mats[2][(p + 64) % 128, p] = 1.0
    cr, sr = rope_tables(T, 128)
    rope_ret = np.stack([np.concatenate([cr.T, cr.T], 0), np.concatenate([-sr.T, sr.T], 0)]).astype(np.float32)
    cm, sm = rope_tables(T, 64)
    rope_mla = np.concatenate([cm, sm], axis=1).astype(np.float32)
    return {"cst": cst, "mats": mats, "rope_ret": np.ascontiguousarray(rope_ret), "rope_mla": np.ascontiguousarray(rope_mla)}


def core_inputs(inputs, b, consts):
    f = lambda a: np.ascontiguousarray(np.asarray(a, dtype=np.float32))
    m = {"x": f(inputs["x"][b]), "ctx": f(inputs["ctx"][b])}
    c = f(inputs["c"][b]).reshape(8, 128).T
    cc = f(inputs["c_ctx"]).reshape(8, 128).T
    m["ccol"] = np.ascontiguousarray(np.concatenate([c, cc], axis=1))
    for k in W_SHAPES:
        m[k] = f(inputs[k])
    m["hgrn_lbl"] = np.ascontiguousarray(f(inputs["hgrn_lb_logit"]).reshape(2, 4, 128).transpose(2, 0, 1))
    m.update(consts)
    return m


_CACHE = {}


def kernel(**inputs):
    T = int(np.asarray(inputs["x"]).shape[1])
    B = int(np.asarray(inputs["x"]).shape[0])
    if T not in _CACHE:
        _CACHE[T] = build(T)[0]
    nc = _CACHE[T]
    consts = const_inputs(T)
    in_maps = [core_inputs(inputs, b, consts) for b in range(B)]
    res = run_bass_kernel_spmd(nc, in_maps, core_ids=list(range(B)))
    return np.stack([np.asarray(r["out"], dtype=np.float32) for r in res.results], axis=0)
```

```python
import contextlib
import numpy as np
import concourse.bass as bass
import concourse.mybir as mybir
from concourse.bass_utils import run_bass_kernel_spmd

F32 = mybir.dt.float32
BF16 = mybir.dt.bfloat16
AF = mybir.ActivationFunctionType
ALU = mybir.AluOpType

ENGS = ("pe", "act", "dve", "pool", "sp")
N_DMA_SEMS = 32

D = 1024
KC = 8
CTX = 256
FFN = 2816
FC = 22
NE = 8
EPS = 1e-6
GRID_W = 64


class Op:
    __slots__ = ("eng", "fn", "reads", "writes", "is_dma", "deps", "has_dep", "ticket", "dsem", "dval",
                 "dprev", "extra")

    def __init__(self, eng, fn, reads, writes, is_dma):
        self.eng = eng
        self.fn = fn
        self.reads = reads
        self.writes = writes
        self.is_dma = is_dma
        self.deps = []
        self.has_dep = False
        self.ticket = None
        self.dsem = None
        self.dval = None
        self.dprev = None
        self.extra = ()


class Prog:
    def __init__(self, nc):
        self.nc = nc
        self.ops = []
        self.phase_start = 0

    def add(self, eng, fn, reads=(), writes=(), is_dma=False):
        self.ops.append(Op(eng, fn, tuple(reads), tuple(writes), is_dma))

    def dma(self, eng, out, in_, reads=(), writes=(), **kw):
        self.add(eng, lambda e: e.dma_start(out=out, in_=in_, **kw), reads, writes, is_dma=True)

    def mm(self, out, lhsT, rhs, start, stop, reads=(), writes=()):
        self.add("pe", lambda e: e.matmul(out, lhsT, rhs, start=start, stop=stop), reads, writes)

    def tr(self, out, in_, ident, reads=(), writes=()):
        self.add("pe", lambda e: e.transpose(out, in_, ident), reads, writes)

    def barrier(self):
        last = {}
        dmas = []
        for i in range(self.phase_start, len(self.ops)):
            op = self.ops[i]
            if op.fn is None:
                continue
            if op.is_dma:
                dmas.append(i)
            else:
                last[op.eng] = i
        extra = tuple(sorted(set(last.values()) | set(dmas)))
        for e in ENGS:
            op = Op(e, None, (), (), False)
            op.extra = extra
            self.ops.append(op)
        self.phase_start = len(self.ops)

    def _analyse(self):
        last_w = {}
        readers = {}
        ops = self.ops
        for i, op in enumerate(ops):
            deps = set()
            for r in op.reads:
                w = last_w.get(r)
                if w is not None:
                    deps.add(w)
            for r in op.writes:
                w = last_w.get(r)
                if w is not None:
                    deps.add(w)
                rd = readers.get(r)
                if rd:
                    deps.update(rd.values())
            keep = []
            for j in deps:
                p = ops[j]
                if p.eng == op.eng and not p.is_dma and not op.is_dma and op.eng == "pe":
                    continue
                keep.append(j)
            for j in op.extra:
                p = ops[j]
                if p.eng == op.eng and not p.is_dma:
                    continue
                keep.append(j)
            op.deps = sorted(set(keep))
            for j in op.deps:
                ops[j].has_dep = True
            key = ("dma", i) if op.is_dma else op.eng
            for r in op.reads:
                d = readers.get(r)
                if d is None:
                    d = readers[r] = {}
                d[key] = i
            for r in op.writes:
                last_w[r] = i
                readers[r] = {}

    def emit(self, final_wait_resources=()):
        nc = self.nc
        self.add("sp", None, reads=tuple(final_wait_resources), writes=())
        self._analyse()
        with contextlib.ExitStack() as st:
            esem = {e: st.enter_context(nc.semaphore("s_" + e)) for e in ENGS}
            dsems = [st.enter_context(nc.semaphore("d%d" % k)) for k in range(N_DMA_SEMS)]
            cnt = {e: 0 for e in ENGS}
            dcnt = [0] * N_DMA_SEMS
            dlast = [None] * N_DMA_SEMS
            k = 0
            kq = {"sp": 0, "pool": 0}
            half = N_DMA_SEMS // 2
            for i, op in enumerate(self.ops):
                if op.is_dma:
                    s = (kq[op.eng] % half) + (0 if op.eng == "sp" else half)
                    kq[op.eng] += 1
                    k += 1
                    op.dsem = s
                    dcnt[s] += 16
                    op.dval = dcnt[s]
                    op.dprev = dlast[s]
                    dlast[s] = i
                elif op.has_dep:
                    cnt[op.eng] += 1
                    op.ticket = cnt[op.eng]
            self.stats = dict(cnt)
            self.stats["n_ops"] = len(self.ops)
            self.stats["n_dma"] = k
            block = st.enter_context(nc.Block())
            ops = self.ops

            def run(engname):
                def body(e):
                    seen_e = {x: 0 for x in ENGS}
                    seen_d = [0] * N_DMA_SEMS
                    for op in ops:
                        if op.eng != engname:
                            continue
                        deps = op.deps
                        if op.is_dma and op.dprev is not None:
                            deps = deps + [op.dprev]
                        for j in deps:
                            p = ops[j]
                            if p.is_dma:
                                if seen_d[p.dsem] < p.dval:
                                    e.wait_ge(dsems[p.dsem], p.dval)
                                    seen_d[p.dsem] = p.dval
                            else:
                                if seen_e[p.eng] < p.ticket:
                                    e.wait_ge(esem[p.eng], p.ticket)
                                    seen_e[p.eng] = p.ticket
                        if op.fn is None:
                            continue
                        ins = op.fn(e)
                        if op.is_dma:
                            ins.then_inc(dsems[op.dsem], 16)
                        elif op.ticket is not None:
                            ins.then_inc(esem[op.eng], 1)
                return body

            block.tensor(run("pe"))
            block.scalar(run("act"))
            block.vector(run("dve"))
            block.gpsimd(run("pool"))
            block.sync(run("sp"))


class Cx:
    pass


class Phase:
    def __init__(self, cx, name):
        self.cx = cx
        self.name = name
        self.st = contextlib.ExitStack()
        self.n = 0

    def __enter__(self):
        self.st.__enter__()
        return self

    def sb(self, shape, dt, name=None):
        self.n += 1
        nm = "%s_%s%d" % (self.name, name or "t", self.n)
        t = self.st.enter_context(self.cx.nc.sbuf_tensor(nm, list(shape), dt))
        return t

    def __exit__(self, *a):
        self.cx.P.barrier()
        return self.st.__exit__(*a)


def bcast_row(ap_row, n):
    return ap_row.to_broadcast([n, ap_row.shape[-1]])


class PrePost:
    def __init__(self, cx, ph, tag):
        self.cx = cx
        self.tag = tag
        self.xt = [ph.sb([128, D], F32, "xt") for _ in range(2)]
        self.t1 = [ph.sb([128, D], F32, "t1") for _ in range(2)]
        self.hb = [ph.sb([128, D], BF16, "hb") for _ in range(2)]
        self.sq = ph.sb([128, D], BF16, "sq")
        self.st = ph.sb([128, 2, 8], F32, "st")
        self.n = 0
        self.nl = 0

    def load(self, src_ap, src_res):
        b = self.nl % 2
        self.nl += 1
        self.cx.P.dma("sp", self.xt[b][:], src_ap, reads=[src_res], writes=[(self.tag, "xt", b)])

    def rstd(self, ss, rs, inv_n, deps_r, deps_w):
        P = self.cx.P
        P.add("dve", lambda e: e.tensor_scalar(rs, ss, inv_n, EPS, op0=ALU.mult, op1=ALU.add),
              reads=deps_r, writes=deps_w)
        P.add("act", lambda e: e.activation(rs, rs, AF.Sqrt), reads=deps_w, writes=deps_w)
        P.add("dve", lambda e: e.reciprocal(rs, rs), reads=deps_w, writes=deps_w)

    def pre(self, src_ap, src_res, A, B, mod_res, hT_dst, hT_res, bank):
        cx, P, tg = self.cx, self.cx.P, self.tag
        i = self.n
        self.n += 1
        b = i % 2
        xt, t1, hb, sq = self.xt[b], self.t1[b], self.hb[b], self.sq
        rx = (tg, "xt", b)
        if self.nl <= i:
            self.load(src_ap, src_res)
        ss = self.st[:, b, 0:1]
        rs = self.st[:, b, 1:2]
        P.add("act", lambda e: e.activation(sq[:], xt[:], AF.Square, accum_out=ss),
              reads=[rx], writes=[(tg, "sq"), (tg, "ss", b)])
        self.rstd(ss, rs, 1.0 / D, [(tg, "ss", b)], [(tg, "rs", b)])
        P.add("dve", lambda e: e.scalar_tensor_tensor(t1[:], xt[:], rs, A, op0=ALU.mult, op1=ALU.mult),
              reads=[rx, (tg, "rs", b), mod_res], writes=[(tg, "t1", b)])
        P.add("pool", lambda e: e.tensor_tensor(hb[:], t1[:], B, op=ALU.add),
              reads=[(tg, "t1", b), mod_res], writes=[(tg, "hb", b)])
        psb = cx.PSB[bank]
        for k in range(KC):
            P.tr(psb[:, k * 128:(k + 1) * 128], hb[:, k * 128:(k + 1) * 128], cx.ident[:],
                 reads=[(tg, "hb", b)], writes=[("ps", bank)])
        P.add("act", lambda e: e.activation(hT_dst, psb[:, 0:D].rearrange("p (k n) -> p k n", k=KC), AF.Copy),
              reads=[], writes=[("ps", bank), hT_res])

    def post(self, o_src, o_res, res_ap, res_res, G, mod_res, dst_ap, dst_res):
        cx, P, tg = self.cx, self.cx.P, self.tag
        i = self.n
        self.n += 1
        b = i % 2
        xt, t1, sq = self.xt[b], self.t1[b], self.sq
        rx = (tg, "xt", b)
        if self.nl <= i:
            self.load(res_ap, res_res)
        ss = self.st[:, b, 2:3]
        rs = self.st[:, b, 3:4]
        P.add("act", lambda e: e.activation(sq[:], o_src, AF.Square, accum_out=ss),
              reads=[o_res], writes=[(tg, "sq"), (tg, "ss2", b)])
        self.rstd(ss, rs, 1.0 / D, [(tg, "ss2", b)], [(tg, "rs2", b)])
        P.add("dve", lambda e: e.scalar_tensor_tensor(t1[:], o_src, rs, G, op0=ALU.mult, op1=ALU.mult),
              reads=[o_res, (tg, "rs2", b), mod_res], writes=[(tg, "t1", b)])
        P.add("pool", lambda e: e.tensor_tensor(xt[:], t1[:], xt[:], op=ALU.add),
              reads=[(tg, "t1", b), rx], writes=[rx])
        P.dma("sp", dst_ap, xt[:], reads=[rx], writes=[dst_res])


def load_mod(cx, ph, layer, stream, tag):
    t = ph.sb([128, 6, D], F32, "mod")
    res = (tag, "mod", stream)
    cx.P.dma("sp", t[:], cx.modb[layer, stream].rearrange("i p n -> p i n"), reads=[("modb", layer, stream)],
             writes=[res])
    return t, res


def tok_tiles(cx, with_ctx):
    tl = []
    if with_ctx:
        tl += [(1, i) for i in range(CTX // 128)]
    tl += [(0, i) for i in range(cx.NT)]
    return tl


def phase_mod(cx):
    P = cx.P
    with Phase(cx, "M") as ph:
        sc = ph.sb([128, 16], F32, "sc")
        lh = ph.sb([128, 16, 128], BF16, "lh")
        P.dma("sp", sc[:], cx.ccol, writes=["M_sc"])
        P.add("act", lambda e: e.activation(sc[:], sc[:], AF.Silu), reads=["M_sc"], writes=["M_sc"])
        for j in range(16):
            P.add("dve", lambda e, j=j: e.tensor_copy(lh[:, j, :], sc[:, j:j + 1].to_broadcast([128, 128])),
                  reads=["M_sc"], writes=["M_lh"])
        NH = 3072
        wb = ph.sb([128, KC, NH], BF16, "wb")
        bb = ph.sb([128, NH], F32, "bb")
        gb = ph.sb([128, 4, D], F32, "gb")
        m = ph.sb([128, NH], F32, "m")
        o = ph.sb([128, 3, D], F32, "o")
        for l in range(2):
            P.dma("sp", gb[:], bcast_row(cx.norm_g[l:l + 1].rearrange("o i n -> o (i n)"), 128)
                  .rearrange("p (i n) -> p i n", i=4), writes=["M_gb"])
            for half in range(2):
                for k in range(KC):
                    P.dma("pool", wb[:, k, :], cx.mod_w[l, k * 128:(k + 1) * 128, half * NH:(half + 1) * NH],
                          writes=[("M_wb", k)])
                P.dma("sp", bb[:], bcast_row(cx.mod_b[l:l + 1, half * NH:(half + 1) * NH], 128), writes=["M_bb"])
                for s in range(2):
                    for nb in range(NH // 512):
                        bank = nb % 4
                        for k in range(KC):
                            P.mm(cx.PS[bank][:], lh[:, s * 8 + k, :], wb[:, k, nb * 512:(nb + 1) * 512],
                                 k == 0, k == KC - 1, reads=["M_lh", ("M_wb", k)], writes=[("ps", bank)])
                        P.add("dve", lambda e, nb=nb, bank=bank: e.tensor_tensor(
                            m[:, nb * 512:(nb + 1) * 512], cx.PS[bank][:], bb[:, nb * 512:(nb + 1) * 512], op=ALU.add),
                            reads=["M_bb"], writes=[("ps", bank), "M_m"])
                    g_a = gb[:, 2 * half, :]
                    g_g = gb[:, 2 * half + 1, :]
                    P.add("dve", lambda e, g_a=g_a: e.scalar_tensor_tensor(o[:, 0, :], m[:, D:2 * D], 1.0, g_a,
                                                                          op0=ALU.add, op1=ALU.mult),
                          reads=["M_m", "M_gb"], writes=["M_o"])
                    P.add("pool", lambda e: e.tensor_copy(o[:, 1, :], m[:, 0:D]), reads=["M_m", "M_o"], writes=["M_o"])
                    P.add("pool", lambda e, g_g=g_g: e.tensor_tensor(o[:, 2, :], m[:, 2 * D:3 * D], g_g, op=ALU.mult),
                          reads=["M_m", "M_gb", "M_o"], writes=["M_o"])
                    P.dma("sp", cx.modb[l, s, 3 * half:3 * half + 3].rearrange("i p n -> p i n"), o[:],
                          reads=["M_o"], writes=[("modb", l, s)])


MOD_A, MOD_B, MOD_G = 0, 1, 2


def phase_ffn(cx, layer, w_in, w_out, router, n_exp, src, dst, with_ctx, tag):
    P = cx.P
    SBT = 12
    FG = [4, 4, 4, 4, 4, 2]
    tiles = tok_tiles(cx, with_ctx)
    with Phase(cx, tag) as ph:
        pp = PrePost(cx, ph, tag)
        mods = {}
        for s in ([0, 1] if with_ctx else [0]):
            mods[s] = load_mod(cx, ph, layer, s, tag)
        hT = ph.sb([128, KC, SBT * 128], BF16, "hT")
        y = ph.sb([128, SBT, D], F32, "y")
        wi = [ph.sb([128, KC, 2, 512], BF16, "wi") for _ in range(2)]
        wo = [ph.sb([128, 4, D], BF16, "wo") for _ in range(2)]
        u = [ph.sb([128, 4, 512], BF16, "u") for _ in range(2)]
        sa = [ph.sb([128, 512], F32, "sa") for _ in range(2)]
        gates = ph.sb([128, SBT, NE], F32, "gates")
        rt = ph.sb([128, 64], F32, "rt")
        if router is not None:
            rw = ph.sb([128, KC, NE], BF16, "rw")
            P.dma("pool", rw[:], router.rearrange("(k p) n -> p k n", p=128), writes=[(tag, "rw")])
        piece = 0
        nblk_total = 0
        for sb0 in range(0, len(tiles), SBT):
            sbt = tiles[sb0:sb0 + SBT]
            n = len(sbt)
            for j, (s, ti) in enumerate(sbt):
                modt, modr = mods[s]
                sap, sres = src[s]
                pp.pre(sap[ti * 128:(ti + 1) * 128, :], (sres, ti), modt[:, 3 + MOD_A, :], modt[:, 3 + MOD_B, :], modr,
                       hT[:, :, j * 128:(j + 1) * 128], (tag, "hT", j), bank=j % 2)
                P.add("pool", lambda e, j=j: e.memset(y[:, j, :], 0.0), writes=[(tag, "y", j)])
                if router is not None:
                    bank = 2 + j % 2
                    for k in range(KC):
                        P.mm(cx.PS[bank][:, 0:NE], hT[:, k, j * 128:(j + 1) * 128], rw[:, k, :], k == 0, k == KC - 1,
                             reads=[(tag, "hT", j), (tag, "rw")], writes=[("ps", bank)])
                    lg = rt[:, 0:8]
                    mx = rt[:, 8:16]
                    sc = rt[:, 16:24]
                    g1 = rt[:, 24:32]
                    g2 = rt[:, 32:40]
                    R = (tag, "rt")
                    P.add("dve", lambda e, bank=bank: e.tensor_copy(lg, cx.PS[bank][:, 0:NE]), reads=[R],
                          writes=[("ps", bank), R])
                    P.add("dve", lambda e: e.max(out=mx, in_=lg), reads=[R], writes=[R])
                    P.add("dve", lambda e: e.tensor_tensor(sc[:, 0:1], mx[:, 1:2], mx[:, 0:1], op=ALU.subtract),
                          reads=[R], writes=[R])
                    P.add("act", lambda e: e.activation(sc[:, 1:2], sc[:, 0:1], AF.Exp), reads=[R], writes=[R])
                    P.add("dve", lambda e: e.tensor_scalar(sc[:, 2:3], sc[:, 1:2], 1.0, None, op0=ALU.add),
                          reads=[R], writes=[R])
                    P.add("dve", lambda e: e.reciprocal(sc[:, 3:4], sc[:, 2:3]), reads=[R], writes=[R])
                    P.add("dve", lambda e: e.tensor_tensor(sc[:, 4:5], sc[:, 1:2], sc[:, 3:4], op=ALU.mult),
                          reads=[R], writes=[R])
                    P.add("dve", lambda e: e.tensor_scalar(g1, lg, mx[:, 0:1], sc[:, 3:4], op0=ALU.is_equal,
                                                           op1=ALU.mult), reads=[R], writes=[R])
                    P.add("dve", lambda e: e.tensor_scalar(g2, lg, mx[:, 1:2], sc[:, 4:5], op0=ALU.is_equal,
                                                           op1=ALU.mult), reads=[R], writes=[R])
                    P.add("dve", lambda e, j=j: e.tensor_tensor(gates[:, j, :], g1, g2, op=ALU.add), reads=[R],
                          writes=[R, (tag, "gates", j)])
            blocks = [(b0, min(4, n - b0)) for b0 in range(0, n, 4)]
            for ex in range(n_exp):
                f0 = 0
                for gsz in FG:
                    pb = piece % 2
                    piece += 1
                    wit, wot = wi[pb], wo[pb]
                    Rwi, Rwo = (tag, "wi", pb), (tag, "wo", pb)
                    for ab in range(2):
                        P.dma("pool", wit[:, :, ab, 0:gsz * 128],
                              w_in[ex, :, ab * FFN + f0 * 128: ab * FFN + (f0 + gsz) * 128]
                              .rearrange("(k p) n -> p k n", p=128), writes=[Rwi])
                    P.dma("pool", wot[:, 0:gsz, :],
                          w_out[ex, f0 * 128:(f0 + gsz) * 128, :].rearrange("(c p) n -> p c n", p=128), writes=[Rwo])
                    for (b0, bn) in blocks:
                        W = bn * 128
                        ub = nblk_total % 2
                        nblk_total += 1
                        ut = u[ub]
                        for c in range(gsz):
                            pa, pbk = (0, 1) if c % 2 == 0 else (2, 3)
                            for ab, bank in ((0, pa), (1, pbk)):
                                for k in range(KC):
                                    P.mm(cx.PS[bank][:, 0:W], wit[:, k, ab, c * 128:(c + 1) * 128],
                                         hT[:, k, b0 * 128:b0 * 128 + W], k == 0, k == KC - 1,
                                         reads=[Rwi] + [(tag, "hT", b0 + q) for q in range(bn)], writes=[("ps", bank)])
                            sat = sa[c % 2]
                            P.add("act", lambda e, sat=sat, pa=pa, W=W: e.activation(sat[:, 0:W], cx.PS[pa][:, 0:W], AF.Silu),
                                  writes=[("ps", pa), (tag, "sa", c % 2)])
                            P.add("dve", lambda e, sat=sat, pbk=pbk, W=W, ut=ut, c=c: e.tensor_tensor(
                                ut[:, c, 0:W], sat[:, 0:W], cx.PS[pbk][:, 0:W], op=ALU.mult),
                                reads=[(tag, "sa", c % 2)], writes=[("ps", pbk), (tag, "u", ub, c)])
                        oi = 0
                        for q in range(bn):
                            j = b0 + q
                            for mh in range(2):
                                bank = 4 + oi % 4
                                oi += 1
                                for c in range(gsz):
                                    P.mm(cx.PS[bank][:], ut[:, c, q * 128:(q + 1) * 128], wot[:, c, mh * 512:(mh + 1) * 512],
                                         c == 0, c == gsz - 1, reads=[Rwo, (tag, "u", ub, c)], writes=[("ps", bank)])
                                gsc = gates[:, j, ex:ex + 1] if router is not None else 1.0
                                P.add("dve", lambda e, bank=bank, j=j, mh=mh, gsc=gsc: e.scalar_tensor_tensor(
                                    y[:, j, mh * 512:(mh + 1) * 512], cx.PS[bank][:], gsc, y[:, j, mh * 512:(mh + 1) * 512],
                                    op0=ALU.mult, op1=ALU.add),
                                    reads=[(tag, "gates", j)], writes=[("ps", bank), (tag, "y", j)])
                    f0 += gsz
            for j, (s, ti) in enumerate(sbt):
                modt, modr = mods[s]
                sap, sres = src[s]
                dap, dres = dst[s]
                if j == 0:
                    pp.load(sap[ti * 128:(ti + 1) * 128, :], (sres, ti))
                if j + 1 < len(sbt):
                    s2, t2 = sbt[j + 1]
                    pp.load(src[s2][0][t2 * 128:(t2 + 1) * 128, :], (src[s2][1], t2))
                pp.post(y[:, j, :], (tag, "y", j), sap[ti * 128:(ti + 1) * 128, :], (sres, ti), modt[:, 3 + MOD_G, :], modr,
                        dap[ti * 128:(ti + 1) * 128, :], (dres, ti))


MLA_H = 8
MLA_QR = 384
MLA_KVR = 256
MLA_SCALE = (128 + 64) ** -0.5


def rope_tok(P, eng, out, x1, x2, cos, sin, tmp_a, tmp_b, R):
    o1, o2 = out
    P.add(eng, lambda e: e.tensor_tensor(tmp_a, x1, cos, op=ALU.mult), reads=R, writes=R)
    P.add(eng, lambda e: e.tensor_tensor(tmp_b, x2, sin, op=ALU.mult), reads=R, writes=R)
    P.add(eng, lambda e: e.tensor_tensor(o1, tmp_a, tmp_b, op=ALU.subtract), reads=R, writes=R)
    P.add(eng, lambda e: e.tensor_tensor(tmp_a, x1, sin, op=ALU.mult), reads=R, writes=R)
    P.add(eng, lambda e: e.tensor_tensor(tmp_b, x2, cos, op=ALU.mult), reads=R, writes=R)
    P.add(eng, lambda e: e.tensor_tensor(o2, tmp_a, tmp_b, op=ALU.add), reads=R, writes=R)


def phase_mla(cx, layer, src, dst):
    P = cx.P
    T, TA, NT, NA = cx.T, cx.TA, cx.NT, cx.NA
    tag = "A"
    tiles = tok_tiles(cx, True)
    with Phase(cx, "A") as ph:
        qnT = ph.sb([128, 3, T], BF16, "qnT")
        ckvT = ph.sb([128, 2, TA], BF16, "ckvT")
        krT = ph.sb([128, TA], BF16, "krT")
        qrT = ph.sb([128, 4, T], BF16, "qrT")
        with Phase(cx, "A1") as p1:
            pp = PrePost(cx, p1, "A1")
            mods = {s: load_mod(cx, p1, layer, s, "A1") for s in (0, 1)}
            wd = p1.sb([128, KC, 704], BF16, "wd")
            for (c0_, c1_) in ((0, 512), (512, 704)):
                P.dma("pool", wd[:, :, c0_:c1_], cx.mla_down_w[0][:, c0_:c1_].rearrange("(k p) n -> p k n", p=128), writes=["A_wd"])
            wqr = p1.sb([128, 3, 8, 64], BF16, "wqr")
            for k in range(3):
                P.dma("pool", wqr[:, k], cx.mla_uq_w[0, k * 128:(k + 1) * 128, :].rearrange("p (h d) -> p h d", d=192)[:, :, 128:192],
                      writes=["A_wqr"])
            gq = p1.sb([128, 640], F32, "gq")
            P.dma("sp", gq[:, 0:384], bcast_row(cx.mla_q_norm_g[0:1, :], 128), writes=["A_gq"])
            P.dma("sp", gq[:, 384:640], bcast_row(cx.mla_kv_norm_g[0:1, :], 128), writes=["A_gq"])
            hT = [p1.sb([128, KC, 128], BF16, "hT") for _ in range(2)]
            dn2 = [p1.sb([128, 704], F32, "dn") for _ in range(2)]
            nb2 = [p1.sb([128, 768], BF16, "nb") for _ in range(2)]
            st2 = [p1.sb([128, 8], F32, "st") for _ in range(2)]
            cs = [p1.sb([128, 64], F32, "cs") for _ in range(2)]
            tmp2 = [p1.sb([128, 2, 256], F32, "tmp") for _ in range(2)]
            qr322 = [p1.sb([128, 8, 64], F32, "qr32") for _ in range(2)]
            qrb2 = [p1.sb([128, 8, 64], BF16, "qrb") for _ in range(2)]
            sqj = pp.sq

            def a1_tile(idx, s, ti, par, hTt, dn, nb, st, tmp, qr32, qrb, cst):
                ta = idx
                modt, modr = mods[s]
                sap, sres = src[s]
                Rh = ("A_hT", par)
                Rdn, Rst, Rst2, Rst2b, Rst3, Rnb = ("A_dn", par), ("A_st", par), ("A_st2", par), ("A_st2b", par), ("A_st3", par), ("A_nb", par)
                Rcs, Rtmp, Rqr32, Rqrb = ("A_cs", par), ("A_tmp", par), ("A_qr32", par), ("A_qrb", par)
                pp.pre(sap[ti * 128:(ti + 1) * 128, :], (sres, ti), modt[:, MOD_A, :], modt[:, MOD_B, :], modr,
                       hTt[:], Rh, bank=par)
                for (bank, c0, c1) in ((2, 0, 512), (3, 512, 704)):
                    for k in range(KC):
                        P.mm(cx.PS[bank][:, 0:c1 - c0], hTt[:, k, :], wd[:, k, c0:c1], k == 0, k == KC - 1,
                             reads=[Rh, "A_wd"], writes=[("ps", bank)])
                    P.add("act", lambda e, bank=bank, c0=c0, c1=c1: e.activation(dn[:, c0:c1], cx.PS[bank][:, 0:c1 - c0], AF.Copy),
                          writes=[("ps", bank), Rdn])
                P.add("act", lambda e: e.activation(sqj[:, 0:384], dn[:, 0:384], AF.Square, accum_out=st[:, 0:1]),
                      reads=[Rdn], writes=["A_sq", Rst])
                P.add("act", lambda e: e.activation(sqj[:, 384:640], dn[:, 384:640], AF.Square, accum_out=st[:, 1:2]),
                      reads=[Rdn], writes=["A_sq", Rst])
                P.add("dve", lambda e: e.tensor_scalar(st[:, 2:3], st[:, 0:1], 1.0 / 384, EPS, op0=ALU.mult, op1=ALU.add),
                      reads=[Rst], writes=[Rst2])
                P.add("dve", lambda e: e.tensor_scalar(st[:, 3:4], st[:, 1:2], 1.0 / 256, EPS, op0=ALU.mult, op1=ALU.add),
                      reads=[Rst], writes=[Rst2])
                P.add("act", lambda e: e.activation(st[:, 6:8], st[:, 2:4], AF.Sqrt), reads=[Rst2], writes=[Rst2b])
                P.add("dve", lambda e: e.reciprocal(st[:, 4:6], st[:, 6:8]), reads=[Rst2b], writes=[Rst3])
                if s == 0:
                    P.add("dve", lambda e: e.scalar_tensor_tensor(nb[:, 0:384], dn[:, 0:384], st[:, 4:5], gq[:, 0:384],
                                                                  op0=ALU.mult, op1=ALU.mult),
                          reads=[Rdn, Rst3, "A_gq"], writes=[Rnb])
                P.add("dve", lambda e: e.scalar_tensor_tensor(nb[:, 384:640], dn[:, 384:640], st[:, 5:6], gq[:, 384:640],
                                                              op0=ALU.mult, op1=ALU.mult),
                      reads=[Rdn, Rst3, "A_gq"], writes=[Rnb])
                if s == 0:
                    P.dma("sp", cst[:], cx.rope_mla[ti * 128:(ti + 1) * 128, :], writes=[Rcs])
                    RR = [Rdn, Rcs, Rtmp, Rnb]
                    rope_tok(P, "dve", (nb[:, 640:672], nb[:, 672:704]), dn[:, 640:672], dn[:, 672:704],
                             cst[:, 0:32], cst[:, 32:64], tmp[:, 0, 0:32], tmp[:, 1, 0:32], RR)
                else:
                    P.add("dve", lambda e: e.tensor_copy(nb[:, 640:704], dn[:, 640:704]), reads=[Rdn], writes=[Rnb])
                P.add("dve", lambda e: e.tensor_copy(nb[:, 704:768], nb[:, 640:704]), reads=[Rnb], writes=[Rnb])
                bank = 4 + par
                psb = cx.PSB[bank]
                tblocks = ([0, 1, 2] if s == 0 else []) + [3, 4, 5]
                for c in tblocks:
                    P.tr(psb[:, c * 128:(c + 1) * 128], nb[:, c * 128:(c + 1) * 128], cx.ident[:], reads=[Rnb],
                         writes=[("ps", bank)])
                if s == 0:
                    P.add("act", lambda e: e.activation(
                        qnT[:, :, ti * 128:(ti + 1) * 128], psb[:, 0:384].rearrange("p (k n) -> p k n", k=3), AF.Copy),
                        writes=[("ps", bank), ("A_qnT", ti)])
                P.add("dve", lambda e: e.tensor_copy(
                    ckvT[:, :, ta * 128:(ta + 1) * 128], psb[:, 384:640].rearrange("p (k n) -> p k n", k=2)),
                    writes=[("ps", bank), ("A_ckvT", ta)])
                P.add("dve", lambda e: e.tensor_copy(krT[:, ta * 128:(ta + 1) * 128], psb[:, 640:768]),
                      writes=[("ps", bank), ("A_krT", ta)])
                if s == 0:
                    bank6 = 6
                    for k in range(3):
                        P.mm(cx.PS[bank6][:], qnT[:, k, ti * 128:(ti + 1) * 128], wqr[:, k].rearrange("p h d -> p (h d)"),
                             k == 0, k == 2, reads=[("A_qnT", ti), "A_wqr"], writes=[("ps", bank6)])
                    P.add("act", lambda e: e.activation(qr32[:].rearrange("p h d -> p (h d)"), cx.PS[bank6][:], AF.Copy),
                          writes=[("ps", bank6), Rqr32])
                    cosb = cst[:, 0:32].unsqueeze(1).to_broadcast([128, 8, 32])
                    sinb = cst[:, 32:64].unsqueeze(1).to_broadcast([128, 8, 32])
                    ta_ = tmp[:, 0, :].rearrange("p (h d) -> p h d", h=8)
                    tb_ = tmp[:, 1, :].rearrange("p (h d) -> p h d", h=8)
                    RR = [Rqr32, Rcs, Rtmp, Rqrb]
                    rope_tok(P, "dve", (qrb[:, :, 0:32], qrb[:, :, 32:64]), qr32[:, :, 0:32], qr32[:, :, 32:64],
                             cosb, sinb, ta_, tb_, RR)
                    bank7 = 7
                    psb7 = cx.PSB[bank7]
                    qrf = qrb[:].rearrange("p h d -> p (h d)")
                    for c in range(4):
                        P.tr(psb7[:, c * 128:(c + 1) * 128], qrf[:, c * 128:(c + 1) * 128], cx.ident[:], reads=[Rqrb],
                             writes=[("ps", bank7)])
                    P.add("act", lambda e: e.activation(
                        qrT[:, :, ti * 128:(ti + 1) * 128], psb7[:, 0:512].rearrange("p (k n) -> p k n", k=4), AF.Copy),
                        writes=[("ps", bank7), ("A_qrT", ti)])

            for idx, (s, ti) in enumerate(tiles):
                par = idx % 2
                a1_tile(idx, s, ti, par, hT[par], dn2[par], nb2[par], st2[par], tmp2[par], qr322[par], qrb2[par], cs[par])
        if getattr(cx, "dbg", None):
            P.dma("sp", cx.dbg["qnT"], qnT[:], reads=[("A_qnT", t) for t in range(NT)], writes=["dbg1"])
            P.dma("sp", cx.dbg["ckvT"], ckvT[:], reads=[("A_ckvT", t) for t in range(NA)], writes=["dbg2"])
            P.dma("sp", cx.dbg["krT"], krT[:], reads=[("A_krT", t) for t in range(NA)], writes=["dbg3"])
            P.dma("sp", cx.dbg["qrT"], qrT[:], reads=[("A_qrT", t) for t in range(NT)], writes=["dbg4"])
        with Phase(cx, "A2") as p2:
            wqn = p2.sb([128, 3, 8, 128], BF16, "wqn")
            for k in range(3):
                P.dma("pool", wqn[:, k], cx.mla_uq_w[0, k * 128:(k + 1) * 128, :].rearrange("p (h d) -> p h d", d=192)[:, :, 0:128],
                      writes=["B_wqn"])
            wkv = p2.sb([128, 2, 2048], BF16, "wkv")
            P.dma("pool", wkv[:], cx.mla_ukv_w[0].rearrange("(k p) n -> p k n", p=128), writes=["B_wkv"])
            KhT = [p2.sb([128, TA], BF16, "KhT") for _ in range(2)]
            Vh = [p2.sb([128, NA, 128], BF16, "Vh") for _ in range(2)]
            QhT = [p2.sb([128, T], BF16, "QhT") for _ in range(2)]
            pT = [p2.sb([128, 512], BF16, "pT") for _ in range(3)]
            rden = p2.sb([128, 512], F32, "rden")
            at = [p2.sb([128, 512], BF16, "at") for _ in range(2)]
            accD = [p2.sb([128, 512], F32, "accD") for _ in range(2)]
            accP = [p2.sb([128, 512], F32, "accP") for _ in range(2)]
            accb = p2.sb([128, 512], BF16, "accb")
            all_ckv = [("A_ckvT", ta) for ta in range(NA)]
            all_qn = [("A_qnT", ti) for ti in range(NT)]

            def proj(h):
                par = h % 2
                pc = 0
                for kb0 in range(0, TA, 512):
                    W = min(512, TA - kb0)
                    bank = 7
                    for k in range(2):
                        P.mm(cx.PS[bank][:, 0:W], wkv[:, k, h * 256:h * 256 + 128], ckvT[:, k, kb0:kb0 + W], k == 0, k == 1,
                             reads=["B_wkv"] + all_ckv, writes=[("ps", bank)])
                    P.add("dve", lambda e, par=par, kb0=kb0, W=W, bank=bank: e.tensor_copy(KhT[par][:, kb0:kb0 + W], cx.PS[bank][:, 0:W]),
                          writes=[("ps", bank), ("B_K", par)])
                for t0 in range(0, NA, 4):
                    tn = min(4, NA - t0)
                    bank = 7
                    for q in range(tn):
                        for k in range(2):
                            P.mm(cx.PS[bank][:, q * 128:(q + 1) * 128], ckvT[:, k, (t0 + q) * 128:(t0 + q + 1) * 128],
                                 wkv[:, k, h * 256 + 128:h * 256 + 256], k == 0, k == 1,
                                 reads=["B_wkv"] + all_ckv, writes=[("ps", bank)])
                    P.add("dve", lambda e, par=par, t0=t0, tn=tn, bank=bank: e.tensor_copy(
                        Vh[par][:, t0:t0 + tn, :], cx.PS[bank][:, 0:tn * 128].rearrange("p (t d) -> p t d", d=128)),
                        writes=[("ps", bank), ("B_V", par)])
                for qb0 in range(0, T, 512):
                    bank = 7
                    for k in range(3):
                        P.mm(cx.PS[bank][:], wqn[:, k, h, :], qnT[:, k, qb0:qb0 + 512], k == 0, k == 2,
                             reads=["B_wqn"] + all_qn, writes=[("ps", bank)])
                    P.add("dve", lambda e, par=par, qb0=qb0, bank=bank: e.tensor_copy(QhT[par][:, qb0:qb0 + 512], cx.PS[bank][:]),
                          writes=[("ps", bank), ("B_Q", par)])

            NQ = T // 512
            steps = [(h, qb, kt) for h in range(MLA_H) for qb in range(NQ) for kt in range(NA)]
            NS = len(steps)

            def emit_S(i):
                h, qb, kt = steps[i]
                par = h % 2
                hp = h % 2
                qb0 = qb * 512
                sbk = i % 3
                P.mm(cx.PS[sbk][:], KhT[par][:, kt * 128:(kt + 1) * 128], QhT[par][:, qb0:qb0 + 512], True, False,
                     reads=[("B_K", par), ("B_Q", par)], writes=[("ps", sbk)])
                P.mm(cx.PS[sbk][:], krT[hp * 64:(hp + 1) * 64, kt * 128:(kt + 1) * 128],
                     qrT[hp * 64:(hp + 1) * 64, h // 2, qb0:qb0 + 512], False, True,
                     reads=[("A_krT", kt)] + [("A_qrT", (qb0 // 128) + q) for q in range(4)], writes=[("ps", sbk)])
                pt = pT[sbk]
                P.add("act", lambda e, pt=pt, sbk=sbk: e.activation(pt[:], cx.PS[sbk][:], AF.Exp, scale=MLA_SCALE),
                      writes=[("ps", sbk), ("B_pT", sbk)])

            def emit_PV(i):
                h, qb, kt = steps[i]
                par = h % 2
                qb0 = qb * 512
                sbk = i % 3
                blk = i // NA
                ob = 3 + 2 * (blk % 2)
                db = ob + 1
                pt = pT[sbk]
                P.mm(cx.PS[ob][:, :], Vh[par][:, kt, :], pt[:], kt == 0, kt == NA - 1,
                     reads=[("B_V", par), ("B_pT", sbk)], writes=[("ps", ob)])
                P.mm(cx.PS[db][:, :], cx.ones[:], pt[:], kt == 0, kt == NA - 1,
                     reads=[("B_pT", sbk)], writes=[("ps", db)])
                if kt == NA - 1:
                    P.add("dve", lambda e, db=db: e.reciprocal(rden[:], cx.PS[db][:]), writes=[("ps", db), "B_rden"])
                    att = at[blk % 2]
                    Rat = ("B_at", blk % 2)
                    P.add("dve", lambda e, att=att, ob=ob: e.tensor_tensor(att[:], cx.PS[ob][:], rden[:], op=ALU.mult),
                          reads=["B_rden"], writes=[("ps", ob), Rat])
                    P.dma("sp", cx.attnT[h, :, qb0:qb0 + 512], att[:], reads=[Rat], writes=[("attnT", qb0 // 512)])

            proj(0)
            proj(1)
            emit_S(0)
            emit_S(1)
            for i in range(NS):
                h, qb, kt = steps[i]
                if qb == 0 and kt == 0 and h >= 1 and h + 1 < MLA_H:
                    proj(h + 1)
                if i + 2 < NS:
                    emit_S(i + 2)
                emit_PV(i)
    with Phase(cx, "A3") as p3:
        pp = PrePost(cx, p3, "A3")
        modt, modr = load_mod(cx, p3, layer, 0, "A3")
        wo = p3.sb([128, 8, D], BF16, "wo")
        P.dma("pool", wo[:], cx.mla_out_w[0].rearrange("(h p) n -> p h n", p=128), writes=["C_wo"])
        a_in = [p3.sb([128, 8, 128], BF16, "a_in") for _ in range(2)]
        o32 = [p3.sb([128, D], F32, "o32") for _ in range(2)]
        sap, sres = src[0]
        dap, dres = dst[0]
        def a3_load(ti_):
            P.dma("sp", a_in[ti_ % 2][:], cx.attnT[:, :, ti_ * 128:(ti_ + 1) * 128].rearrange("h p n -> p h n"),
                  reads=[("attnT", ti_ // 4)], writes=[("C_a", ti_ % 2)])
            pp.load(sap[ti_ * 128:(ti_ + 1) * 128, :], (sres, ti_))

        a3_load(0)
        for ti in range(NT):
            ab = ti % 2
            if ti + 1 < NT:
                a3_load(ti + 1)
            for mh in range(2):
                bank = 2 * ab + mh
                for h in range(8):
                    P.mm(cx.PS[bank][:], a_in[ab][:, h, :], wo[:, h, mh * 512:(mh + 1) * 512], h == 0, h == 7,
                         reads=[("C_a", ab), "C_wo"], writes=[("ps", bank)])
                P.add("act", lambda e, ab=ab, mh=mh, bank=bank: e.activation(o32[ab][:, mh * 512:(mh + 1) * 512], cx.PS[bank][:], AF.Copy),
                      writes=[("ps", bank), ("C_o", ab)])
            pp.post(o32[ab][:], ("C_o", ab), sap[ti * 128:(ti + 1) * 128, :], (sres, ti), modt[:, MOD_G, :], modr,
                    dap[ti * 128:(ti + 1) * 128, :], (dres, ti))


CH = 64
C_MHF, C_MHB, C_MSK, C_END = 0, 128, 256, 768


def phase_mix0(cx, src, dst):
    P = cx.P
    T, TA, NT, NA = cx.T, cx.TA, cx.NT, cx.NA
    NC = TA // CH
    tiles = tok_tiles(cx, True)
    blocks = [(0, CTX)] + [(CTX + b, 512) for b in range(0, T, 512)]
    with Phase(cx, "X1") as p1:
        pp = PrePost(cx, p1, "X1")
        mods = {s: load_mod(cx, p1, 0, s, "X1") for s in (0, 1)}
        hTt = [p1.sb([128, KC, 128], BF16, "hTt") for _ in range(2)]
        for idx, (s, ti) in enumerate(tiles):
            modt, modr = mods[s]
            sap, sres = src[s]
            R = ("X1_hT", idx % 2)
            if idx == 0:
                pp.load(sap[ti * 128:(ti + 1) * 128, :], (sres, ti))
            if idx + 1 < len(tiles):
                s2, t2 = tiles[idx + 1]
                pp.load(src[s2][0][t2 * 128:(t2 + 1) * 128, :], (src[s2][1], t2))
            pp.pre(sap[ti * 128:(ti + 1) * 128, :], (sres, ti), modt[:, MOD_A, :], modt[:, MOD_B, :], modr,
                   hTt[idx % 2][:], R, bank=idx % 2)
            P.dma("sp", cx.hTd[:, :, idx * 128:(idx + 1) * 128].rearrange("k p n -> p k n"), hTt[idx % 2][:],
                  reads=[R], writes=[("hTd", idx // 4)])
    with Phase(cx, "X2") as ph:
        cst = ph.sb([128, C_END], F32, "cst")
        P.dma("sp", cst[:], cx.cst, writes=["X_cst"])
        lgt = ph.sb([128, 8], F32, "lgt")
        P.dma("sp", lgt[:], bcast_row(cx.ret_decay_logit[0:1].rearrange("o a b -> o (a b)"), 128), writes=["X_lg"])
        P.add("act", lambda e: e.activation(lgt[:], lgt[:], AF.Sigmoid), reads=["X_lg"], writes=["X_lg"])
        P.add("act", lambda e: e.activation(lgt[:], lgt[:], AF.Ln), reads=["X_lg"], writes=["X_lg"])
        lb = ph.sb([128, 3, 4], F32, "lb")
        P.dma("sp", lb[:, 0:2, :], cx.hgrn_lbl, writes=["X_lb"])
        P.add("dve", lambda e: e.tensor_tensor(lb[:, 2, :], lb[:, 0, :], lb[:, 1, :], op=ALU.subtract), reads=["X_lb"],
              writes=["X_lb2"])
        P.add("act", lambda e: e.activation(lb[:, 0, :], lb[:, 2, :], AF.Sigmoid), reads=["X_lb2"], writes=["X_lb3"])
        P.add("dve", lambda e: e.tensor_scalar(lb[:, 1, :], lb[:, 0, :], -1.0, 1.0, op0=ALU.mult, op1=ALU.add),
              reads=["X_lb3"], writes=["X_lb4"])
        LB = ["X_lb3", "X_lb4"]
        hTb = [ph.sb([128, KC, 512], BF16, "hTb") for _ in range(2)]
        wh = ph.sb([128, KC, 5, 128], BF16, "wh")
        qs = ph.sb([128, TA], BF16, "qs")
        gs = ph.sb([128, TA], BF16, "gs")
        kkr = ph.sb([128, TA], BF16, "kkr")
        v_h = ph.sb([128, NA, 128], BF16, "v_h")
        o_acc = ph.sb([128, TA], F32, "o_acc")
        q1 = ph.sb([128, TA], BF16, "q1")
        k1 = ph.sb([128, TA], BF16, "k1")
        k1t = ph.sb([128, NA, 128], BF16, "k1t")
        S_all = ph.sb([128, NC, 128], BF16, "S_all")
        S32 = [ph.sb([128, 128], F32, "S32") for _ in range(2)]
        er = ph.sb([128, NC], F32, "er")
        etad = ph.sb([128, NC], F32, "etad")
        ek = ph.sb([128, NC], F32, "ek")
        F = [ph.sb([128, 512], F32, "F%d" % i) for i in range(8)]
        raw = [ph.sb([128, 512], BF16, "raw%d" % i) for i in range(2)]
        csb = [ph.sb([128, 2, 512], F32, "csb") for _ in range(2)]
        am = [ph.sb([128, 128], BF16, "am") for _ in range(2)]
        kvt = [ph.sb([128, 128], F32, "kvt") for _ in range(4)]
        gconst = ph.sb([128, 512], F32, "gconst")
        sqb = ph.sb([128, 512], BF16, "sqb")
        yT = [ph.sb([128, 512], BF16, "yT") for _ in range(2)]
        nld = [0]

        def load_h(bi):
            b0, W = blocks[bi]
            i = nld[0] % 2
            nld[0] += 1
            R = ("X_hTb", i)
            P.dma("sp", hTb[i][:, :, 0:W], cx.hTd[:, :, b0:b0 + W].rearrange("k p n -> p k n"),
                  reads=[("hTd", q) for q in range(b0 // 512, (b0 + W + 511) // 512)], writes=[R])
            return hTb[i], R

        def proj_fm(ht, R, W, widx, bank):
            for k in range(KC):
                P.mm(cx.PS[bank][:, 0:W], wh[:, k, widx, :], ht[:, k, 0:W], k == 0, k == KC - 1, reads=[R, "X_wh"],
                     writes=[("ps", bank)])

        for hd in range(8):
            is_ret = hd < 4
            h = hd % 4
            cols = ([0, 512, 1536, 1024] if is_ret else [2048, 2560, 3072, 4096, 3584])
            for wi_, c0 in enumerate(cols):
                P.dma("pool", wh[:, :, wi_, :], cx.mix_in_w[0][:, c0 + h * 128:c0 + (h + 1) * 128]
                      .rearrange("(k p) n -> p k n", p=128), writes=["X_wh"])
            VI = 3 if is_ret else 4
            GI = 2 if is_ret else 3
            for bi, (b0, W) in enumerate(blocks):
                ht, R = load_h(bi)
                sl = slice(b0, b0 + W)
                lat = b0 >= CTX
                proj_fm(ht, R, W, 0, 0)
                if is_ret:
                    proj_fm(ht, R, W, 1, 1)
                proj_fm(ht, R, W, GI, 2)
                P.add("act", lambda e, sl=sl, W=W: e.activation(gs[:, sl], cx.PS[2][:, 0:W], AF.Silu),
                      writes=[("ps", 2), "X_gs"])
                if not is_ret:
                    P.add("act", lambda e, sl=sl, W=W: e.activation(qs[:, sl], cx.PS[0][:, 0:W], AF.Silu),
                          writes=[("ps", 0), "X_qs"])
                else:
                    for (bank, dstt, scl, Rd) in ((0, qs, 1.0, "X_qs"), (1, kkr, 128 ** -0.5, "X_kk")):
                        if not lat:
                            P.add("act", lambda e, bank=bank, dstt=dstt, scl=scl, sl=sl, W=W: e.activation(
                                dstt[:, sl], cx.PS[bank][:, 0:W], AF.Copy, scale=scl), writes=[("ps", bank), Rd])
                            continue
                        rw_ = raw[bank]
                        Rr = ("X_raw", bank)
                        P.add("act", lambda e, bank=bank, rw_=rw_, scl=scl, W=W: e.activation(
                            rw_[:, 0:W], cx.PS[bank][:, 0:W], AF.Copy, scale=scl), writes=[("ps", bank), Rr])
                        ci = (bi + bank) % 2
                        Rc = ("X_cs", ci)
                        if bank == 0:
                            P.dma("sp", csb[ci][:, :, 0:W], cx.rope_ret[:, :, b0 - CTX:b0 - CTX + W].rearrange("c p n -> p c n"),
                                  writes=[Rc])
                        else:
                            ci = bi % 2
                            Rc = ("X_cs", ci)
                        sb_ = 3
                        P.mm(cx.PS[sb_][:, 0:W], cx.perm[:], rw_[:, 0:W], True, True, reads=[Rr], writes=[("ps", sb_)])
                        P.add("pool", lambda e, rw_=rw_, ci=ci, W=W: e.tensor_tensor(F[0][:, 0:W], rw_[:, 0:W], csb[ci][:, 0, 0:W], op=ALU.mult),
                              reads=[Rr, Rc], writes=["X_F0"])
                        P.add("dve", lambda e, ci=ci, W=W, sb_=sb_: e.tensor_tensor(F[1][:, 0:W], cx.PS[sb_][:, 0:W], csb[ci][:, 1, 0:W], op=ALU.mult),
                              reads=[Rc], writes=[("ps", sb_), "X_F1"])
                        P.add("pool", lambda e, dstt=dstt, sl=sl, W=W: e.tensor_tensor(dstt[:, sl], F[0][:, 0:W], F[1][:, 0:W], op=ALU.add),
                              reads=["X_F0", "X_F1"], writes=[Rd])
                for q in range(W // 128):
                    ta = b0 // 128 + q
                    bank = 4 + ta % 2
                    for k in range(KC):
                        P.mm(cx.PS[bank][:, 0:128], ht[:, k, q * 128:(q + 1) * 128], wh[:, k, VI, :], k == 0, k == KC - 1,
                             reads=[R, "X_wh"], writes=[("ps", bank)])
                    P.add("dve", lambda e, ta=ta, bank=bank: e.tensor_copy(v_h[:, ta, :], cx.PS[bank][:, 0:128]),
                          writes=[("ps", bank), "X_v"])
            for dr in range(2):
                mh = cst[:, C_MHF:C_MHF + 128] if dr == 0 else cst[:, C_MHB:C_MHB + 128]
                if is_ret:
                    col = dr * 4 + h
                    P.add("dve", lambda e, col=col: e.tensor_copy(gconst[:], lgt[:, col:col + 1].to_broadcast([128, 512])),
                          reads=["X_lg"], writes=["X_gc"])
                def pipe(W, c0, g_ap, Rg):
                    nch = W // CH
                    v3 = lambda t, W=W: t[:, 0:W].rearrange("p (c n) -> p c n", n=CH)
                    P.add("dve", lambda e, W=W, g_ap=g_ap: e.tensor_tensor_scan(F[2][:, 0:W], cst[:, C_MSK:C_MSK + W], g_ap[:, 0:W], 0.0,
                                                                          op0=ALU.mult, op1=ALU.add),
                          reads=[Rg, "X_cst"], writes=["X_F2"])
                    tot = v3(F[2])[:, :, CH - 1:CH]
                    if dr == 0:
                        b_t = F[2]
                        Rb = "X_F2"
                    else:
                        P.add("dve", lambda e, W=W, g_ap=g_ap: e.tensor_tensor(F[3][:, 0:W], g_ap[:, 0:W], F[2][:, 0:W], op=ALU.subtract),
                              reads=[Rg, "X_F2"], writes=["X_F3"])
                        P.add("pool", lambda e, v3=v3, tot=tot, nch=nch: e.tensor_tensor(v3(F[3]), v3(F[3]), tot.to_broadcast([128, nch, CH]), op=ALU.add),
                              reads=["X_F2", "X_F3"], writes=["X_F3"])
                        b_t = F[3]
                        Rb = "X_F3"
                    rr = v3(b_t)[:, :, 31:32]
                    P.add("act", lambda e, rr=rr, c0=c0, nch=nch: e.activation(er[:, c0:c0 + nch].unsqueeze(2), rr, AF.Exp),
                          reads=[Rb], writes=["X_er"])
                    P.add("act", lambda e, tot=tot, c0=c0, nch=nch: e.activation(etad[:, c0:c0 + nch].unsqueeze(2), tot, AF.Exp),
                          reads=["X_F2"], writes=["X_etad"])
                    P.add("dve", lambda e, tot=tot, rr=rr, c0=c0, nch=nch: e.tensor_tensor(ek[:, c0:c0 + nch].unsqueeze(2), tot, rr, op=ALU.subtract),
                          reads=["X_F2", Rb], writes=["X_ek"])
                    P.add("act", lambda e, c0=c0, nch=nch: e.activation(ek[:, c0:c0 + nch], ek[:, c0:c0 + nch], AF.Exp),
                          reads=["X_ek"], writes=["X_ek"])
                    P.add("dve", lambda e, v3=v3, b_t=b_t, rr=rr, nch=nch: e.tensor_tensor(v3(F[4]), v3(b_t), rr.to_broadcast([128, nch, CH]), op=ALU.subtract),
                          reads=[Rb], writes=["X_F4"])
                    P.add("act", lambda e, W=W: e.activation(F[5][:, 0:W], F[4][:, 0:W], AF.Exp), reads=["X_F4"], writes=["X_F5"])
                    P.add("act", lambda e, W=W: e.activation(F[6][:, 0:W], F[4][:, 0:W], AF.Exp, scale=-1.0), reads=["X_F4"], writes=["X_F6"])

                if is_ret:
                    pipe(512, 0, gconst, "X_gc")
                    for tl_, Rt in ((er, "X_er"), (etad, "X_etad"), (ek, "X_ek")):
                        P.add("dve", lambda e, tl_=tl_: e.tensor_copy(tl_[:, 8:NC], tl_[:, 0:1].to_broadcast([128, NC - 8])),
                              reads=[Rt], writes=[Rt])
                for bi, (b0, W) in enumerate(blocks):
                    sl = slice(b0, b0 + W)
                    if is_ret:
                        kk_ap = kkr[:, sl]
                        Rkk = "X_kk"
                    else:
                        ht, R = load_h(bi)
                        proj_fm(ht, R, W, 1 + dr, 0)
                        P.add("act", lambda e, W=W: e.activation(F[0][:, 0:W], cx.PS[0][:, 0:W], AF.Sigmoid),
                              writes=[("ps", 0), "X_F0"])
                        P.add("dve", lambda e, W=W, h=h: e.tensor_scalar(F[0][:, 0:W], F[0][:, 0:W], lb[:, 1, h:h + 1], lb[:, 0, h:h + 1],
                                                                       op0=ALU.mult, op1=ALU.add), reads=["X_F0"] + LB, writes=["X_F0"])
                        P.add("act", lambda e, W=W: e.activation(F[1][:, 0:W], F[0][:, 0:W], AF.Ln), reads=["X_F0"], writes=["X_F1"])
                        rk = raw[bi % 2]
                        P.add("dve", lambda e, W=W, rk=rk: e.tensor_scalar(rk[:, 0:W], F[0][:, 0:W], -1.0, 1.0, op0=ALU.mult, op1=ALU.add),
                              reads=["X_F0"], writes=[("X_raw", bi % 2)])
                        kk_ap = rk[:, 0:W]
                        Rkk = ("X_raw", bi % 2)
                        pipe(W, b0 // CH, F[1], "X_F1")
                    P.add("pool", lambda e, sl=sl, W=W: e.tensor_tensor(q1[:, sl], qs[:, sl], F[5][:, 0:W], op=ALU.mult),
                          reads=["X_qs", "X_F5"], writes=["X_q1"])
                    P.add("dve", lambda e, sl=sl, W=W, kk_ap=kk_ap: e.tensor_tensor(k1[:, sl], kk_ap, F[6][:, 0:W], op=ALU.mult),
                          reads=[Rkk, "X_F6"], writes=["X_k1"])
                    bank = 6 + bi % 2
                    nq = W // 128
                    for q in range(nq):
                        P.tr(cx.PSB[bank][:, q * 128:(q + 1) * 128], k1[:, b0 + q * 128:b0 + (q + 1) * 128], cx.ident[:],
                             reads=["X_k1"], writes=[("ps", bank)])
                    P.add("act", lambda e, b0=b0, nq=nq, bank=bank: e.activation(
                        k1t[:, b0 // 128:b0 // 128 + nq, :], cx.PSB[bank][:, 0:nq * 128].rearrange("p (t d) -> p t d", d=128), AF.Copy),
                        writes=[("ps", bank), "X_k1t"])
                if dr == 0:
                    order = list(range(NA))
                else:
                    order = [1, 0] + list(range(NA - 1, 1, -1))
                halves = (0, 1) if dr == 0 else (1, 0)
                P.add("pool", lambda e: e.memset(S32[0][:], 0.0), writes=[("X_S32", 0)])
                n_t = len(order)

                def st_A(i):
                    ta = order[i]
                    for hi, hf in enumerate(halves):
                        k = 2 * i + hi
                        c = ta * 2 + hf
                        kb = k % 2
                        kt_ = kvt[k % 4]
                        Rkt = ("X_kvt", k % 4)
                        P.mm(cx.PS[kb][:, 0:128], k1t[hf * 64:(hf + 1) * 64, ta, :], v_h[hf * 64:(hf + 1) * 64, ta, :], True, True,
                             reads=["X_k1t", "X_v"], writes=[("ps", kb)])
                        P.add("act", lambda e, kt_=kt_, kb=kb, c=c: e.activation(kt_[:], cx.PS[kb][:, 0:128], AF.Copy, scale=ek[:, c:c + 1]),
                              reads=["X_ek"], writes=[("ps", kb), Rkt])
                    ab_ = 2 + i % 2
                    tsl = slice(ta * 128, (ta + 1) * 128)
                    P.mm(cx.PS[ab_][:, 0:128], k1[:, tsl], q1[:, tsl], True, True, reads=["X_k1", "X_q1"], writes=[("ps", ab_)])

                def st_B(i):
                    ta = order[i]
                    for hi, hf in enumerate(halves):
                        k = 2 * i + hi
                        c = ta * 2 + hf
                        src, dst = S32[k % 2], S32[(k + 1) % 2]
                        P.add("act", lambda e, c=c, src=src: e.activation(S_all[:, c, :], src[:], AF.Copy, scale=er[:, c:c + 1]),
                              reads=[("X_S32", k % 2), "X_er"], writes=[("X_Sall", c % 8)])
                        kt_ = kvt[k % 4]
                        P.add("dve", lambda e, kt_=kt_, c=c, src=src, dst=dst: e.scalar_tensor_tensor(
                            dst[:], src[:], etad[:, c:c + 1], kt_[:], op0=ALU.mult, op1=ALU.add),
                            reads=[("X_S32", k % 2), "X_etad", ("X_kvt", k % 4)], writes=[("X_S32", (k + 1) % 2)])
                    ab_ = 2 + i % 2
                    amt = am[i % 2]
                    P.add("dve", lambda e, amt=amt, ab_=ab_, mh=mh: e.tensor_tensor(amt[:], cx.PS[ab_][:, 0:128], mh, op=ALU.mult),
                          reads=["X_cst"], writes=[("ps", ab_), ("X_am", i % 2)])

                def st_C(i):
                    ta = order[i]
                    amt = am[i % 2]
                    ob = 4 + i % 2
                    P.mm(cx.PS[ob][:, 0:128], v_h[:, ta, :], amt[:], True, False, reads=["X_v", ("X_am", i % 2)], writes=[("ps", ob)])
                    for hf in (0, 1):
                        c = ta * 2 + hf
                        P.mm(cx.PS[ob][:, hf * 64:(hf + 1) * 64], S_all[:, c, :], q1[:, ta * 128 + hf * 64:ta * 128 + (hf + 1) * 64],
                             False, hf == 1, reads=[("X_Sall", c % 8), "X_q1"], writes=[("ps", ob)])

                def st_D(i):
                    ta = order[i]
                    ob = 4 + i % 2
                    tsl = slice(ta * 128, (ta + 1) * 128)
                    if dr == 0:
                        P.add("dve", lambda e, tsl=tsl, ob=ob: e.tensor_copy(o_acc[:, tsl], cx.PS[ob][:, 0:128]),
                              writes=[("ps", ob), "X_oacc"])
                    else:
                        P.add("dve", lambda e, tsl=tsl, ob=ob: e.tensor_tensor(o_acc[:, tsl], o_acc[:, tsl], cx.PS[ob][:, 0:128], op=ALU.add),
                              reads=["X_oacc"], writes=[("ps", ob), "X_oacc"])

                for i in range(n_t + 3):
                    if i < n_t:
                        st_A(i)
                    if 0 <= i - 1 < n_t:
                        st_B(i - 1)
                    if 0 <= i - 2 < n_t:
                        st_C(i - 2)
                    if 0 <= i - 3 < n_t:
                        st_D(i - 3)
            for bi, (b0, W) in enumerate(blocks):
                sl = slice(b0, b0 + W)
                P.add("act", lambda e, sl=sl, W=W: e.activation(sqb[:, 0:W], o_acc[:, sl], AF.Square),
                      reads=["X_oacc"], writes=["X_sqb"])
                bank = 7
                P.mm(cx.PS[bank][:, 0:W], cx.ones[:], sqb[:, 0:W], True, True, reads=["X_sqb"], writes=[("ps", bank)])
                P.add("dve", lambda e, W=W, bank=bank: e.tensor_scalar(F[7][:, 0:W], cx.PS[bank][:, 0:W], 1.0 / 128, EPS, op0=ALU.mult, op1=ALU.add),
                      writes=[("ps", bank), "X_F7"])
                P.add("act", lambda e, W=W: e.activation(F[7][:, 0:W], F[7][:, 0:W], AF.Sqrt), reads=["X_F7"], writes=["X_F7"])
                P.add("dve", lambda e, W=W: e.reciprocal(F[7][:, 0:W], F[7][:, 0:W]), reads=["X_F7"], writes=["X_F7"])
                P.add("dve", lambda e, sl=sl, W=W: e.tensor_tensor(F[7][:, 0:W], F[7][:, 0:W], o_acc[:, sl], op=ALU.mult),
                      reads=["X_F7", "X_oacc"], writes=["X_F7"])
                yt = yT[bi % 2]
                Ry = ("X_yT", bi % 2)
                P.add("pool", lambda e, sl=sl, W=W, yt=yt: e.tensor_tensor(yt[:, 0:W], F[7][:, 0:W], gs[:, sl], op=ALU.mult),
                      reads=["X_F7", "X_gs"], writes=[Ry])
                P.dma("sp", cx.yTd[hd, :, b0:b0 + W], yt[:, 0:W], reads=[Ry], writes=[("yTd", b0 // 512)])
    with Phase(cx, "X3") as p3:
        pp = PrePost(cx, p3, "X3")
        mods = {s: load_mod(cx, p3, 0, s, "X3") for s in (0, 1)}
        wo = p3.sb([128, 8, D], BF16, "wo")
        P.dma("pool", wo[:], cx.mix_out_w[0].rearrange("(h p) n -> p h n", p=128), writes=["X3_wo"])
        a_in = [p3.sb([128, 8, 128], BF16, "a_in") for _ in range(2)]
        o32 = [p3.sb([128, D], F32, "o32") for _ in range(2)]
        def x3_load(idx):
            s_, ti_ = tiles[idx]
            P.dma("sp", a_in[idx % 2][:], cx.yTd[:, :, idx * 128:(idx + 1) * 128].rearrange("h p n -> p h n"),
                  reads=[("yTd", q) for q in range(0, (TA + 511) // 512)], writes=[("X3_a", idx % 2)])
            pp.load(src[s_][0][ti_ * 128:(ti_ + 1) * 128, :], (src[s_][1], ti_))

        x3_load(0)
        for idx, (s, ti) in enumerate(tiles):
            ab = idx % 2
            modt, modr = mods[s]
            sap, sres = src[s]
            dap, dres = dst[s]
            if idx + 1 < len(tiles):
                x3_load(idx + 1)
            for mh_ in range(2):
                bank = 2 * ab + mh_
                for hh in range(8):
                    P.mm(cx.PS[bank][:], a_in[ab][:, hh, :], wo[:, hh, mh_ * 512:(mh_ + 1) * 512], hh == 0, hh == 7,
                         reads=[("X3_a", ab), "X3_wo"], writes=[("ps", bank)])
                P.add("act", lambda e, ab=ab, mh_=mh_, bank=bank: e.activation(o32[ab][:, mh_ * 512:(mh_ + 1) * 512], cx.PS[bank][:], AF.Copy),
                      writes=[("ps", bank), ("X3_o", ab)])
            pp.post(o32[ab][:], ("X3_o", ab), sap[ti * 128:(ti + 1) * 128, :], (sres, ti), modt[:, MOD_G, :], modr,
                    dap[ti * 128:(ti + 1) * 128, :], (dres, ti))


ALL_PHASES = ("mix0", "ffn0", "mla", "moe")

W_SHAPES = {
    "mod_w": [2, D, 6 * D], "mod_b": [2, 6 * D], "norm_g": [2, 4, D], "mix_in_w": [1, D, 4608],
    "ret_decay_logit": [1, 2, 4], "mix_out_w": [1, D, D], "ffn_in_w": [1, D, 2 * FFN], "ffn_out_w": [1, FFN, D],
    "mla_down_w": [1, D, 704], "mla_q_norm_g": [1, 384], "mla_kv_norm_g": [1, 256], "mla_uq_w": [1, 384, 1536],
    "mla_ukv_w": [1, 256, 2048], "mla_out_w": [1, D, D], "router_w": [1, D, NE], "moe_in_w": [1, NE, D, 2 * FFN],
    "moe_out_w": [1, NE, FFN, D],
}


def build(T, phases=ALL_PHASES):
    nc = bass.Bass("TRN2", target_bir_lowering=False)
    cx = Cx()
    cx.nc = nc
    cx.T, cx.TA, cx.NT, cx.NA = T, T + CTX, T // 128, (T + CTX) // 128
    TA = cx.TA

    def din(name, shape, dt=F32):
        return nc.dram_tensor(name, list(shape), dt, kind="ExternalInput").ap()

    def dscr(name, shape, dt=F32):
        return nc.dram_tensor(name, list(shape), dt, kind="Internal").ap()

    cx.x = din("x", [T, D])
    cx.ctx = din("ctx", [CTX, D])
    cx.ccol = din("ccol", [128, 16])
    for k, shp in W_SHAPES.items():
        setattr(cx, k, din(k, shp))
    cx.hgrn_lbl = din("hgrn_lbl", [128, 2, 4])
    cx.cst = din("cst", [128, C_END])
    cx.mats = din("mats", [3, 128, 128])
    cx.rope_ret = din("rope_ret", [2, 128, T])
    cx.rope_mla = din("rope_mla", [T, 64])
    cx.out = nc.dram_tensor("out", [T, D], F32, kind="ExternalOutput").ap()
    cx.modb = dscr("modb", [2, 2, 6, 128, D])
    cx.rx = dscr("rx", [T, D])
    cx.rc = dscr("rc", [CTX, D])
    cx.hTd = dscr("hTd", [KC, 128, TA], BF16)
    cx.yTd = dscr("yTd", [8, 128, TA], BF16)
    import os
    DBG = os.environ.get("KDBG", "")
    cx.attnT = (nc.dram_tensor("attnT", [8, 128, T], BF16, kind="ExternalOutput").ap() if DBG == "attnT"
                else dscr("attnT", [8, 128, T], BF16))
    P = cx.P = Prog(nc)
    if DBG == "attnT":
        cx.dbg = {"qnT": nc.dram_tensor("d_qnT", [128, 3, T], BF16, kind="ExternalOutput").ap(),
                  "ckvT": nc.dram_tensor("d_ckvT", [128, 2, TA], BF16, kind="ExternalOutput").ap(),
                  "krT": nc.dram_tensor("d_krT", [128, TA], BF16, kind="ExternalOutput").ap(),
                  "qrT": nc.dram_tensor("d_qrT", [128, 4, T], BF16, kind="ExternalOutput").ap()}
    with contextlib.ExitStack() as gst:
        cx.PS = [gst.enter_context(nc.psum_tensor("ps%d" % b, [128, 512], F32)) for b in range(8)]
        cx.PSB = [p[:].bitcast(BF16) for p in cx.PS]
        mt = gst.enter_context(nc.sbuf_tensor("mats_sb", [128, 3, 128], BF16))
        P.dma("pool", mt[:], cx.mats.rearrange("i p n -> p i n"), writes=["mats"])
        P.barrier()
        cx.ident, cx.ones, cx.perm = mt[:, 0, :], mt[:, 1, :], mt[:, 2, :]
        phase_mod(cx)
        cur = {0: (cx.x, "x_in"), 1: (cx.ctx, "c_in")}
        res = {0: (cx.rx, "rx"), 1: (cx.rc, "rc")}
        for i, phn in enumerate(phases):
            last = i == len(phases) - 1
            dst = dict(res)
            if last:
                dst[0] = (cx.out, "out")
            if phn == "mix0":
                phase_mix0(cx, cur, dst)
            elif phn == "ffn0":
                phase_ffn(cx, 0, cx.ffn_in_w, cx.ffn_out_w, None, 1, cur, dst, True, "F0")
            elif phn == "mla":
                phase_mla(cx, 1, cur, dst)
            elif phn == "moe":
                phase_ffn(cx, 1, cx.moe_in_w[0], cx.moe_out_w[0], cx.router_w[0], NE, cur, dst, False, "F1")
            if phn in ("mix0", "ffn0"):
                cur = dict(dst)
            else:
                cur = {0: dst[0], 1: cur[1]}
        P.emit(final_wait_resources=[("out", t) for t in range(cx.NT)])
    cx.stats = P.stats
    return nc, cx


def rope_tables(T, dim):
    n_rows = T // GRID_W
    rows = np.repeat(np.arange(n_rows), GRID_W).astype(np.float32)
    cols = np.tile(np.arange(GRID_W), n_rows).astype(np.float32)
    n_freq = dim // 4
    inv = (np.float32(10000.0) ** (-np.arange(n_freq, dtype=np.float32) / np.float32(n_freq))).astype(np.float32)
    ang = np.concatenate([rows[:, None] * inv, cols[:, None] * inv], axis=-1).astype(np.float32)
    return np.cos(ang).astype(np.float32), np.sin(ang).astype(np.float32)


def const_inputs(T):
    p = np.arange(128)
    same = (p[:, None] // CH) == (p[None, :] // CH)
    cst = np.zeros((128, C_END), np.float32)
    cst[:, C_MHF:C_MHF + 128] = (same & (p[:, None] <= p[None, :])).astype(np.float32)
    cst[:, C_MHB:C_MHB + 128] = (same & (p[:, None] >= p[None, :])).astype(np.float32)
    msk = np.ones(512, np.float32)
    msk[::CH] = 0.0
    cst[:, C_MSK:C_MSK + 512] = msk[None, :]
    mats = np.zeros((3, 128, 128), np.float32)
    mats[0] = np.eye(128)
    mats[1] = 1.0
    mats[2][(p + 64) % 128, p] = 1.0
    cr, sr = rope_tables(T, 128)
    rope_ret = np.stack([np.concatenate([cr.T, cr.T], 0), np.concatenate([-sr.T, sr.T], 0)]).astype(np.float32)
    cm, sm = rope_tables(T, 64)
    rope_mla = np.concatenate([cm, sm], axis=1).astype(np.float32)
    return {"cst": cst, "mats": mats, "rope_ret": np.ascontiguousarray(rope_ret), "rope_mla": np.ascontiguousarray(rope_mla)}


def core_inputs(inputs, b, consts):
    f = lambda a: np.ascontiguousarray(np.asarray(a, dtype=np.float32))
    m = {"x": f(inputs["x"][b]), "ctx": f(inputs["ctx"][b])}
    c = f(inputs["c"][b]).reshape(8, 128).T
    cc = f(inputs["c_ctx"]).reshape(8, 128).T
    m["ccol"] = np.ascontiguousarray(np.concatenate([c, cc], axis=1))
    for k in W_SHAPES:
        m[k] = f(inputs[k])
    m["hgrn_lbl"] = np.ascontiguousarray(f(inputs["hgrn_lb_logit"]).reshape(2, 4, 128).transpose(2, 0, 1))
    m.update(consts)
    return m


_CACHE = {}


def kernel(**inputs):
    T = int(np.asarray(inputs["x"]).shape[1])
    B = int(np.asarray(inputs["x"]).shape[0])
    if T not in _CACHE:
        _CACHE[T] = build(T)[0]
    nc = _CACHE[T]
    consts = const_inputs(T)
    in_maps = [core_inputs(inputs, b, consts) for b in range(B)]
    res = run_bass_kernel_spmd(nc, in_maps, core_ids=list(range(B)))
    return np.stack([np.asarray(r["out"], dtype=np.float32) for r in res.results], axis=0)
```
